# Optimizing a Trainium2 kernel written in Bass

```python
import jax, jax.numpy as jnp
from jax import lax
import numpy as np

D_MODEL = 2048
BATCH = 2
SEQ = 4096
DEPTH = 1
DEC_BATCH = 8
DEC_SEQ = 8
PAST_LEN = 16384
PAGE_SIZE = 128

DILATED_GROUPS = ((128, 1), (512, 4), (2048, 16))
N_GROUPS = len(DILATED_GROUPS)
HEAD_DIM = 128
N_HEADS = D_MODEL // (2 * HEAD_DIM)
ATTN_WIDTH = N_GROUPS * N_HEADS * HEAD_DIM
ATTN_OUT = N_HEADS * HEAD_DIM
D_CONV = 3 * D_MODEL // 4
CONV_K = 31
D_IN = 3 * ATTN_WIDTH + 2 * D_CONV + 2 * D_MODEL
N_EXPERTS = 32
TOP_K = 4
D_FF = D_MODEL
SWIGLU_ALPHA = 1.702
SWIGLU_LIMIT = 7.0
N_MOD = 6
Q_BLOCK = 128
NORM_EPS = 1e-6
NEG_INF = -1e30

kernel_name = 'hybrid_dilated_attn_conformer_moe_decode_step'


def _rmsnorm(x, g):
    xf = x.astype(jnp.float32)
    y = xf * lax.rsqrt(jnp.mean(xf * xf, axis=-1, keepdims=True) + NORM_EPS)
    return (y * g.astype(jnp.float32)).astype(x.dtype)


def _layernorm(x, g, b):
    xf = x.astype(jnp.float32)
    mu = jnp.mean(xf, axis=-1, keepdims=True)
    xc = xf - mu
    var = jnp.mean(xc * xc, axis=-1, keepdims=True)
    y = xc * lax.rsqrt(var + NORM_EPS) * g.astype(jnp.float32) + b.astype(jnp.float32)
    return y.astype(x.dtype)


def _adaln(c, w_ada, b_ada):
    return jnp.split(jax.nn.silu(c) @ w_ada + b_ada, N_MOD, axis=-1)


def _modulate(h, shift, scale):
    return h * (1 + scale[:, None, :]) + shift[:, None, :]


def _split_in(z):
    cuts = [ATTN_WIDTH, 2 * ATTN_WIDTH, 3 * ATTN_WIDTH, 3 * ATTN_WIDTH + 2 * D_CONV]
    q, k, v, u2, gates = jnp.split(z, cuts, axis=-1)
    heads = lambda t: t.reshape(t.shape[0], t.shape[1], N_GROUPS, N_HEADS, HEAD_DIM)
    ua, ub = jnp.split(u2, 2, axis=-1)
    return heads(q), heads(k), heads(v), ua * jax.nn.sigmoid(ub), gates


def _dilated_group(q, k_ctx, v_ctx, pos, ctx_start, window, dilation):
    offsets = jnp.arange(window // dilation + 1, dtype=jnp.int32) * dilation
    key_pos = pos[:, None] - offsets[None, :]
    valid = key_pos >= 0
    idx = jnp.clip(key_pos - ctx_start, 0, k_ctx.shape[1] - 1)
    k = jnp.take(k_ctx, idx, axis=1)
    v = jnp.take(v_ctx, idx, axis=1)
    logits = jnp.einsum('bqhd,bqjhd->bqhj', q, k, preferred_element_type=jnp.float32) * (HEAD_DIM ** -0.5)
    return jnp.where(valid[None, :, None, :], logits, NEG_INF), v


def _dilated_mixture(q, kv_ctx, pos, starts):
    m = jnp.full(q.shape[:2] + (N_HEADS,), NEG_INF, jnp.float32)
    den = jnp.zeros(q.shape[:2] + (N_HEADS,), jnp.float32)
    num = jnp.zeros(q.shape[:2] + (N_HEADS, HEAD_DIM), jnp.float32)
    for g, ((window, dilation), (k_ctx, v_ctx), start) in enumerate(zip(DILATED_GROUPS, kv_ctx, starts)):
        logits, v = _dilated_group(q[:, :, g], k_ctx, v_ctx, pos, start, window, dilation)
        m_new = jnp.maximum(m, jnp.max(logits, axis=-1))
        p = jnp.exp(logits - m_new[..., None])
        corr = jnp.exp(m - m_new)
        den = den * corr + jnp.sum(p, axis=-1)
        num = num * corr[..., None] + jnp.einsum('bqhj,bqjhd->bqhd', p, v, preferred_element_type=jnp.float32)
        m = m_new
    return num / den[..., None]


def _prompt_attention(q, k, v):
    b, s = q.shape[:2]
    n_blocks = s // Q_BLOCK
    kv_ctx = [(k[:, :, g], v[:, :, g]) for g in range(N_GROUPS)]
    starts = [0] * N_GROUPS
    q_blocks = q.reshape(b, n_blocks, Q_BLOCK, N_GROUPS, N_HEADS, HEAD_DIM).swapaxes(0, 1)
    pos_blocks = jnp.arange(s, dtype=jnp.int32).reshape(n_blocks, Q_BLOCK)
    o = lax.map(lambda qp: _dilated_mixture(qp[0], kv_ctx, qp[1], starts), (q_blocks, pos_blocks))
    return o.swapaxes(0, 1).reshape(b, s, ATTN_OUT).astype(q.dtype)


def _sample_attention(q, k, v, caches):
    pos = PAST_LEN + jnp.arange(q.shape[1], dtype=jnp.int32)
    kv_ctx, starts = [], []
    for g, cache in enumerate(caches):
        kv_ctx.append((jnp.concatenate([cache[:, :, 0], k[:, :, g]], axis=1),
                       jnp.concatenate([cache[:, :, 1], v[:, :, g]], axis=1)))
        starts.append(PAST_LEN - cache.shape[1])
    o = _dilated_mixture(q, kv_ctx, pos, starts)
    return o.reshape(q.shape[0], q.shape[1], ATTN_OUT).astype(q.dtype)


def _conv_branch(ctx, w_dw, b_dw, g_ln, b_ln):
    y = lax.conv_general_dilated(ctx, w_dw[:, None, :], window_strides=(1,), padding='VALID',
                                 dimension_numbers=('NWC', 'WIO', 'NWC'),
                                 feature_group_count=ctx.shape[-1]) + b_dw
    return jax.nn.silu(_layernorm(y, g_ln, b_ln))


def _merge(attn_o, conv_f, gates, w_attn_out, b_attn_out, w_conv_out, b_conv_out, w_o):
    a = attn_o @ w_attn_out + b_attn_out
    cb = conv_f @ w_conv_out + b_conv_out
    ga, gb = jnp.split(gates, 2, axis=-1)
    return (jax.nn.sigmoid(ga) * a + jax.nn.sigmoid(gb) * cb) @ w_o


def _moe(h, w_router, b_router, w1, b1, w2, b2):
    logits = (h @ w_router + b_router).astype(jnp.float32)
    top_val, top_idx = lax.top_k(logits, TOP_K)
    top_w = jax.nn.softmax(top_val, axis=-1)
    gate = jnp.sum(jax.nn.one_hot(top_idx, N_EXPERTS, dtype=jnp.float32) * top_w[..., None], axis=1)
    out = jnp.zeros(h.shape, jnp.float32)
    for e in range(N_EXPERTS):
        z = h @ w1[e] + b1[e]
        z_glu = jnp.minimum(z[:, :D_FF], SWIGLU_LIMIT)
        z_lin = jnp.clip(z[:, D_FF:], -SWIGLU_LIMIT, SWIGLU_LIMIT)
        act = z_glu * jax.nn.sigmoid(SWIGLU_ALPHA * z_glu) * (z_lin + 1)
        out = out + gate[:, e:e + 1] * (act @ w2[e] + b2[e])
    return out.astype(h.dtype)


def setup_inputs(seed: int = 0) -> dict:
    key = jax.random.key(seed)
    ks = iter(jax.random.split(key, 40))
    nrm = lambda shape, s: s * jax.random.normal(next(ks), shape, jnp.float32)
    L, D = DEPTH, D_MODEL
    inp = {}
    inp['x_prompt'] = nrm((BATCH, SEQ, D), 1.0)
    inp['x_sample'] = nrm((DEC_BATCH, DEC_SEQ, D), 1.0)
    for w, _ in DILATED_GROUPS:
        inp['cache_kv_w%d' % w] = nrm((L, DEC_BATCH, min(w, PAST_LEN), 2, N_HEADS, HEAD_DIM), 1.0)
    inp['state_conv'] = nrm((L, DEC_BATCH, CONV_K - 1, D_CONV), 0.5)
    inp['c_prompt'] = nrm((BATCH, D), 1.0)
    inp['c_sample'] = nrm((DEC_BATCH, D), 1.0)
    inp['w_ada'] = nrm((L, D, N_MOD * D), 0.5 * D ** -0.5)
    inp['b_ada'] = nrm((L, N_MOD * D), 0.02)
    inp['g_norm1'] = 1.0 + nrm((L, D), 0.05)
    inp['w_in'] = nrm((L, D, D_IN), D ** -0.5)
    inp['b_in'] = nrm((L, D_IN), 0.02)
    inp['w_dw'] = nrm((L, CONV_K, D_CONV), CONV_K ** -0.5)
    inp['b_dw'] = nrm((L, D_CONV), 0.02)
    inp['g_ln_conv'] = 1.0 + nrm((L, D_CONV), 0.05)
    inp['b_ln_conv'] = nrm((L, D_CONV), 0.02)
    inp['w_conv_out'] = nrm((L, D_CONV, D), D_CONV ** -0.5)
    inp['b_conv_out'] = nrm((L, D), 0.02)
    inp['w_attn_out'] = nrm((L, ATTN_OUT, D), ATTN_OUT ** -0.5)
    inp['b_attn_out'] = nrm((L, D), 0.02)
    inp['w_o'] = nrm((L, D, D), D ** -0.5)
    inp['g_norm2'] = 1.0 + nrm((L, D), 0.05)
    inp['w_router'] = nrm((L, D, N_EXPERTS), D ** -0.5)
    inp['b_router'] = nrm((L, N_EXPERTS), 0.01)
    inp['w_moe1'] = nrm((L, N_EXPERTS, D, 2 * D_FF), D ** -0.5)
    inp['b_moe1'] = nrm((L, N_EXPERTS, 2 * D_FF), 0.02)
    inp['w_moe2'] = nrm((L, N_EXPERTS, D_FF, D), D_FF ** -0.5)
    inp['b_moe2'] = nrm((L, N_EXPERTS, D), 0.02)
    inp['g_final'] = 1.0 + nrm((D,), 0.05)
    return inp


def reference(x_prompt, x_sample, cache_kv_w128, cache_kv_w512, cache_kv_w2048, state_conv,
              c_prompt, c_sample, w_ada, b_ada, g_norm1, w_in, b_in, w_dw, b_dw, g_ln_conv, b_ln_conv,
              w_conv_out, b_conv_out, w_attn_out, b_attn_out, w_o, g_norm2, w_router, b_router,
              w_moe1, b_moe1, w_moe2, b_moe2, g_final):
    xp, xs = x_prompt, x_sample
    caches = (cache_kv_w128, cache_kv_w512, cache_kv_w2048)
    n_p = xp.shape[0] * xp.shape[1]
    new_kv_p = [[] for _ in DILATED_GROUPS]
    new_kv_s = [[] for _ in DILATED_GROUPS]
    new_conv_p, new_conv_s = [], []
    for l in range(DEPTH):
        mod_p = _adaln(c_prompt, w_ada[l], b_ada[l])
        mod_s = _adaln(c_sample, w_ada[l], b_ada[l])
        hp = _modulate(_rmsnorm(xp, g_norm1[l]), mod_p[0], mod_p[1])
        hs = _modulate(_rmsnorm(xs, g_norm1[l]), mod_s[0], mod_s[1])
        qp, kp, vp, up, gp = _split_in(hp @ w_in[l] + b_in[l])
        qs, ks, vs, us, gs = _split_in(hs @ w_in[l] + b_in[l])
        ap = _prompt_attention(qp, kp, vp)
        a_s = _sample_attention(qs, ks, vs, [c[l] for c in caches])
        ctx_p = jnp.concatenate([jnp.zeros((up.shape[0], CONV_K - 1, D_CONV), up.dtype), up], axis=1)
        ctx_s = jnp.concatenate([state_conv[l], us], axis=1)
        fp = _conv_branch(ctx_p, w_dw[l], b_dw[l], g_ln_conv[l], b_ln_conv[l])
        fs = _conv_branch(ctx_s, w_dw[l], b_dw[l], g_ln_conv[l], b_ln_conv[l])
        for g, (w, _) in enumerate(DILATED_GROUPS):
            new_kv_p[g].append(jnp.stack([kp[:, :, g], vp[:, :, g]], axis=2)[:, -min(w, kp.shape[1]):])
            new_kv_s[g].append(jnp.stack([ks[:, :, g], vs[:, :, g]], axis=2))
        new_conv_p.append(ctx_p[:, -(CONV_K - 1):])
        new_conv_s.append(ctx_s[:, -(CONV_K - 1):])
        xp = xp + mod_p[2][:, None, :] * _merge(ap, fp, gp, w_attn_out[l], b_attn_out[l], w_conv_out[l], b_conv_out[l], w_o[l])
        xs = xs + mod_s[2][:, None, :] * _merge(a_s, fs, gs, w_attn_out[l], b_attn_out[l], w_conv_out[l], b_conv_out[l], w_o[l])
        hp = _modulate(_rmsnorm(xp, g_norm2[l]), mod_p[3], mod_p[4])
        hs = _modulate(_rmsnorm(xs, g_norm2[l]), mod_s[3], mod_s[4])
        h_all = jnp.concatenate([hp.reshape(-1, D_MODEL), hs.reshape(-1, D_MODEL)], axis=0)
        m = _moe(h_all, w_router[l], b_router[l], w_moe1[l], b_moe1[l], w_moe2[l], b_moe2[l])
        xp = xp + mod_p[5][:, None, :] * m[:n_p].reshape(xp.shape)
        xs = xs + mod_s[5][:, None, :] * m[n_p:].reshape(xs.shape)
    y_prompt = _rmsnorm(xp, g_final)
    y_sample = _rmsnorm(xs, g_final)
    return (y_prompt, y_sample,
            jnp.stack(new_kv_p[0]), jnp.stack(new_kv_p[1]), jnp.stack(new_kv_p[2]), jnp.stack(new_conv_p),
            jnp.stack(new_kv_s[0]), jnp.stack(new_kv_s[1]), jnp.stack(new_kv_s[2]), jnp.stack(new_conv_s))
```

```python
import numpy as np
from contextlib import ExitStack
import concourse.bass as bass
import concourse.mybir as mybir
from concourse.bass_utils import run_bass_kernel_spmd

dt = mybir.dt
F32, BF16, I32, U32 = dt.float32, dt.bfloat16, dt.int32, dt.uint32
AF = mybir.ActivationFunctionType
ALU = mybir.AluOpType

D = 2048
NT = 1024
NS = 8
NTS = NT + NS
HALO = 2048
NCORES = 8
CAP = 256
import os
DBG = os.environ.get('KDBG', '')


class T:
    def __init__(self, h, name):
        self.h = h
        self.name = name
        self.w = {}
        self.r = {}
        self.dsem = {}
        self.excl = False

    def __getitem__(self, k):
        return self.h[k]


class KB:
    def __init__(self, nc, es):
        self.nc = nc
        self.es = es
        self.eng = dict(pe=nc.tensor, act=nc.scalar, dve=nc.vector, pool=nc.gpsimd, sp=nc.sync)
        self.sems = []
        self.semcnt = []
        self.esem = {}
        for k in self.eng:
            self.esem[k] = self.new_sem('e_' + k)
        self.cnt = {k: 0 for k in self.eng}
        self.waited = {k: {} for k in self.eng}
        self.dfree = {'sw': [self.new_sem('dsw%d' % i) for i in range(48)], 'hw': [self.new_sem('dhw%d' % i) for i in range(44)]}
        self.dused = []
        self.uid = 0

    def new_sem(self, name):
        s = self.es.enter_context(self.nc.semaphore(name))
        self.sems.append(s)
        self.semcnt.append(0)
        return len(self.sems) - 1

    def sb(self, name, shape, dtype=F32, es=None):
        self.uid += 1
        es = es or self.es
        return T(es.enter_context(self.nc.sbuf_tensor("%s_%d" % (name, self.uid), list(shape), dtype)), name)

    def ps(self, name, shape, dtype=F32, es=None):
        self.uid += 1
        es = es or self.es
        t = T(es.enter_context(self.nc.psum_tensor("%s_%d" % (name, self.uid), list(shape), dtype)), name)
        t.excl = True
        return t

    def dram(self, name, shape, dtype=F32, kind="Internal"):
        return T(self.nc.dram_tensor(name, list(shape), dtype, kind=kind).ap(), name)

    def _wait(self, e, deps, skip=None):
        wd = self.waited[e]
        for si, v in deps.items():
            if si == skip or wd.get(si, 0) >= v:
                continue
            self.eng[e].wait_ge(self.sems[si], v)
            wd[si] = v

    @staticmethod
    def _merge(d, o):
        for k, v in o.items():
            if d.get(k, 0) < v:
                d[k] = v

    def _deps(self, reads, writes):
        deps = {}
        for t in reads:
            self._merge(deps, t.w)
            if t.excl:
                self._merge(deps, t.r)
        for t in writes:
            self._merge(deps, t.w)
            self._merge(deps, t.r)
        return deps

    def _post(self, ev, reads, writes):
        for t in writes:
            t.w = dict(ev)
            t.r = {}
        for t in reads:
            if t not in writes:
                self._merge(t.r, ev)

    def op(self, e, fn, reads=(), writes=()):
        si = self.esem[e]
        self._wait(e, self._deps(reads, writes), skip=si if e == 'pe' else None)
        inst = fn(self.eng[e])
        self.cnt[e] += 1
        self.semcnt[si] = self.cnt[e]
        inst.then_inc(self.sems[si], 1)
        self._post({si: self.cnt[e]}, reads, writes)
        return inst

    def dma(self, q, out, in_, reads=(), writes=(), prim=None, fn=None):
        kind = 'sw' if q == 'pool' else 'hw'
        if kind not in prim.dsem:
            prim.dsem[kind] = self.dfree[kind].pop()
            if prim not in self.dused:
                self.dused.append(prim)
        si = prim.dsem[kind]
        self._wait(q, self._deps(reads, writes), skip=si if prim in writes else None)
        inst = self.eng[q].dma_start(out=out, in_=in_) if fn is None else fn(self.eng[q])
        self.semcnt[si] += 16
        inst.then_inc(self.sems[si], 16)
        self._post({si: self.semcnt[si]}, reads, writes)
        return inst

    def barrier(self, keep=()):
        allv = {si: v for si, v in enumerate(self.semcnt) if v > 0}
        for e in self.eng:
            self._wait(e, allv, skip=self.esem[e])
        rest = []
        for t in self.dused:
            if t in keep:
                rest.append(t)
            else:
                for kind, si in t.dsem.items():
                    self.dfree[kind].append(si)
                t.dsem = {}
        self.dused = rest

    def finish(self):
        allv = {si: v for si, v in enumerate(self.semcnt) if v > 0}
        for e in self.eng:
            self._wait(e, allv, skip=self.esem[e])


def build():
    nc = bass.Bass("TRN2", target_bir_lowering=False)
    es = ExitStack()
    kb = KB(nc, es)
    IN = lambda n, s, d=F32: kb.dram(n, s, d, "ExternalInput")
    OUT = lambda n, s, d=F32: kb.dram(n, s, d, "ExternalOutput")
    xh = IN("xh", [HALO + NT, D]); xs = IN("xs", [NS, D]); cT = IN("cT", [128, 16, 2])
    ident_d = IN("ident", [128, 128])
    w_ada = IN("w_ada", [D, 6 * D]); b_adaT = IN("b_adaT", [128, 96]); b_ada_row = IN("b_ada_row", [1, 6 * D])
    g1T = IN("g1T", [128, 16])
    M1_d = IN("M1", [128, 256]); kval_d = IN("kval", [128, 53]); SMc_d = IN("SMc", [128, 21, NS]); SMn_d = IN("SMn", [NS, 3, NS])
    cache_d = [IN("cache%d" % g, [w, 2, 8, 128]) for g, w in enumerate((128, 512, 2048))]
    w_dwT = IN("w_dwT", [128, 12, 31]); cparT = IN("cparT", [128, 3, 12]); hprev_d = IN("hprev", [128, 1])
    stT_d = IN("stT", [1536, 30]); st_d = IN("st", [30, 1536])
    w_ao = IN("w_ao", [1024, D]); w_co = IN("w_co", [1536, D]); boT = IN("boT", [128, 2, 16]); w_o = IN("w_o", [D, D])
    g2_row = IN("g2_row", [1, D]); w_router = IN("w_router", [D, 32]); b_router = IN("b_router", [1, 32])
    w_moe1 = IN("w_moe1", [32, D, 2 * D]); b1T_d = IN("b1T", [128, 32, 32]); w_moe2 = IN("w_moe2", [32, D, D]); b_moe2 = IN("b_moe2", [32, D]); gf_row = IN("gf_row", [1, D])
    w_in = IN("w_in", [D, 16384]); b_inT = IN("b_inT", [128, 128]); b_in_row = IN("b_in_row", [1, 16384])
    y_o = OUT("y_o", [NT, D]); ys_o = OUT("ys_o", [NS, D])
    kv_o = [OUT("kv1_o", [128, 2, 8, 128]), OUT("kv2_o", [512, 2, 8, 128]), OUT("kv3_o", [1024, 2, 8, 128])]
    kvs_o = OUT("kvs_o", [3, NS, 2, 8, 128])
    convp_o = OUT("convp_o", [30, 1536]); convs_o = OUT("convs_o", [30, 1536])
    GH = [128, 512, 2048]
    NCTX = [GH[g] + NT for g in range(3)]
    kT_d = [kb.dram("kT_d%d" % g, [8, 128, NCTX[g]], BF16) for g in range(3)]
    V_d = [kb.dram("V_d%d" % g, [NCTX[g], 8, 128], BF16) for g in range(3)]
    qT_d = kb.dram("qT_d", [3, 8, 128, NT], BF16)
    qsT_d = kb.dram("qsT_d", [3, 8, 128, NS], BF16); ksT_d = kb.dram("ksT_d", [3, 8, 128, NS], BF16)
    Vs_d = kb.dram("Vs_d", [3, NS, 8, 128], BF16)
    u_d = kb.dram("u_d", [1536, 128 + NT]); us_d = kb.dram("us_d", [1536, NS])
    sg_d = kb.dram("sg_d", [4096, NTS])
    mod_d = kb.dram("mod_d", [2, 6 * D])

    ident = kb.sb("ident", [128, 128]); kb.dma('sp', ident[:], ident_d[:], [ident_d], [ident], prim=ident)
    identb = kb.sb("identb", [128, 128], BF16)
    kb.op('dve', lambda e: e.tensor_copy(out=identb[:], in_=ident[:]), [ident], [identb])
    A1 = kb.sb("A1", [128, 16, 2]); B1 = kb.sb("B1", [128, 16, 2])
    PS = [kb.ps("ps%d" % i, [128, 512]) for i in range(8)]

    ph = ExitStack()
    sil = kb.sb("sil", [128, 16, 2], F32, ph); silb = kb.sb("silb", [128, 16, 2], BF16, ph)
    badT = kb.sb("badT", [128, 96], F32, ph); g1s = kb.sb("g1s", [128, 16], F32, ph)
    modT = kb.sb("modT", [128, 32, 2], F32, ph)
    kb.dma('sp', sil[:], cT[:], [cT], [sil], prim=sil)
    kb.dma('sp', badT[:], b_adaT[:], [b_adaT], [badT], prim=badT)
    kb.dma('sp', g1s[:], g1T[:], [g1T], [g1s], prim=g1s)
    kb.op('act', lambda e: e.activation(out=silb[:], in_=sil[:], func=AF.Silu), [sil], [silb])
    stg = [kb.sb("stg%d" % i, [128, 16, 256], F32, ph) for i in range(2)]
    KSPL = [(0, 6, 'act'), (6, 12, 'dve'), (12, 16, 'pool')]
    wbs = [[kb.sb("wb%d_%d" % (i, j), [128, KSPL[j][1] - KSPL[j][0], 256], BF16, ph) for j in range(3)] for i in range(2)]

    def cast_block(stage, wb3, nk=16, n=256):
        for j, (k0, k1, e) in enumerate(KSPL):
            k1 = min(k1, nk)
            if k0 >= k1:
                continue
            if e == 'act':
                kb.op('act', lambda en, k0=k0, k1=k1, j=j: en.activation(out=wb3[j][:, 0:k1 - k0, 0:n], in_=stage[:, k0:k1, 0:n], func=AF.Copy), [stage], [wb3[j]])
            else:
                kb.op(e, lambda en, k0=k0, k1=k1, j=j: en.tensor_copy(out=wb3[j][:, 0:k1 - k0, 0:n], in_=stage[:, k0:k1, 0:n]), [stage], [wb3[j]])

    class WP:
        def __init__(self, t, kl):
            self.t, self.kl = t, kl

        def __getitem__(self, key):
            p, k, c = key
            return self.t[p, self.kl, c]

    def wpart(wb3, k):
        for j, (k0, k1, e) in enumerate(KSPL):
            if k0 <= k < k1:
                return WP(wb3[j], k - k0)

    def load_block(w_ap2d, col0, n, stage, nk=16):
        kb.dma('sp', stage[:, 0:nk, 0:n], w_ap2d[:, col0:col0 + n].rearrange("(k p) n -> p k n", p=128), [], [stage], prim=stage)

    modrow = kb.sb("modrow", [2, 256], F32, ph); badrow = kb.sb("badrow", [2, 6 * D], F32, ph)
    kb.dma('pool', badrow[:], b_ada_row[:].partition_broadcast(2), [b_ada_row], [badrow], prim=badrow)
    nblk = 6 * D // 256
    load_block(w_ada, 0, 256, stg[0])
    for bi in range(nblk):
        st, wb3 = stg[bi % 2], wbs[bi % 2]
        if bi + 1 < nblk:
            load_block(w_ada, (bi + 1) * 256, 256, stg[(bi + 1) % 2])
        cast_block(st, wb3)
        if bi < 16:
            for sub in range(2):
                p = PS[sub]
                for k in range(16):
                    wp = wpart(wb3, k)
                    kb.op('pe', lambda e, k=k, wp=wp, sub=sub, p=p: e.matmul(p[:, 0:2], wp[:, k, sub * 128:(sub + 1) * 128], silb[:, k, :], start=(k == 0), stop=(k == 15)), [wp.t, silb], [p])
                blk = bi * 2 + sub
                kb.op('dve', lambda e, p=p, blk=blk: e.tensor_scalar(out=modT[:, blk, :], in0=p[:, 0:2], scalar1=badT[:, blk:blk + 1], scalar2=None, op0=ALU.add), [p, badT], [modT])
        else:
            p = PS[2 + bi % 2]
            for k in range(16):
                wp = wpart(wb3, k)
                kb.op('pe', lambda e, k=k, wp=wp, p=p: e.matmul(p[0:2, 0:256], silb[:, k, :], wp[:, k, :], start=(k == 0), stop=(k == 15)), [wp.t, silb], [p])
            kb.op('dve', lambda e, p=p, bi=bi: e.tensor_tensor(out=modrow[:], in0=p[0:2, 0:256], in1=badrow[:, bi * 256:(bi + 1) * 256], op=ALU.add), [p, badrow], [modrow])
            kb.dma('pool', mod_d[:, bi * 256:(bi + 1) * 256], modrow[:], [modrow], [mod_d], prim=modrow)
    for c in range(2):
        kb.op('dve', lambda e, c=c: e.scalar_tensor_tensor(out=A1[:, :, c], in0=modT[:, 16:32, c], scalar=1.0, in1=g1s[:], op0=ALU.add, op1=ALU.mult), [modT, g1s], [A1])
        kb.op('dve', lambda e, c=c: e.tensor_copy(out=B1[:, :, c], in_=modT[:, 0:16, c]), [modT], [B1])
    kb.barrier()
    ph.close()

    ph = ExitStack()
    hT = kb.sb("hT", [128, 16, NTS], BF16, ph)
    xt = [kb.sb("xt%d" % i, [128, D], F32, ph) for i in range(2)]
    xn = [kb.sb("xn%d" % i, [128, D], F32, ph) for i in range(2)]
    junk = kb.sb("junk", [128, D], BF16, ph)
    ssq = [kb.sb("ssq%d" % i, [128, 1], F32, ph) for i in range(2)]
    rstd = [kb.sb("rstd%d" % i, [128, 1], F32, ph) for i in range(2)]
    binT = kb.sb("binT", [128, 128], F32, ph)
    kb.dma('sp', binT[:], b_inT[:], [b_inT], [binT], prim=binT)
    stg = [kb.sb("stg%d" % i, [128, 16, 256], F32, ph) for i in range(2)]
    wbs = [[kb.sb("wb%d_%d" % (i, j), [128, KSPL[j][1] - KSPL[j][0], 256], BF16, ph) for j in range(3)] for i in range(2)]
    ev_bf = [kb.sb("evbf%d" % i, [128, 512], BF16, ph) for i in range(4)]
    ev_f = [kb.sb("evf%d" % i, [128, 512], F32, ph) for i in range(4)]
    sgt = [kb.sb("sgt%d" % i, [128, 512], F32, ph) for i in range(2)]
    vb = [kb.sb("vb%d" % i, [128, 256], F32, ph) for i in range(2)]
    ko = [kb.sb("ko%d" % i, [128, 128], F32, ph) for i in range(4)]
    cnt = {'ev': 0, 'ps': 0, 'ko': 0, 'x': 0, 'vb': 0, 'sg': 0}

    def rr(key, n):
        cnt[key] += 1
        return (cnt[key] - 1) % n

    def make_hT(src_ap, nrows, col0, grp):
        i = rr('x', 2)
        x, xnn, ss, rs = xt[i], xn[i], ssq[i], rstd[i]
        kb.dma('sp', x[0:nrows, :], src_ap, [], [x], prim=x)
        kb.op('act', lambda e: e.activation(out=junk[0:nrows, :], in_=x[0:nrows, :], func=AF.Square, accum_out=ss[0:nrows, :]), [x], [junk, ss])
        kb.op('dve', lambda e: e.tensor_scalar(out=ss[0:nrows, :], in0=ss[0:nrows, :], scalar1=1.0 / D, scalar2=1e-6, op0=ALU.mult, op1=ALU.add), [ss], [ss])
        kb.op('act', lambda e: e.activation(out=ss[0:nrows, :], in_=ss[0:nrows, :], func=AF.Sqrt), [ss], [ss])
        kb.op('dve', lambda e: e.reciprocal(out=rs[0:nrows, :], in_=ss[0:nrows, :]), [ss], [rs])
        kb.op('act', lambda e: e.activation(out=xnn[0:nrows, :], in_=x[0:nrows, :], func=AF.Copy, scale=rs[0:nrows, :]), [x, rs], [xnn])
        for kq in range(4):
            p = PS[4 + rr('ps', 4)]
            for kk in range(4):
                k = kq * 4 + kk
                kb.op('pe', lambda e, k=k, kk=kk, p=p: e.transpose(p[:, kk * 128:kk * 128 + nrows], xnn[0:nrows, k * 128:(k + 1) * 128], ident[0:nrows, 0:nrows]), [xnn, ident], [p])
            for kk in range(4):
                k = kq * 4 + kk
                kb.op('dve' if kk % 2 == 0 else 'pool' if False else 'dve', lambda e, k=k, kk=kk, p=p: e.tensor_scalar(out=hT[:, k, col0:col0 + nrows], in0=p[:, kk * 128:kk * 128 + nrows], scalar1=A1[:, k, grp:grp + 1], scalar2=B1[:, k, grp:grp + 1], op0=ALU.mult, op1=ALU.add), [p, A1, B1], [hT])

    QO, KO, VO, UAO, UBO, GAO, GBO = 0, 3072, 6144, 9216, 10752, 12288, 14336
    wq = {'i': 0}

    def stream(blocks, body):
        def issue(bi):
            st = stg[(wq['i'] + bi) % 2]
            for (c0, n, d0) in blocks[bi]:
                kb.dma('sp', st[:, :, d0:d0 + n], w_in[:, c0:c0 + n].rearrange("(k p) n -> p k n", p=128), [], [st], prim=st)
        issue(0)
        for bi in range(len(blocks)):
            if bi + 1 < len(blocks):
                issue(bi + 1)
            st, wb3 = stg[(wq['i'] + bi) % 2], wbs[(wq['i'] + bi) % 2]
            cast_block(st, wb3)
            body(bi, wb3)
        wq['i'] += len(blocks)

    def fm_mm(wb3, sub, tok0, ntok):
        p = PS[rr('ps', 4)]
        for k in range(16):
            wp = wpart(wb3, k)
            kb.op('pe', lambda e, k=k, wp=wp: e.matmul(p[:, 0:ntok], wp[:, k, sub * 128:(sub + 1) * 128], hT[:, k, tok0:tok0 + ntok], start=(k == 0), stop=(k == 15)), [wp.t, hT], [p])
        return p

    def k_block(g, h, wb3, sub, tokblocks, ctx0, out_rows):
        blk = (KO + (g * 8 + h) * 128) // 128
        for (c0, ntok, cc) in tokblocks:
            p = fm_mm(wb3, sub, c0, ntok)
            i = rr('ev', 4)
            eb, ef = ev_bf[i], ev_f[i]
            need = [(t0, r0) for (t0, r0) in out_rows if c0 <= t0 < c0 + ntok]
            if need:
                kb.op('dve', lambda e, p=p, ef=ef: e.tensor_scalar(out=ef[:, 0:ntok], in0=p[:, 0:ntok], scalar1=binT[:, blk:blk + 1], scalar2=None, op0=ALU.add), [p, binT], [ef])
                kb.op('act', lambda e, ef=ef, eb=eb: e.activation(out=eb[:, 0:ntok], in_=ef[:, 0:ntok], func=AF.Copy), [ef], [eb])
            else:
                kb.op('act', lambda e, p=p, eb=eb: e.activation(out=eb[:, 0:ntok], in_=p[:, 0:ntok], func=AF.Identity, bias=binT[:, blk:blk + 1]), [p, binT], [eb])
            kb.dma('pool', kT_d[g][h, :, cc:cc + ntok], eb[:, 0:ntok], [eb], [kT_d[g]], prim=eb)
            if need:
                for (t0, r0) in need:
                    pt = PS[4 + rr('ps', 4)]
                    kb.op('pe', lambda e, pt=pt, ef=ef, t0=t0: e.transpose(pt[:, 0:128], ef[:, t0 - c0:t0 - c0 + 128], ident[:]), [ef, ident], [pt])
                    kk = ko[rr('ko', 4)]
                    kb.op('act', lambda e, pt=pt, kk=kk: e.activation(out=kk[:], in_=pt[:, 0:128], func=AF.Copy), [pt], [kk])
                    if 'd' not in os.environ.get('KSKIP', ''):
                        kb.dma(os.environ.get('KSQ', 'pool'), kv_o[g][r0:r0 + 128, 0, h, :], kk[:], [kk], [kv_o[g]], prim=kk)

    def v_block(g, h0, wb3, toktiles, out_rows):
        c0 = VO + (g * 8 + h0) * 128
        b = vb[rr('vb', 2)]
        kb.dma('pool', b[:], b_in_row[:, c0:c0 + 256].partition_broadcast(128), [b_in_row], [b], prim=b)
        for (t0, cr) in toktiles:
            p = PS[rr('ps', 4)]
            for k in range(16):
                wp = wpart(wb3, k)
                kb.op('pe', lambda e, k=k, wp=wp, p=p: e.matmul(p[:, 0:256], hT[:, k, t0:t0 + 128], wp[:, k, :], start=(k == 0), stop=(k == 15)), [wp.t, hT], [p])
            i = rr('ev', 4)
            eb, ef = ev_bf[i], ev_f[i]
            kb.op('dve', lambda e, p=p, ef=ef: e.tensor_tensor(out=ef[:, 0:256], in0=p[:, 0:256], in1=b[:], op=ALU.add), [p, b], [ef])
            kb.op('act', lambda e, eb=eb, ef=ef: e.activation(out=eb[:, 0:256], in_=ef[:, 0:256], func=AF.Copy), [ef], [eb])
            kb.dma('pool', V_d[g][cr:cr + 128, h0:h0 + 2, :], eb[:, 0:256].rearrange("p (h d) -> p h d", h=2), [eb], [V_d[g]], prim=eb)
            for (tt0, r0) in out_rows:
                if tt0 == t0:
                    kb.dma('pool', kv_o[g][r0:r0 + 128, 1, h0:h0 + 2, :], ef[:, 0:256].rearrange("p (h d) -> p h d", h=2), [ef], [kv_o[g]], prim=ef)

    def u_block(j, wb3, tokblocks):
        ba, bb = (UAO // 128) + j, (UBO // 128) + j
        for (c0, ntok, dst, dc) in tokblocks:
            pa = fm_mm(wb3, 0, c0, ntok)
            pb = fm_mm(wb3, 1, c0, ntok)
            s = sgt[rr('sg', 2)]
            i = rr('ev', 4)
            ef = ev_f[i]
            kb.op('act', lambda e, pb=pb, s=s: e.activation(out=s[:, 0:ntok], in_=pb[:, 0:ntok], func=AF.Sigmoid, bias=binT[:, bb:bb + 1]), [pb, binT], [s])
            kb.op('dve', lambda e, pa=pa, s=s, ef=ef: e.scalar_tensor_tensor(out=ef[:, 0:ntok], in0=pa[:, 0:ntok], scalar=binT[:, ba:ba + 1], in1=s[:, 0:ntok], op0=ALU.add, op1=ALU.mult), [pa, s, binT], [ef])
            kb.dma('pool', dst[j * 128:(j + 1) * 128, dc:dc + ntok], ef[:, 0:ntok], [ef], [dst], prim=ef)

    for grp_i in range(2 if DBG in ('', 'A', 'B') else 0):
        if DBG == 'A' and grp_i == 1:
            break
        base = grp_i * 1024
        for t in range(8):
            make_hT(xh[base + t * 128: base + (t + 1) * 128, :], 128, t * 128, 0)
        blocks, kinds = [], []
        for hp in range(4):
            blocks.append([(KO + (2 * 8 + 2 * hp) * 128, 256, 0)]); kinds.append(('k', 2, 2 * hp))
        for hp in range(4):
            blocks.append([(VO + (2 * 8 + 2 * hp) * 128, 256, 0)]); kinds.append(('v', 2, 2 * hp))
        if grp_i == 1:
            for g in (1, 0):
                for hp in range(4):
                    blocks.append([(KO + (g * 8 + 2 * hp) * 128, 256, 0)]); kinds.append(('k', g, 2 * hp))
                for hp in range(4):
                    blocks.append([(VO + (g * 8 + 2 * hp) * 128, 256, 0)]); kinds.append(('v', g, 2 * hp))
            for j in range(12):
                blocks.append([(UAO + j * 128, 128, 0), (UBO + j * 128, 128, 128)]); kinds.append(('u', j, 0))

        def body(bi, wb3, kinds=kinds, base=base):
            kind, g, h0 = kinds[bi]
            if kind == 'k':
                for sub in range(2):
                    if g == 2:
                        tb = [(0, 512, base), (512, 512, base + 512)]
                    elif g == 1:
                        tb = [(512, 512, 0)]
                    else:
                        tb = [(896, 128, 0)]
                    k_block(g, h0 + sub, wb3, sub, tb, 0, [])
            elif kind == 'v':
                if g == 2:
                    tt = [(t * 128, base + t * 128) for t in range(8)]
                elif g == 1:
                    tt = [(512 + t * 128, t * 128) for t in range(4)]
                else:
                    tt = [(896, 0)]
                v_block(g, h0, wb3, tt, [])
            else:
                u_block(g, wb3, [(896, 128, u_d, 0)])
        stream(blocks, body)

    for t in range(8 if DBG in ('', 'C') else 0):
        make_hT(xh[HALO + t * 128: HALO + (t + 1) * 128, :], 128, t * 128, 0)
    if DBG in ('', 'C'):
        make_hT(xs[:, :], NS, NT, 1)
    blocks, kinds = [], []
    for g in range(3):
        for hp in range(4):
            blocks.append([(QO + (g * 8 + 2 * hp) * 128, 256, 0)]); kinds.append(('q', g, 2 * hp))
        for hp in range(4):
            blocks.append([(KO + (g * 8 + 2 * hp) * 128, 256, 0)]); kinds.append(('k', g, 2 * hp))
        for hp in range(4):
            blocks.append([(VO + (g * 8 + 2 * hp) * 128, 256, 0)]); kinds.append(('v', g, 2 * hp))
    for j in range(12):
        blocks.append([(UAO + j * 128, 128, 0), (UBO + j * 128, 128, 128)]); kinds.append(('u', j, 0))
    for j in range(16):
        blocks.append([(GAO + j * 128, 256, 0)] if False else [(GAO + 2 * j * 128, 256, 0)]); kinds.append(('g', 2 * j, 0))
    vs_sb = kb.sb("vs_sb", [NS, 256], F32, ph); vs_bf = kb.sb("vs_bf", [NS, 256], BF16, ph)
    sm_bf = kb.sb("sm_bf", [128, NS], BF16, ph); sm_f = kb.sb("sm_f", [128, NS], F32, ph); sm_t = kb.sb("sm_t", [NS, 128], F32, ph)

    def bodyC(bi, wb3):
        kind, g, h0 = kinds[bi]
        if kind == 'q':
            for sub in range(2):
                h = h0 + sub
                blk = (QO + (g * 8 + h) * 128) // 128
                for half in range(2):
                    p = fm_mm(wb3, sub, half * 512, 512)
                    eb = ev_bf[rr('ev', 4)]
                    kb.op('act', lambda e, p=p, eb=eb, blk=blk: e.activation(out=eb[:, :], in_=p[:, :], func=AF.Identity, bias=binT[:, blk:blk + 1]), [p, binT], [eb])
                    kb.dma('pool', qT_d[g, h, :, half * 512:(half + 1) * 512], eb[:, :], [eb], [qT_d], prim=eb)
                p = fm_mm(wb3, sub, NT, NS)
                kb.op('act', lambda e, p=p, blk=blk: e.activation(out=sm_bf[:, :], in_=p[:, 0:NS], func=AF.Identity, bias=binT[:, blk:blk + 1]), [p, binT], [sm_bf])
                kb.dma('pool', qsT_d[g, h, :, :], sm_bf[:, :], [sm_bf], [qsT_d], prim=sm_bf)
        elif kind == 'k':
            W = GH[g]
            nout = min(W, NT)
            outs = [(NT - nout + t * 128, t * 128) for t in range(nout // 128)]
            for sub in range(2):
                h = h0 + sub
                KS = os.environ.get('KSUB', 'os')
                k_block(g, h, wb3, sub, [(0, 512, W), (512, 512, W + 512)], 0, outs if 'o' in KS else [])
                if 's' not in KS:
                    continue
                blk = (KO + (g * 8 + h) * 128) // 128
                p = fm_mm(wb3, sub, NT, NS)
                kb.op('dve', lambda e, p=p, blk=blk: e.tensor_scalar(out=sm_f[:, :], in0=p[:, 0:NS], scalar1=binT[:, blk:blk + 1], scalar2=None, op0=ALU.add), [p, binT], [sm_f])
                kb.op('act', lambda e: e.activation(out=sm_bf[:, :], in_=sm_f[:, :], func=AF.Copy), [sm_f], [sm_bf])
                kb.dma('pool', ksT_d[g, h, :, :], sm_bf[:, :], [sm_bf], [ksT_d], prim=sm_bf)
                pt = PS[4 + rr('ps', 4)]
                kb.op('pe', lambda e, pt=pt: e.transpose(pt[0:NS, 0:128], sm_f[:, :], ident[:]), [sm_f, ident], [pt])
                kb.op('act', lambda e, pt=pt: e.activation(out=sm_t[:, :], in_=pt[0:NS, 0:128], func=AF.Copy), [pt], [sm_t])
                kb.dma('pool', kvs_o[g, :, 0, h, :], sm_t[:, :], [sm_t], [kvs_o], prim=sm_t)
        elif kind == 'v':
            W = GH[g]
            nout = min(W, NT)
            outs = [(NT - nout + t * 128, t * 128) for t in range(nout // 128)]
            v_block(g, h0, wb3, [(t * 128, W + t * 128) for t in range(8)], outs)
            c0 = VO + (g * 8 + h0) * 128
            b = vb[rr('vb', 2)]
            kb.dma('pool', b[:], b_in_row[:, c0:c0 + 256].partition_broadcast(128), [b_in_row], [b], prim=b)
            p = PS[rr('ps', 4)]
            for k in range(16):
                wp = wpart(wb3, k)
                kb.op('pe', lambda e, k=k, wp=wp, p=p: e.matmul(p[0:NS, 0:256], hT[:, k, NT:NTS], wp[:, k, :], start=(k == 0), stop=(k == 15)), [wp.t, hT], [p])
            kb.op('dve', lambda e, p=p, b=b: e.tensor_tensor(out=vs_sb[:, :], in0=p[0:NS, 0:256], in1=b[0:NS, :], op=ALU.add), [p, b], [vs_sb])
            kb.op('act', lambda e: e.activation(out=vs_bf[:, :], in_=vs_sb[:, :], func=AF.Copy), [vs_sb], [vs_bf])
            kb.dma('pool', Vs_d[g, :, h0:h0 + 2, :], vs_bf[:, :].rearrange("p (h d) -> p h d", h=2), [vs_bf], [Vs_d], prim=vs_bf)
            kb.dma('pool', kvs_o[g, :, 1, h0:h0 + 2, :], vs_sb[:, :].rearrange("p (h d) -> p h d", h=2), [vs_sb], [kvs_o], prim=vs_sb)
        elif kind == 'u':
            u_block(g, wb3, [(0, 512, u_d, 128), (512, 512, u_d, 128 + 512), (NT, NS, us_d, 0)])
        else:
            for sub in range(2):
                jb = g + sub
                blk = GAO // 128 + jb
                for (c0, ntok) in ((0, 512), (512, 512), (NT, NS)):
                    p = fm_mm(wb3, sub, c0, ntok)
                    ef = ev_f[rr('ev', 4)]
                    kb.op('act', lambda e, p=p, ef=ef, blk=blk, ntok=ntok: e.activation(out=ef[:, 0:ntok], in_=p[:, 0:ntok], func=AF.Sigmoid, bias=binT[:, blk:blk + 1]), [p, binT], [ef])
                    kb.dma('pool', sg_d[jb * 128:(jb + 1) * 128, c0:c0 + ntok], ef[:, 0:ntok], [ef], [sg_d], prim=ef)
    KK = os.environ.get('KKINDS', 'qkvug')
    sel = [i for i in range(len(blocks)) if kinds[i][0] in KK]
    blocks = [blocks[i] for i in sel]; kinds = [kinds[i] for i in sel]
    if DBG in ('', 'C'):
        stream(blocks, bodyC)
    kb.barrier()
    ph.close()


    mid = ExitStack()
    attn_oT = kb.sb("attn_oT", [128, 8, NTS], BF16, mid)
    SCALE = 128.0 ** -0.5
    if DBG in ('', 'C', 'ATT'):
        ph = ExitStack()
        M1 = kb.sb("M1", [128, 256], F32, ph); kb.dma('sp', M1[:], M1_d[:], [], [M1], prim=M1)
        kval = kb.sb("kval", [128, 53], F32, ph); kb.dma('sp', kval[:], kval_d[:], [], [kval], prim=kval)
        SMc = kb.sb("SMc", [128, 21, NS], F32, ph); kb.dma('sp', SMc[:], SMc_d[:], [], [SMc], prim=SMc)
        SMn = kb.sb("SMn", [NS, 3, NS], F32, ph); kb.dma('sp', SMn[:], SMn_d[:], [], [SMn], prim=SMn)
        onesb = kb.sb("onesb", [128, 128], BF16, ph)
        kb.op('dve', lambda e: e.memset(onesb[:], 1.0), [], [onesb])
        qt = [kb.sb("qt%d" % i, [128, 3, NT], BF16, ph) for i in range(2)]
        kt = [[kb.sb("kt%d_%d" % (i, g), [128, NCTX[g]], BF16, ph) for g in range(3)] for i in range(2)]
        vt1 = [kb.sb("vt1_%d" % i, [128, 9, 128], BF16, ph) for i in range(2)]
        vt2 = [kb.sb("vt2_%d" % i, [128, 4, 3, 128], BF16, ph) for i in range(2)]
        vt3 = [kb.sb("vt3_%d" % i, [128, 16, 2, 128], BF16, ph) for i in range(2)]
        Et = [kb.sb("Et%d" % i, [128, 256], F32, ph) for i in range(3)]
        Pt = [kb.sb("Pt%d" % i, [128, 256], BF16, ph) for i in range(3)]
        acc = kb.sb("acc", [128, NT], F32, ph); dacc = kb.sb("dacc", [128, NT], F32, ph)
        qs = [kb.sb("qs%d" % i, [128, 3, NS], BF16, ph) for i in range(2)]
        ksn = [kb.sb("ksn%d" % i, [128, 3, NS], BF16, ph) for i in range(2)]
        vsn = [kb.sb("vsn%d" % i, [NS, 3, 128], BF16, ph) for i in range(2)]
        ck = [kb.sb("ck%d" % i, [128, 128], F32, ph) for i in range(3)]
        cv = [kb.sb("cv%d" % i, [128, 128], F32, ph) for i in range(3)]
        ckT = [kb.sb("ckT%d" % i, [128, 128], BF16, ph) for i in range(3)]
        cvb = [kb.sb("cvb%d" % i, [128, 128], BF16, ph) for i in range(3)]
        Es = [kb.sb("Es%d" % i, [128, NS], F32, ph) for i in range(3)]
        Psm = [kb.sb("Psm%d" % i, [128, NS], BF16, ph) for i in range(3)]
        osn = kb.sb("osn", [128, NS], F32, ph); dsn = kb.sb("dsn", [128, NS], F32, ph)
        ac = {'s': 0, 'c': 0}

        def load_head(h, i):
            for g in range(3):
                kb.dma('sp', qt[i][:, g, :], qT_d[g, h, :, :], [qT_d], [qt[i]], prim=qt[i])
                kb.dma('sp', kt[i][g][:, :], kT_d[g][h, :, :], [kT_d[g]], [kt[i][g]], prim=kt[i][g])
            kb.dma('sp', vt1[i][:], V_d[0][:, h, :].rearrange("(j p) d -> p j d", p=128), [V_d[0]], [vt1[i]], prim=vt1[i])
            for r in range(4):
                kb.dma('sp', vt2[i][:, r, :, :], V_d[1][:, h, :].rearrange("(j p r) d -> p r j d", p=128, r=4)[:, r, :, :], [V_d[1]], [vt2[i]], prim=vt2[i])
            v3 = V_d[2][:, h, :].rearrange("(m r) d -> m r d", r=16)
            kb.dma('sp', vt3[i][:, :, 0, :], v3[0:128, :, :], [V_d[2]], [vt3[i]], prim=vt3[i])
            kb.dma('sp', vt3[i][0:64, :, 1, :], v3[128:192, :, :], [V_d[2]], [vt3[i]], prim=vt3[i])
            kb.dma('sp', qs[i][:], qsT_d[:, h, :, :].rearrange("g d q -> d g q"), [qsT_d], [qs[i]], prim=qs[i])
            kb.dma('sp', ksn[i][:], ksT_d[:, h, :, :].rearrange("g d q -> d g q"), [ksT_d], [ksn[i]], prim=ksn[i])
            kb.dma('sp', vsn[i][:], Vs_d[:, :, h, :].rearrange("g t d -> t g d"), [Vs_d], [vsn[i]], prim=vsn[i])

        def tile_attn(kT_ap, nk, q_ap, N, v_ap, kv_idx, m0, qstart, first):
            sp_ = PS[4 + ac['s'] % 3]; Ei = Et[ac['s'] % 3]; Pi = Pt[ac['s'] % 3]; ac['s'] += 1
            kb.op('pe', lambda e: e.matmul(sp_[0:nk, 0:N], kT_ap, q_ap, start=True, stop=True), kt_reads, [sp_])
            kb.op('act', lambda e: e.activation(out=Ei[0:nk, 0:N], in_=sp_[0:nk, 0:N], func=AF.Exp, scale=SCALE), [sp_], [Ei])
            kb.op('dve', lambda e: e.scalar_tensor_tensor(out=Pi[0:nk, 0:N], in0=Ei[0:nk, 0:N], scalar=kval[0:nk, kv_idx:kv_idx + 1], in1=M1[0:nk, m0:m0 + N], op0=ALU.mult, op1=ALU.mult), [Ei, kval, M1], [Pi])
            segs = []
            a, b = qstart, qstart + N
            if a < 512 and b > 512:
                segs = [(a, 512, 0), (512, b, 512 - a)]
            else:
                segs = [(a, b, 0)]
            for (qa, qb, po) in segs:
                bank = qa // 512
                st = first[bank]
                first[bank] = False
                n = qb - qa
                kb.op('pe', lambda e: e.matmul(PS[bank][:, qa - bank * 512:qb - bank * 512], v_ap, Pi[0:nk, po:po + n], start=st, stop=True, skip_group_check=True), [Pi] + v_reads, [PS[bank]])
                kb.op('pe', lambda e: e.matmul(PS[2 + bank][:, qa - bank * 512:qb - bank * 512], onesb[0:nk, :], Pi[0:nk, po:po + n], start=st, stop=True, skip_group_check=True), [Pi, onesb], [PS[2 + bank]])

        load_head(0, 0)
        for h in range(8):
            i = h % 2
            if h + 1 < 8:
                load_head(h + 1, (h + 1) % 2)
            kt_reads = [kt[i][0], kt[i][1], kt[i][2], qt[i]]
            v_reads = [vt1[i], vt2[i], vt3[i]]
            first = [True, True]
            for j in range(9):
                t0 = max(0, 128 * (j - 1)); t1 = min(NT, 128 * (j - 1) + 256)
                m0 = 128 if j == 0 else 0
                tile_attn(kt[i][0][:, 128 * j:128 * j + 128], 128, qt[i][:, 0, t0:t1], t1 - t0, vt1[i][:, j, :], j, m0, t0, first)
            for b in range(2):
                kb.op('act', lambda e, b=b: e.activation(out=acc[:, 512 * b:512 * b + 512], in_=PS[b][:, :], func=AF.Copy), [PS[b]], [acc])
                kb.op('dve', lambda e, b=b: e.tensor_copy(out=dacc[:, 512 * b:512 * b + 512], in_=PS[2 + b][:, :]), [PS[2 + b]], [dacc])
            first = [True, True]
            k2 = kt[i][1][:, :].rearrange("d (j p r) -> d j r p", p=128, r=4)
            q2 = qt[i][:, 1, :].rearrange("d (i r) -> d r i", r=4)
            for r in range(4):
                for j in range(3):
                    i0 = 0 if j < 2 else 128
                    N = 128 if j != 1 else 256
                    m0 = 128 if j == 0 else 0
                    tile_attn(k2[:, j, r, :], 128, q2[:, r, i0:i0 + N], N, vt2[i][:, r, j, :], 9 + r * 3 + j, m0, r * 256 + i0, first)
            for (A, Pb) in ((acc, 0), (dacc, 2)):
                Av = A[:, :].rearrange("p (i r) -> p r i", r=4)
                for b in range(2):
                    kb.op('dve', lambda e, b=b, Av=Av, Pb=Pb: e.tensor_tensor(out=Av[:, 2 * b:2 * b + 2, :], in0=Av[:, 2 * b:2 * b + 2, :], in1=PS[Pb + b][:, :].rearrange("p (r i) -> p r i", r=2), op=ALU.add), [A, PS[Pb + b]], [A])
            first = [True, True]
            k3 = kt[i][2][:, :].rearrange("d (m r) -> d r m", r=16)
            q3 = qt[i][:, 2, :].rearrange("d (i r) -> d r i", r=16)
            for r in range(16):
                tile_attn(k3[:, r, 0:128], 128, q3[:, r, :], 64, vt3[i][:, r, 0, :], 21 + 2 * r, 128, r * 64, first)
                tile_attn(k3[:, r, 128:192], 64, q3[:, r, :], 64, vt3[i][0:64, r, 1, :], 21 + 2 * r + 1, 0, r * 64, first)
            for (A, Pb) in ((acc, 0), (dacc, 2)):
                Av = A[:, :].rearrange("p (i r) -> p r i", r=16)
                for b in range(2):
                    kb.op('dve', lambda e, b=b, Av=Av, Pb=Pb: e.tensor_tensor(out=Av[:, 8 * b:8 * b + 8, :], in0=Av[:, 8 * b:8 * b + 8, :], in1=PS[Pb + b][:, :].rearrange("p (r i) -> p r i", r=8), op=ALU.add), [A, PS[Pb + b]], [A])
            kb.op('dve', lambda e: e.reciprocal(out=dacc[:, :], in_=dacc[:, :]), [dacc], [dacc])
            kb.op('dve', lambda e: e.tensor_tensor(out=attn_oT[:, h, 0:NT], in0=acc[:, :], in1=dacc[:, :], op=ALU.mult), [acc, dacc], [attn_oT])
            po, pd = PS[7], PS[7]
            firsts = [True]
            tiles = []
            for g, W in enumerate(GH):
                for j in range(W // 128):
                    tiles.append((g, j))
            ti = 0
            for (g, j) in tiles:
                c = ac['c'] % 3; ac['c'] += 1
                kb.dma('sp', ck[c][:], cache_d[g][128 * j:128 * j + 128, 0, h, :], [], [ck[c]], prim=ck[c])
                kb.dma('sp', cv[c][:], cache_d[g][128 * j:128 * j + 128, 1, h, :], [], [cv[c]], prim=cv[c])
                pt = PS[4 + ac['s'] % 3]; ac['s'] += 1
                kb.op('pe', lambda e, c=c, pt=pt: e.transpose(pt[:, 0:128], ck[c][:], ident[:]), [ck[c], ident], [pt])
                kb.op('act', lambda e, c=c, pt=pt: e.activation(out=ckT[c][:], in_=pt[:, 0:128], func=AF.Copy), [pt], [ckT[c]])
                kb.op('dve', lambda e, c=c: e.tensor_copy(out=cvb[c][:], in_=cv[c][:]), [cv[c]], [cvb[c]])
                sp_ = PS[4 + ac['s'] % 3]; Ei = Es[ac['s'] % 3]; Pi = Psm[ac['s'] % 3]; ac['s'] += 1
                kb.op('pe', lambda e, c=c, sp_=sp_, g=g: e.matmul(sp_[:, 0:NS], ckT[c][:], qs[i][:, g, :], start=True, stop=True), [ckT[c], qs[i]], [sp_])
                kb.op('act', lambda e, sp_=sp_, Ei=Ei: e.activation(out=Ei[:, :], in_=sp_[:, 0:NS], func=AF.Exp, scale=SCALE), [sp_], [Ei])
                kb.op('dve', lambda e, Ei=Ei, Pi=Pi, ti=ti: e.tensor_tensor(out=Pi[:, :], in0=Ei[:, :], in1=SMc[:, ti, :], op=ALU.mult), [Ei, SMc], [Pi])
                st = firsts[0]; firsts[0] = False
                kb.op('pe', lambda e, c=c, Pi=Pi, st=st: e.matmul(PS[7][:, 0:NS], cvb[c][:], Pi[:, :], start=st, stop=True, skip_group_check=True), [cvb[c], Pi], [PS[7]])
                kb.op('pe', lambda e, Pi=Pi, st=st: e.matmul(PS[7][:, 64:64 + NS], onesb[:, :], Pi[:, :], start=False, stop=True, skip_group_check=True), [onesb, Pi], [PS[7]])
                ti += 1
            for g in range(3):
                sp_ = PS[4 + ac['s'] % 3]; Ei = Es[ac['s'] % 3]; Pi = Psm[ac['s'] % 3]; ac['s'] += 1
                kb.op('pe', lambda e, sp_=sp_, g=g: e.matmul(sp_[0:NS, 0:NS], ksn[i][:, g, :], qs[i][:, g, :], start=True, stop=True), [ksn[i], qs[i]], [sp_])
                kb.op('act', lambda e, sp_=sp_, Ei=Ei: e.activation(out=Ei[0:NS, :], in_=sp_[0:NS, 0:NS], func=AF.Exp, scale=SCALE), [sp_], [Ei])
                kb.op('dve', lambda e, Ei=Ei, Pi=Pi, g=g: e.tensor_tensor(out=Pi[0:NS, :], in0=Ei[0:NS, :], in1=SMn[:, g, :], op=ALU.mult), [Ei, SMn], [Pi])
                kb.op('pe', lambda e, Pi=Pi, g=g: e.matmul(PS[7][:, 0:NS], vsn[i][:, g, :], Pi[0:NS, :], start=False, stop=True, skip_group_check=True), [vsn[i], Pi], [PS[7]])
                kb.op('pe', lambda e, Pi=Pi: e.matmul(PS[7][:, 64:64 + NS], onesb[0:NS, :], Pi[0:NS, :], start=False, stop=True, skip_group_check=True), [onesb, Pi], [PS[7]])
            kb.op('dve', lambda e: e.reciprocal(out=dsn[:, :], in_=PS[7][:, 64:64 + NS]), [PS[7]], [dsn])
            kb.op('dve', lambda e: e.tensor_tensor(out=attn_oT[:, h, NT:NTS], in0=PS[7][:, 0:NS], in1=dsn[:, :], op=ALU.mult), [PS[7], dsn], [attn_oT])
        if os.environ.get('KDUMPA'):
            adbg = kb.dram("attn_dbg", [128, 8, NTS], BF16, "ExternalOutput")
            kb.dma('pool', adbg[:], attn_oT[:], [attn_oT], [adbg], prim=attn_oT)
        kb.barrier()
        ph.close()


    conv_fT = kb.sb("conv_fT", [128, 12, NTS], BF16, mid)
    x1_d = kb.dram("x1_d", [NT + 128, D])
    if DBG in ('', 'CONV'):
        ph = ExitStack()
        wdw = kb.sb("wdw", [128, 12, 31], F32, ph); kb.dma('sp', wdw[:], w_dwT[:], [], [wdw], prim=wdw)
        cpar = kb.sb("cpar", [128, 3, 12], F32, ph); kb.dma('sp', cpar[:], cparT[:], [], [cpar], prim=cpar)
        hprev = kb.sb("hprev", [128, 1], F32, ph); kb.dma('sp', hprev[:], hprev_d[:], [], [hprev], prim=hprev)
        onesf = kb.sb("onesf", [128, 128], F32, ph); kb.op('dve', lambda e: e.memset(onesf[:], 1.0), [], [onesf])
        yc = kb.sb("yc", [128, 12, NTS], F32, ph)
        ub = [kb.sb("ub%d" % i, [128, 30 + NT], F32, ph) for i in range(2)]
        ubs = [kb.sb("ubs%d" % i, [128, 30 + NS], F32, ph) for i in range(2)]
        cpo = kb.sb("cpo", [32, 1536], F32, ph); cso = kb.sb("cso", [NS, 1536], F32, ph)
        sq = [kb.sb("sq%d" % i, [128, 512], F32, ph) for i in range(2)]
        mu = kb.sb("mu", [128, NTS], F32, ph); rsd = kb.sb("rsd", [128, NTS], F32, ph); tmpc = [kb.sb("tmpc%d" % i, [128, 512], F32, ph) for i in range(2)]
        kb.dma('pool', convs_o[0:22, :], st_d[8:30, :], [st_d], [convs_o], prim=hprev)
        for j in range(12):
            u, us_ = ub[j % 2], ubs[j % 2]
            kb.dma('sp', u[:, :], u_d[j * 128:(j + 1) * 128, 98:128 + NT], [u_d], [u], prim=u)
            kb.dma('sp', us_[:, 0:30], stT_d[j * 128:(j + 1) * 128, :], [stT_d], [us_], prim=us_)
            kb.dma('sp', us_[:, 30:30 + NS], us_d[j * 128:(j + 1) * 128, :], [us_d], [us_], prim=us_)
            kb.op('dve', lambda e, u=u: e.tensor_scalar(out=u[:, 0:30], in0=u[:, 0:30], scalar1=hprev[:, 0:1], scalar2=None, op0=ALU.mult), [u, hprev], [u])
            for (src, L, c0) in ((u, NT, 0), (us_, NS, NT)):
                kb.op('dve', lambda e, src=src, L=L, c0=c0: e.tensor_scalar(out=yc[:, j, c0:c0 + L], in0=src[:, 0:L], scalar1=wdw[:, j, 0:1], scalar2=cpar[:, 0, j:j + 1], op0=ALU.mult, op1=ALU.add), [src, wdw, cpar], [yc])
                for k in range(1, 31):
                    kb.op('dve', lambda e, src=src, L=L, c0=c0, k=k: e.scalar_tensor_tensor(out=yc[:, j, c0:c0 + L], in0=src[:, k:k + L], scalar=wdw[:, j, k:k + 1], in1=yc[:, j, c0:c0 + L], op0=ALU.mult, op1=ALU.add), [src, wdw], [yc])
            pt = PS[4 + j % 2]
            kb.op('pe', lambda e, pt=pt, u=u: e.transpose(pt[0:32, 0:128], u[:, 30 + NT - 32:30 + NT], ident[:]), [u, ident], [pt])
            kb.op('act', lambda e, pt=pt: e.activation(out=cpo[:, j * 128:(j + 1) * 128], in_=pt[0:32, 0:128], func=AF.Copy), [pt], [cpo])
            pt2 = PS[6 + j % 2]
            kb.op('pe', lambda e, pt2=pt2, us_=us_: e.transpose(pt2[0:NS, 0:128], us_[:, 30:30 + NS], ident[:]), [us_, ident], [pt2])
            kb.op('act', lambda e, pt2=pt2: e.activation(out=cso[:, j * 128:(j + 1) * 128], in_=pt2[0:NS, 0:128], func=AF.Copy), [pt2], [cso])
        kb.dma('pool', convp_o[:, :], cpo[2:32, :], [cpo], [convp_o], prim=cpo)
        kb.dma('pool', convs_o[22:30, :], cso[:, :], [cso], [convs_o], prim=cso)
        for (c0, n) in ((0, 512), (512, 512), (NT, NS)):
            p1, p2 = PS[0], PS[1]
            for j in range(12):
                s_ = sq[j % 2]
                kb.op('act', lambda e, s_=s_: e.activation(out=s_[:, 0:n], in_=yc[:, j, c0:c0 + n], func=AF.Square), [yc], [s_])
                kb.op('pe', lambda e: e.matmul(p1[:, 0:n], onesf[:, :], yc[:, j, c0:c0 + n], start=(j == 0), stop=(j == 11)), [onesf, yc], [p1])
                kb.op('pe', lambda e, s_=s_: e.matmul(p2[:, 0:n], onesf[:, :], s_[:, 0:n], start=(j == 0), stop=(j == 11)), [onesf, s_], [p2])
            t_ = tmpc[0]
            kb.op('dve', lambda e: e.tensor_scalar(out=mu[:, c0:c0 + n], in0=p1[:, 0:n], scalar1=1.0 / 1536, scalar2=None, op0=ALU.mult), [p1], [mu])
            kb.op('dve', lambda e: e.tensor_tensor(out=t_[:, 0:n], in0=mu[:, c0:c0 + n], in1=mu[:, c0:c0 + n], op=ALU.mult), [mu], [t_])
            kb.op('dve', lambda e: e.scalar_tensor_tensor(out=t_[:, 0:n], in0=p2[:, 0:n], scalar=1.0 / 1536, in1=t_[:, 0:n], op0=ALU.mult, op1=ALU.subtract), [p2, t_], [t_])
            kb.op('dve', lambda e: e.tensor_scalar(out=t_[:, 0:n], in0=t_[:, 0:n], scalar1=1e-6, scalar2=None, op0=ALU.add), [t_], [t_])
            kb.op('act', lambda e: e.activation(out=t_[:, 0:n], in_=t_[:, 0:n], func=AF.Sqrt), [t_], [t_])
            kb.op('dve', lambda e: e.reciprocal(out=rsd[:, c0:c0 + n], in_=t_[:, 0:n]), [t_], [rsd])
            for j in range(12):
                t2 = tmpc[1]
                kb.op('dve', lambda e, t2=t2: e.tensor_tensor(out=t2[:, 0:n], in0=yc[:, j, c0:c0 + n], in1=mu[:, c0:c0 + n], op=ALU.subtract), [yc, mu], [t2])
                kb.op('pool', lambda e, t2=t2: e.tensor_tensor(out=t2[:, 0:n], in0=t2[:, 0:n], in1=rsd[:, c0:c0 + n], op=ALU.mult), [t2, rsd], [t2])
                kb.op('dve', lambda e, t2=t2: e.tensor_scalar(out=t2[:, 0:n], in0=t2[:, 0:n], scalar1=cpar[:, 1, j:j + 1], scalar2=cpar[:, 2, j:j + 1], op0=ALU.mult, op1=ALU.add), [t2, cpar], [t2])
                kb.op('act', lambda e, t2=t2: e.activation(out=conv_fT[:, j, c0:c0 + n], in_=t2[:, 0:n], func=AF.Silu), [t2], [conv_fT])
        kb.barrier()
        ph.close()

    if DBG in ('', 'CONV'):
        ph = ExitStack()
        sT = kb.sb("sT", [128, 16, NTS], BF16, ph)
        bo = kb.sb("bo", [128, 2, 16], F32, ph); kb.dma('sp', bo[:], boT[:], [], [bo], prim=bo)
        stg = [kb.sb("stg%d" % i, [128, 16, 256], F32, ph) for i in range(2)]
        wbs = [[kb.sb("wb%d_%d" % (i, j), [128, KSPL[j][1] - KSPL[j][0], 256], BF16, ph) for j in range(3)] for i in range(2)]
        sga = [kb.sb("sga%d" % i, [128, 512], F32, ph) for i in range(2)]; sgb = [kb.sb("sgb%d" % i, [128, 512], F32, ph) for i in range(2)]
        t1 = [kb.sb("t1_%d" % i, [128, 512], F32, ph) for i in range(2)]; t2_ = [kb.sb("t2_%d" % i, [128, 512], F32, ph) for i in range(2)]
        mc = {'i': 0}
        seq = []
        for cb in range(8):
            seq.append(('a', cb)); seq.append(('c', cb))

        def issue(idx):
            kind, cb = seq[idx]
            st = stg[idx % 2]
            if kind == 'a':
                kb.dma('sp', st[:, 0:8, :], w_ao[:, cb * 256:(cb + 1) * 256].rearrange("(k p) n -> p k n", p=128), [], [st], prim=st)
            else:
                kb.dma('sp', st[:, 0:12, :], w_co[:, cb * 256:(cb + 1) * 256].rearrange("(k p) n -> p k n", p=128), [], [st], prim=st)
        issue(0)
        pa_t = {}
        for idx in range(len(seq)):
            if idx + 1 < len(seq):
                issue(idx + 1)
            kind, cb = seq[idx]
            st, wb3 = stg[idx % 2], wbs[idx % 2]
            nk = 8 if kind == 'a' else 12
            cast_block(st, wb3, nk=nk)
            src = attn_oT if kind == 'a' else conv_fT
            for sub in range(2):
                jb = cb * 2 + sub
                for ci, (c0, n) in enumerate(((0, 512), (512, 512), (NT, NS))):
                    if kind == 'a':
                        p = PS[(sub * 3 + ci) % 6]
                    else:
                        p = PS[6 + (sub * 3 + ci) % 2]
                    for k in range(nk):
                        wp = wpart(wb3, k)
                        kb.op('pe', lambda e, k=k, wp=wp, p=p: e.matmul(p[:, 0:n], wp[:, k, sub * 128:(sub + 1) * 128], src[:, k, c0:c0 + n], start=(k == 0), stop=(k == nk - 1)), [wp.t, src], [p])
                    if kind == 'a':
                        pa_t[(sub, ci)] = p
                    else:
                        m = mc['i'] % 2; mc['i'] += 1
                        pa = pa_t[(sub, ci)]
                        kb.dma('sp', sga[m][:, 0:n], sg_d[jb * 128:(jb + 1) * 128, c0:c0 + n], [sg_d], [sga[m]], prim=sga[m])
                        kb.dma('sp', sgb[m][:, 0:n], sg_d[2048 + jb * 128:2048 + (jb + 1) * 128, c0:c0 + n], [sg_d], [sgb[m]], prim=sgb[m])
                        kb.op('dve', lambda e, pa=pa, m=m: e.scalar_tensor_tensor(out=t1[m][:, 0:n], in0=pa[:, 0:n], scalar=bo[:, 0, jb:jb + 1], in1=sga[m][:, 0:n], op0=ALU.add, op1=ALU.mult), [pa, bo, sga[m]], [t1[m]])
                        kb.op('dve', lambda e, p=p, m=m: e.scalar_tensor_tensor(out=t2_[m][:, 0:n], in0=p[:, 0:n], scalar=bo[:, 1, jb:jb + 1], in1=sgb[m][:, 0:n], op0=ALU.add, op1=ALU.mult), [p, bo, sgb[m]], [t2_[m]])
                        kb.op('pool', lambda e, m=m: e.tensor_tensor(out=sT[:, jb, c0:c0 + n], in0=t1[m][:, 0:n], in1=t2_[m][:, 0:n], op=ALU.add), [t1[m], t2_[m]], [sT])
        g1bc = kb.sb("g1bc", [128, D], F32, ph); g1bs = kb.sb("g1bs", [NS, D], F32, ph)
        kb.dma('pool', g1bc[:], mod_d[0:1, 2 * D:3 * D].partition_broadcast(128), [mod_d], [g1bc], prim=g1bc)
        kb.dma('pool', g1bs[:], mod_d[1:2, 2 * D:3 * D].partition_broadcast(NS), [mod_d], [g1bs], prim=g1bs)
        xp_ = [kb.sb("xp%d" % i, [128, 256], F32, ph) for i in range(3)]
        xo_ = [kb.sb("xo%d" % i, [128, 256], F32, ph) for i in range(3)]
        wo_i = {'i': 0}

        def issue_o(cb):
            st = stg[cb % 2]
            kb.dma('sp', st[:, :, :], w_o[:, cb * 256:(cb + 1) * 256].rearrange("(k p) n -> p k n", p=128), [], [st], prim=st)
        issue_o(0)
        for cb in range(8):
            if cb + 1 < 8:
                issue_o(cb + 1)
            st, wb3 = stg[cb % 2], wbs[cb % 2]
            cast_block(st, wb3)
            for t in range(9):
                rows = 128 if t < 8 else NS
                tok0 = t * 128
                p = PS[t % 6]
                for k in range(16):
                    wp = wpart(wb3, k)
                    kb.op('pe', lambda e, k=k, wp=wp, p=p: e.matmul(p[0:rows, 0:256], sT[:, k, tok0:tok0 + rows], wp[:, k, :], start=(k == 0), stop=(k == 15)), [wp.t, sT], [p])
                m = wo_i['i'] % 3; wo_i['i'] += 1
                xsrc = xh[HALO + tok0:HALO + tok0 + rows, cb * 256:(cb + 1) * 256] if t < 8 else xs[:, cb * 256:(cb + 1) * 256]
                gb = g1bc if t < 8 else g1bs
                kb.dma('sp', xp_[m][0:rows, :], xsrc, [], [xp_[m]], prim=xp_[m])
                kb.op('dve', lambda e, p=p, m=m, gb=gb: e.tensor_tensor(out=xo_[m][0:rows, :], in0=p[0:rows, 0:256], in1=gb[0:rows, cb * 256:(cb + 1) * 256], op=ALU.mult), [p, gb], [xo_[m]])
                kb.op('pool', lambda e, m=m: e.tensor_tensor(out=xo_[m][0:rows, :], in0=xo_[m][0:rows, :], in1=xp_[m][0:rows, :], op=ALU.add), [xo_[m], xp_[m]], [xo_[m]])
                kb.dma('pool', x1_d[tok0:tok0 + rows, cb * 256:(cb + 1) * 256], xo_[m][0:rows, :], [xo_[m]], [x1_d], prim=xo_[m])
        kb.barrier()
        ph.close()


    mid.close()
    AX = mybir.AxisListType.X
    NE = int(os.environ.get('KNE', '32'))
    late = ExitStack()
    h2T = kb.sb("h2T", [128, 16, NTS], BF16, late)
    gateM = kb.sb("gateM", [128, 9, 32], F32, late)
    kb.op('dve', lambda e: e.memset(gateM[:], 0.0), [], [gateM])
    ph = ExitStack()
    A2 = [kb.sb("A2_%d" % r, [128 if r == 0 else NS, D], F32, ph) for r in range(2)]
    B2 = [kb.sb("B2_%d" % r, [128 if r == 0 else NS, D], F32, ph) for r in range(2)]
    g2bc = kb.sb("g2bc", [128, D], F32, ph)
    kb.dma('pool', g2bc[:], g2_row[:, :].partition_broadcast(128), [], [g2bc], prim=g2bc)
    for r in range(2):
        n = 128 if r == 0 else NS
        kb.dma('pool', A2[r][:], mod_d[r:r + 1, 4 * D:5 * D].partition_broadcast(n), [mod_d], [A2[r]], prim=A2[r])
        kb.dma('pool', B2[r][:], mod_d[r:r + 1, 3 * D:4 * D].partition_broadcast(n), [mod_d], [B2[r]], prim=B2[r])
        kb.op('dve', lambda e, r=r, n=n: e.scalar_tensor_tensor(out=A2[r][:], in0=A2[r][:], scalar=1.0, in1=g2bc[0:n, :], op0=ALU.add, op1=ALU.mult), [A2[r], g2bc], [A2[r]])
    wr = kb.sb("wr", [128, 16, 32], F32, ph); kb.dma('sp', wr[:], w_router[:, :].rearrange("(k p) e -> p k e", p=128), [], [wr], prim=wr)
    brbc = kb.sb("brbc", [128, 32], F32, ph); kb.dma('pool', brbc[:], b_router[:, :].partition_broadcast(128), [], [brbc], prim=brbc)
    x1t = [kb.sb("x1t%d" % i, [128, D], F32, ph) for i in range(2)]
    hn = [kb.sb("hn%d" % i, [128, D], F32, ph) for i in range(2)]
    h2f = [kb.sb("h2f%d" % i, [128, 16, 128], F32, ph) for i in range(2)]
    junk2 = kb.sb("junk2", [128, D], BF16, ph)
    sm = {n: kb.sb("sm_" + n, [128, w], F32, ph) for n, w in (("ss", 1), ("rs", 1), ("lg", 32), ("mx", 8), ("mk", 32), ("nm", 1), ("ex", 32), ("su", 1))}
    for t in range(9):
        rows = 128 if t < 8 else NS
        tok0 = t * 128
        r = 0 if t < 8 else 1
        x, h_, hf = x1t[t % 2], hn[t % 2], h2f[t % 2]
        ss, rs = sm["ss"], sm["rs"]
        kb.dma('sp', x[0:rows, :], x1_d[tok0:tok0 + rows, :], [x1_d], [x], prim=x)
        kb.op('act', lambda e: e.activation(out=junk2[0:rows, :], in_=x[0:rows, :], func=AF.Square, accum_out=ss[0:rows, :]), [x], [junk2, ss])
        kb.op('dve', lambda e: e.tensor_scalar(out=ss[0:rows, :], in0=ss[0:rows, :], scalar1=1.0 / D, scalar2=1e-6, op0=ALU.mult, op1=ALU.add), [ss], [ss])
        kb.op('act', lambda e: e.activation(out=ss[0:rows, :], in_=ss[0:rows, :], func=AF.Sqrt), [ss], [ss])
        kb.op('dve', lambda e: e.reciprocal(out=rs[0:rows, :], in_=ss[0:rows, :]), [ss], [rs])
        kb.op('act', lambda e: e.activation(out=h_[0:rows, :], in_=x[0:rows, :], func=AF.Copy, scale=rs[0:rows, :]), [x, rs], [h_])
        kb.op('dve', lambda e: e.tensor_tensor(out=h_[0:rows, :], in0=h_[0:rows, :], in1=A2[r][0:rows, :], op=ALU.mult), [h_, A2[r]], [h_])
        kb.op('pool', lambda e: e.tensor_tensor(out=h_[0:rows, :], in0=h_[0:rows, :], in1=B2[r][0:rows, :], op=ALU.add), [h_, B2[r]], [h_])
        for kq in range(4):
            p = PS[kq]
            for kk in range(4):
                k = kq * 4 + kk
                kb.op('pe', lambda e, k=k, kk=kk, p=p: e.transpose(p[:, kk * 128:kk * 128 + rows], h_[0:rows, k * 128:(k + 1) * 128], ident[0:rows, 0:rows]), [h_, ident], [p])
            pv = p[:, :].rearrange("p (k t) -> p k t", k=4)
            kb.op('act', lambda e, pv=pv, kq=kq: e.activation(out=h2T[:, 4 * kq:4 * kq + 4, tok0:tok0 + rows], in_=pv[:, :, 0:rows], func=AF.Copy), [p], [h2T])
            kb.op('dve', lambda e, pv=pv, kq=kq: e.tensor_copy(out=hf[:, 4 * kq:4 * kq + 4, 0:rows], in_=pv[:, :, 0:rows]), [p], [hf])
        pr = PS[4 + t % 2]
        for k in range(16):
            kb.op('pe', lambda e, k=k: e.matmul(pr[0:rows, 0:32], hf[:, k, 0:rows], wr[:, k, :], start=(k == 0), stop=(k == 15)), [hf, wr], [pr])
        lg, mx, mk, nm, ex, su = sm["lg"], sm["mx"], sm["mk"], sm["nm"], sm["ex"], sm["su"]
        kb.op('dve', lambda e: e.tensor_tensor(out=lg[0:rows, :], in0=pr[0:rows, 0:32], in1=brbc[0:rows, :], op=ALU.add), [pr, brbc], [lg])
        kb.op('dve', lambda e: e.max(out=mx[0:rows, :], in_=lg[0:rows, :]), [lg], [mx])
        kb.op('dve', lambda e: e.tensor_scalar(out=mk[0:rows, :], in0=lg[0:rows, :], scalar1=mx[0:rows, 3:4], scalar2=None, op0=ALU.is_ge), [lg, mx], [mk])
        kb.op('dve', lambda e: e.tensor_scalar(out=nm[0:rows, :], in0=mx[0:rows, 0:1], scalar1=-1.0, scalar2=None, op0=ALU.mult), [mx], [nm])
        kb.op('act', lambda e: e.activation(out=ex[0:rows, :], in_=lg[0:rows, :], func=AF.Exp, bias=nm[0:rows, :]), [lg, nm], [ex])
        kb.op('dve', lambda e: e.tensor_tensor(out=ex[0:rows, :], in0=ex[0:rows, :], in1=mk[0:rows, :], op=ALU.mult), [ex, mk], [ex])
        kb.op('dve', lambda e: e.reduce_sum(out=su[0:rows, :], in_=ex[0:rows, :], axis=AX), [ex], [su])
        kb.op('dve', lambda e: e.reciprocal(out=su[0:rows, :], in_=su[0:rows, :]), [su], [su])
        kb.op('dve', lambda e: e.tensor_scalar(out=gateM[0:rows, t, :], in0=ex[0:rows, :], scalar1=su[0:rows, 0:1], scalar2=None, op0=ALU.mult), [ex, su], [gateM])
    kb.barrier()
    ph.close()

    late2 = ExitStack()
    accM = kb.sb("accM", [128, 9, D], F32, late2)
    ph = ExitStack()
    actT = kb.sb("actT", [128, 8, NTS], BF16, ph)
    b1 = kb.sb("b1", [128, 32, 32], F32, ph); kb.dma('sp', b1[:], b1T_d[:], [], [b1], prim=b1)
    b2 = kb.sb("b2", [32, D], F32, ph); kb.dma('sp', b2[:], b_moe2[:, :], [], [b2], prim=b2)
    gT = kb.sb("gT", [32, 128], F32, ph)
    stgm = kb.sb("stgm", [128, 16, 256], F32, ph)
    wbm = [[kb.sb("wbm%d_%d" % (i, j), [128, KSPL[j][1] - KSPL[j][0], 256], BF16, ph) for j in range(3)] for i in range(2)]
    gsT = kb.sb("gsT", [128, 2, NTS], F32, ph)
    gt = kb.sb("gt", [128, 512], F32, ph); st_ = kb.sb("st_", [128, 512], F32, ph); lt = kb.sb("lt", [128, 512], F32, ph)
    for t in range(9):
        pt = PS[6]
        kb.op('pe', lambda e, t=t, pt=pt: e.transpose(pt[0:32, 0:128], gateM[:, t, :], ident[:]), [gateM, ident], [pt])
        kb.op('act', lambda e, pt=pt: e.activation(out=gT[:, :], in_=pt[0:32, 0:128], func=AF.Copy), [pt], [gT])
        for c4 in range(4):
            p = PS[c4]
            kb.op('pe', lambda e, p=p, c4=c4: e.matmul(p[:, :], gT[:, :], b2[:, c4 * 512:(c4 + 1) * 512], start=True, stop=True), [gT, b2], [p])
            kb.op('act', lambda e, p=p, c4=c4, t=t: e.activation(out=accM[:, t, c4 * 512:(c4 + 1) * 512], in_=p[:, :], func=AF.Copy), [p], [accM])
    CH = ((0, 512), (512, 512), (NT, NS))
    wi = {'i': 0, 'p': 0}

    def mblock(src_ap, nk):
        wb3 = wbm[wi['i'] % 2]; wi['i'] += 1
        kb.dma('sp', stgm[:, 0:nk, :], src_ap.rearrange("(k p) n -> p k n", p=128), [], [stgm], prim=stgm)
        cast_block(stgm, wb3, nk=nk)
        return wb3

    def nps():
        wi['p'] += 1
        return PS[wi['p'] % 6]

    for ex_ in range(NE):
        for hf in range(2):
            for jj in range(4):
                jg = hf * 4 + jj
                wb3 = mblock(w_moe1[ex_, :, jg * 256:(jg + 1) * 256], 16)
                for sub in range(2):
                    jb = jg * 2 + sub
                    for (c0, n) in CH:
                        p = nps()
                        for k in range(16):
                            wp = wpart(wb3, k)
                            kb.op('pe', lambda e, k=k, wp=wp, p=p: e.matmul(p[:, 0:n], wp[:, k, sub * 128:(sub + 1) * 128], h2T[:, k, c0:c0 + n], start=(k == 0), stop=(k == 15)), [wp.t, h2T], [p])
                        kb.op('dve', lambda e, p=p: e.tensor_scalar(out=gt[:, 0:n], in0=p[:, 0:n], scalar1=b1[:, ex_, jb:jb + 1], scalar2=7.0, op0=ALU.add, op1=ALU.min), [p, b1], [gt])
                        kb.op('act', lambda e: e.activation(out=st_[:, 0:n], in_=gt[:, 0:n], func=AF.Sigmoid, scale=1.702), [gt], [st_])
                        kb.op('pool', lambda e: e.tensor_tensor(out=gsT[:, sub, c0:c0 + n], in0=gt[:, 0:n], in1=st_[:, 0:n], op=ALU.mult), [gt, st_], [gsT])
                wb3 = mblock(w_moe1[ex_, :, 2048 + jg * 256:2048 + (jg + 1) * 256], 16)
                for sub in range(2):
                    jb = jg * 2 + sub
                    for (c0, n) in CH:
                        p = nps()
                        for k in range(16):
                            wp = wpart(wb3, k)
                            kb.op('pe', lambda e, k=k, wp=wp, p=p: e.matmul(p[:, 0:n], wp[:, k, sub * 128:(sub + 1) * 128], h2T[:, k, c0:c0 + n], start=(k == 0), stop=(k == 15)), [wp.t, h2T], [p])
                        kb.op('dve', lambda e, p=p: e.tensor_scalar(out=lt[:, 0:n], in0=p[:, 0:n], scalar1=b1[:, ex_, 16 + jb:16 + jb + 1], scalar2=7.0, op0=ALU.add, op1=ALU.min), [p, b1], [lt])
                        kb.op('pool', lambda e: e.tensor_scalar(out=lt[:, 0:n], in0=lt[:, 0:n], scalar1=-7.0, scalar2=1.0, op0=ALU.max, op1=ALU.add), [lt], [lt])
                        kb.op('dve', lambda e: e.tensor_tensor(out=actT[:, jj * 2 + sub, c0:c0 + n], in0=gsT[:, sub, c0:c0 + n], in1=lt[:, 0:n], op=ALU.mult), [gsT, lt], [actT])
            for cb in range(8):
                wb3 = mblock(w_moe2[ex_, hf * 1024:(hf + 1) * 1024, cb * 256:(cb + 1) * 256], 8)
                for t in range(9):
                    rows = 128 if t < 8 else NS
                    tok0 = t * 128
                    p = nps()
                    for k in range(8):
                        wp = wpart(wb3, k)
                        kb.op('pe', lambda e, k=k, wp=wp, p=p: e.matmul(p[0:rows, 0:256], actT[:, k, tok0:tok0 + rows], wp[:, k, :], start=(k == 0), stop=(k == 7)), [wp.t, actT], [p])
                    kb.op('dve', lambda e, p=p, t=t: e.scalar_tensor_tensor(out=accM[0:rows, t, cb * 256:(cb + 1) * 256], in0=p[0:rows, 0:256], scalar=gateM[0:rows, t, ex_:ex_ + 1], in1=accM[0:rows, t, cb * 256:(cb + 1) * 256], op0=ALU.mult, op1=ALU.add), [p, gateM, accM], [accM])
    kb.barrier()
    ph.close()

    ph = ExitStack()
    g5 = [kb.sb("g5_%d" % r, [128 if r == 0 else NS, D], F32, ph) for r in range(2)]
    gfbc = kb.sb("gfbc", [128, D], F32, ph)
    kb.dma('pool', gfbc[:], gf_row[:, :].partition_broadcast(128), [], [gfbc], prim=gfbc)
    for r in range(2):
        kb.dma('pool', g5[r][:], mod_d[r:r + 1, 5 * D:6 * D].partition_broadcast(128 if r == 0 else NS), [mod_d], [g5[r]], prim=g5[r])
    x1t = [kb.sb("x1f%d" % i, [128, D], F32, ph) for i in range(2)]
    yo = [kb.sb("yo%d" % i, [128, D], F32, ph) for i in range(2)]
    junk3 = kb.sb("junk3", [128, D], BF16, ph)
    ss2 = kb.sb("ss2", [128, 1], F32, ph); rs2 = kb.sb("rs2", [128, 1], F32, ph)
    for t in range(9):
        rows = 128 if t < 8 else NS
        tok0 = t * 128
        r = 0 if t < 8 else 1
        x, y = x1t[t % 2], yo[t % 2]
        kb.dma('sp', x[0:rows, :], x1_d[tok0:tok0 + rows, :], [x1_d], [x], prim=x)
        kb.op('dve', lambda e: e.tensor_tensor(out=y[0:rows, :], in0=accM[0:rows, t, :], in1=g5[r][0:rows, :], op=ALU.mult), [accM, g5[r]], [y])
        kb.op('pool', lambda e: e.tensor_tensor(out=y[0:rows, :], in0=y[0:rows, :], in1=x[0:rows, :], op=ALU.add), [y, x], [y])
        kb.op('act', lambda e: e.activation(out=junk3[0:rows, :], in_=y[0:rows, :], func=AF.Square, accum_out=ss2[0:rows, :]), [y], [junk3, ss2])
        kb.op('dve', lambda e: e.tensor_scalar(out=ss2[0:rows, :], in0=ss2[0:rows, :], scalar1=1.0 / D, scalar2=1e-6, op0=ALU.mult, op1=ALU.add), [ss2], [ss2])
        kb.op('act', lambda e: e.activation(out=ss2[0:rows, :], in_=ss2[0:rows, :], func=AF.Sqrt), [ss2], [ss2])
        kb.op('dve', lambda e: e.reciprocal(out=rs2[0:rows, :], in_=ss2[0:rows, :]), [ss2], [rs2])
        kb.op('act', lambda e: e.activation(out=y[0:rows, :], in_=y[0:rows, :], func=AF.Copy, scale=rs2[0:rows, :]), [y, rs2], [y])
        kb.op('dve', lambda e: e.tensor_tensor(out=y[0:rows, :], in0=y[0:rows, :], in1=gfbc[0:rows, :], op=ALU.mult), [y, gfbc], [y])
        if t < 8:
            kb.dma('pool', y_o[tok0:tok0 + 128, :], y[:, :], [y], [y_o], prim=y)
        else:
            kb.dma('pool', ys_o[:, :], y[0:NS, :], [y], [ys_o], prim=y)
    kb.finish()
    ph.close()
    late2.close()
    late.close()
    es.close()
    return nc


_NC = None
CORES = list(range(NCORES))


def kernel(**inp):
    global _NC
    f = lambda a: np.ascontiguousarray(np.asarray(a, dtype=np.float32))
    xp = f(inp['x_prompt']); xs = f(inp['x_sample'])
    shared = {
        "ident": np.eye(128, dtype=np.float32),
        "w_ada": f(inp['w_ada'][0]), "b_adaT": f(inp['b_ada'][0].reshape(96, 128).T), "b_ada_row": f(inp['b_ada'][0].reshape(1, -1)),
        "g1T": f(inp['g_norm1'][0].reshape(16, 128).T),
        "w_in": f(inp['w_in'][0]), "b_inT": f(inp['b_in'][0].reshape(128, 128).T), "b_in_row": f(inp['b_in'][0].reshape(1, -1)),
    }
    shared["w_dwT"] = f(inp['w_dw'][0].T.reshape(12, 128, 31).transpose(1, 0, 2))
    shared["cparT"] = f(np.stack([inp['b_dw'][0].reshape(12, 128).T, inp['g_ln_conv'][0].reshape(12, 128).T, inp['b_ln_conv'][0].reshape(12, 128).T], axis=1))
    shared["w_ao"] = f(inp['w_attn_out'][0]); shared["w_co"] = f(inp['w_conv_out'][0]); shared["w_o"] = f(inp['w_o'][0])
    shared["boT"] = f(np.stack([inp['b_attn_out'][0].reshape(16, 128).T, inp['b_conv_out'][0].reshape(16, 128).T], axis=1))
    shared["g2_row"] = f(inp['g_norm2'][0].reshape(1, -1)); shared["w_router"] = f(inp['w_router'][0]); shared["b_router"] = f(inp['b_router'][0].reshape(1, -1))
    shared["w_moe1"] = f(inp['w_moe1'][0]); shared["w_moe2"] = f(inp['w_moe2'][0]); shared["b_moe2"] = f(inp['b_moe2'][0]); shared["gf_row"] = f(inp['g_final'].reshape(1, -1))
    shared["b1T"] = f(inp['b_moe1'][0].reshape(32, 32, 128).transpose(2, 0, 1))
    pp = np.arange(128)[:, None]; nn = np.arange(256)[None, :]
    shared["M1"] = ((nn - pp >= 0) & (nn - pp <= 128)).astype(np.float32)
    SMc = np.zeros((128, 21, NS), np.float32); SMn = np.zeros((NS, 3, NS), np.float32)
    ti = 0
    for g, (W, dd) in enumerate(((128, 1), (512, 4), (2048, 16))):
        for j in range(W // 128):
            m = 128 * j + np.arange(128)[:, None]; ii = np.arange(NS)[None, :]
            SMc[:, ti, :] = (((ii - m) % dd == 0) & (m >= ii)).astype(np.float32)
            ti += 1
        i1 = np.arange(NS)[:, None]; ii = np.arange(NS)[None, :]
        SMn[:, g, :] = ((i1 <= ii) & ((ii - i1) % dd == 0)).astype(np.float32)
    shared["SMc"] = SMc; shared["SMn"] = SMn
    in_maps = []
    for c in CORES:
        b, q = c // 4, c % 4
        s = q * NT
        xh = np.zeros((HALO + NT, D), np.float32)
        lo = max(0, s - HALO)
        xh[HALO - (s - lo):] = xp[b, lo:s + NT]
        cT = np.stack([f(inp['c_prompt'][b]).reshape(16, 128).T, f(inp['c_sample'][c]).reshape(16, 128).T], axis=-1)
        m = dict(shared)
        kval = np.zeros((128, 53), np.float32)
        p1 = np.arange(128)
        for j in range(9):
            kval[:, j] = (s - 128 + 128 * j + p1 >= 0)
        for r in range(4):
            for j in range(3):
                kval[:, 9 + r * 3 + j] = (s - 512 + 512 * j + 4 * p1 + r >= 0)
        for r in range(16):
            kval[:, 21 + 2 * r] = (s - 2048 + 16 * p1 + r >= 0)
            kval[:, 21 + 2 * r + 1] = (s - 2048 + 16 * (128 + p1) + r >= 0)
        m.update({"xh": xh, "xs": f(xs[c]), "cT": f(cT), "kval": kval, "hprev": np.full((128, 1), 1.0 if q > 0 else 0.0, np.float32),
                  "stT": f(inp['state_conv'][0, c].T), "st": f(inp['state_conv'][0, c]),
                  "cache0": f(inp['cache_kv_w128'][0, c]), "cache1": f(inp['cache_kv_w512'][0, c]), "cache2": f(inp['cache_kv_w2048'][0, c])})
        in_maps.append(m)
    if _NC is None:
        _NC = build()
    res = run_bass_kernel_spmd(_NC, in_maps, core_ids=list(range(len(CORES)))).results
    B, S = 2, 4096
    y_p = np.zeros((B, S, D), np.float32); y_s = np.zeros((8, NS, D), np.float32)
    kvp = [np.zeros((1, B, w, 2, 8, 128), np.float32) for w in (128, 512, 2048)]
    kvs = [np.zeros((1, 8, NS, 2, 8, 128), np.float32) for _ in range(3)]
    conv_p = np.zeros((1, B, 30, 1536), np.float32); conv_s = np.zeros((1, 8, 30, 1536), np.float32)
    for ci, c in enumerate(CORES):
        b, q = c // 4, c % 4
        r = res[ci]
        y_p[b, q * NT:(q + 1) * NT] = r["y_o"]
        y_s[c] = r["ys_o"]
        if q == 3:
            kvp[0][0, b] = r["kv1_o"]; kvp[1][0, b] = r["kv2_o"]; conv_p[0, b] = r["convp_o"]
        if q >= 2:
            kvp[2][0, b, (q - 2) * NT:(q - 1) * NT] = r["kv3_o"]
        for g in range(3):
            kvs[g][0, c] = r["kvs_o"][g]
        conv_s[0, c] = r["convs_o"]
    return (y_p, y_s, kvp[0], kvp[1], kvp[2], conv_p, kvs[0], kvs[1], kvs[2], conv_s)
```

```python
import numpy as np
from contextlib import ExitStack
import concourse.bass as bass
import concourse.mybir as mybir
from concourse.bass_utils import run_bass_kernel_spmd

dt = mybir.dt
F32, BF16, I32, U32 = dt.float32, dt.bfloat16, dt.int32, dt.uint32
AF = mybir.ActivationFunctionType
ALU = mybir.AluOpType

D = 2048
NT = 1024
NS = 8
NTS = NT + NS
HALO = 2048
NCORES = 8
CAP = 256
import os
DBG = os.environ.get('KDBG', '')


class T:
    def __init__(self, h, name):
        self.h = h
        self.name = name
        self.w = {}
        self.r = {}
        self.dsem = {}
        self.excl = False
        self.nowaw = False

    def __getitem__(self, k):
        return self.h[k]


class KB:
    def __init__(self, nc, es):
        self.nc = nc
        self.es = es
        self.eng = dict(pe=nc.tensor, act=nc.scalar, dve=nc.vector, pool=nc.gpsimd, sp=nc.sync)
        self.sems = []
        self.semcnt = []
        self.esem = {}
        for k in self.eng:
            self.esem[k] = self.new_sem('e_' + k)
        self.cnt = {k: 0 for k in self.eng}
        self.waited = {k: {} for k in self.eng}
        self.dfree = {'sw': [self.new_sem('dsw%d' % i) for i in range(48)], 'hw': [self.new_sem('dhw%d' % i) for i in range(44)]}
        self.dused = []
        self.uid = 0

    def new_sem(self, name):
        s = self.es.enter_context(self.nc.semaphore(name))
        self.sems.append(s)
        self.semcnt.append(0)
        return len(self.sems) - 1

    def sb(self, name, shape, dtype=F32, es=None):
        self.uid += 1
        es = es or self.es
        return T(es.enter_context(self.nc.sbuf_tensor("%s_%d" % (name, self.uid), list(shape), dtype)), name)

    def ps(self, name, shape, dtype=F32, es=None):
        self.uid += 1
        es = es or self.es
        t = T(es.enter_context(self.nc.psum_tensor("%s_%d" % (name, self.uid), list(shape), dtype)), name)
        t.excl = True
        return t

    def dram(self, name, shape, dtype=F32, kind="Internal"):
        t = T(self.nc.dram_tensor(name, list(shape), dtype, kind=kind).ap(), name)
        t.nowaw = True
        return t

    def _wait(self, e, deps, skip=None):
        wd = self.waited[e]
        for si, v in deps.items():
            if si == skip or wd.get(si, 0) >= v:
                continue
            self.eng[e].wait_ge(self.sems[si], v)
            wd[si] = v

    @staticmethod
    def _merge(d, o):
        for k, v in o.items():
            if d.get(k, 0) < v:
                d[k] = v

    def _deps(self, reads, writes):
        deps = {}
        for t in reads:
            self._merge(deps, t.w)
            if t.excl:
                self._merge(deps, t.r)
        for t in writes:
            if not t.nowaw:
                self._merge(deps, t.w)
            self._merge(deps, t.r)
        return deps

    def _post(self, ev, reads, writes):
        for t in writes:
            if t.nowaw:
                self._merge(t.w, ev)
            else:
                t.w = dict(ev)
            t.r = {}
        for t in reads:
            if t not in writes:
                self._merge(t.r, ev)

    def op(self, e, fn, reads=(), writes=()):
        si = self.esem[e]
        self._wait(e, self._deps(reads, writes), skip=si if e == 'pe' else None)
        inst = fn(self.eng[e])
        self.cnt[e] += 1
        self.semcnt[si] = self.cnt[e]
        inst.then_inc(self.sems[si], 1)
        self._post({si: self.cnt[e]}, reads, writes)
        return inst

    def dma(self, q, out, in_, reads=(), writes=(), prim=None, fn=None):
        kind = 'sw' if q == 'pool' else 'hw'
        if kind not in prim.dsem:
            prim.dsem[kind] = self.dfree[kind].pop()
            if prim not in self.dused:
                self.dused.append(prim)
        si = prim.dsem[kind]
        self._wait(q, self._deps(reads, writes), skip=si if prim in writes else None)
        inst = self.eng[q].dma_start(out=out, in_=in_) if fn is None else fn(self.eng[q])
        self.semcnt[si] += 16
        inst.then_inc(self.sems[si], 16)
        self._post({si: self.semcnt[si]}, reads, writes)
        return inst

    def barrier(self, keep=()):
        allv = {si: v for si, v in enumerate(self.semcnt) if v > 0}
        for e in self.eng:
            self._wait(e, allv, skip=self.esem[e])
        rest = []
        for t in self.dused:
            if t in keep:
                rest.append(t)
            else:
                for kind, si in t.dsem.items():
                    self.dfree[kind].append(si)
                t.dsem = {}
        self.dused = rest

    def finish(self):
        allv = {si: v for si, v in enumerate(self.semcnt) if v > 0}
        for e in self.eng:
            self._wait(e, allv, skip=self.esem[e])


def build():
    nc = bass.Bass("TRN2", target_bir_lowering=False)
    es = ExitStack()
    kb = KB(nc, es)
    IN = lambda n, s, d=F32: kb.dram(n, s, d, "ExternalInput")
    OUT = lambda n, s, d=F32: kb.dram(n, s, d, "ExternalOutput")
    xh = IN("xh", [HALO + NT, D]); xs = IN("xs", [NS, D]); cT = IN("cT", [128, 16, 2])
    ident_d = IN("ident", [128, 128])
    w_ada = IN("w_ada", [D, 6 * D]); b_adaT = IN("b_adaT", [128, 96]); b_ada_row = IN("b_ada_row", [1, 6 * D])
    g1T = IN("g1T", [128, 16])
    M1_d = IN("M1", [128, 256]); kval_d = IN("kval", [128, 53]); SMc_d = IN("SMc", [128, 21, NS]); SMn_d = IN("SMn", [NS, 3, NS])
    cache_d = [IN("cache%d" % g, [w, 2, 8, 128]) for g, w in enumerate((128, 512, 2048))]
    w_dwT = IN("w_dwT", [128, 12, 31]); cparT = IN("cparT", [128, 3, 12]); hprev_d = IN("hprev", [128, 1])
    stT_d = IN("stT", [1536, 30]); st_d = IN("st", [30, 1536])
    w_ao = IN("w_ao", [1024, D]); w_co = IN("w_co", [1536, D]); boT = IN("boT", [128, 2, 16]); w_o = IN("w_o", [D, D])
    g2_row = IN("g2_row", [1, D]); w_router = IN("w_router", [D, 32]); b_router = IN("b_router", [1, 32])
    w_moe1 = IN("w_moe1", [32, D, 2 * D]); b1T_d = IN("b1T", [128, 32, 32]); w_moe2 = IN("w_moe2", [32, D, D]); b_moe2 = IN("b_moe2", [32, D]); gf_row = IN("gf_row", [1, D])
    w_in = IN("w_in", [D, 16384]); b_inT = IN("b_inT", [128, 128]); b_in_row = IN("b_in_row", [1, 16384])
    y_o = OUT("y_o", [NT, D]); ys_o = OUT("ys_o", [NS, D])
    kv_o = [OUT("kv1_o", [128, 2, 8, 128]), OUT("kv2_o", [512, 2, 8, 128]), OUT("kv3_o", [1024, 2, 8, 128])]
    kvs_o = OUT("kvs_o", [3, NS, 2, 8, 128])
    convp_o = OUT("convp_o", [30, 1536]); convs_o = OUT("convs_o", [30, 1536])
    GH = [128, 512, 2048]
    NCTX = [GH[g] + NT for g in range(3)]
    kT_d = [kb.dram("kT_d%d" % g, [8, 128, NCTX[g]], BF16) for g in range(3)]
    V_d = [kb.dram("V_d%d" % g, [NCTX[g], 8, 128], BF16) for g in range(3)]
    qT_d = kb.dram("qT_d", [3, 8, 128, NT], BF16)
    qsT_d = kb.dram("qsT_d", [3, 8, 128, NS], BF16); ksT_d = kb.dram("ksT_d", [3, 8, 128, NS], BF16)
    Vs_d = kb.dram("Vs_d", [3, NS, 8, 128], BF16)
    u_d = kb.dram("u_d", [1536, 128 + NT]); us_d = kb.dram("us_d", [1536, NS])
    sg_d = kb.dram("sg_d", [4096, NTS])
    mod_d = kb.dram("mod_d", [2, 6 * D])

    ident = kb.sb("ident", [128, 128]); kb.dma('sp', ident[:], ident_d[:], [ident_d], [ident], prim=ident)
    identb = kb.sb("identb", [128, 128], BF16)
    kb.op('dve', lambda e: e.tensor_copy(out=identb[:], in_=ident[:]), [ident], [identb])
    A1 = kb.sb("A1", [128, 16, 2]); B1 = kb.sb("B1", [128, 16, 2])
    PS = [kb.ps("ps%d" % i, [128, 512]) for i in range(8)]

    ph = ExitStack()
    sil = kb.sb("sil", [128, 16, 2], F32, ph); silb = kb.sb("silb", [128, 16, 2], BF16, ph)
    badT = kb.sb("badT", [128, 96], F32, ph); g1s = kb.sb("g1s", [128, 16], F32, ph)
    modT = kb.sb("modT", [128, 32, 2], F32, ph)
    kb.dma('sp', sil[:], cT[:], [cT], [sil], prim=sil)
    kb.dma('sp', badT[:], b_adaT[:], [b_adaT], [badT], prim=badT)
    kb.dma('sp', g1s[:], g1T[:], [g1T], [g1s], prim=g1s)
    kb.op('act', lambda e: e.activation(out=silb[:], in_=sil[:], func=AF.Silu), [sil], [silb])
    stg = [kb.sb("stg%d" % i, [128, 16, 256], F32, ph) for i in range(2)]
    KSPL = [(0, 6, 'act'), (6, 12, 'dve'), (12, 16, 'pool')]
    wbs = [[kb.sb("wb%d_%d" % (i, j), [128, KSPL[j][1] - KSPL[j][0], 256], BF16, ph) for j in range(3)] for i in range(2)]

    def cast_block(stage, wb3, nk=16, n=256):
        for j, (k0, k1, e) in enumerate(KSPL):
            k1 = min(k1, nk)
            if k0 >= k1:
                continue
            if e == 'act':
                kb.op('act', lambda en, k0=k0, k1=k1, j=j: en.activation(out=wb3[j][:, 0:k1 - k0, 0:n], in_=stage[:, k0:k1, 0:n], func=AF.Copy), [stage], [wb3[j]])
            else:
                kb.op(e, lambda en, k0=k0, k1=k1, j=j: en.tensor_copy(out=wb3[j][:, 0:k1 - k0, 0:n], in_=stage[:, k0:k1, 0:n]), [stage], [wb3[j]])

    class WP:
        def __init__(self, t, kl):
            self.t, self.kl = t, kl

        def __getitem__(self, key):
            p, k, c = key
            return self.t[p, self.kl, c]

    def wpart(wb3, k):
        for j, (k0, k1, e) in enumerate(KSPL):
            if k0 <= k < k1:
                return WP(wb3[j], k - k0)

    def load_block(w_ap2d, col0, n, stage, nk=16):
        kb.dma('sp', stage[:, 0:nk, 0:n], w_ap2d[:, col0:col0 + n].rearrange("(k p) n -> p k n", p=128), [], [stage], prim=stage)

    modrow = kb.sb("modrow", [2, 256], F32, ph); badrow = kb.sb("badrow", [2, 6 * D], F32, ph)
    kb.dma('pool', badrow[:], b_ada_row[:].partition_broadcast(2), [b_ada_row], [badrow], prim=badrow)
    nblk = 6 * D // 256
    load_block(w_ada, 0, 256, stg[0])
    for bi in range(nblk):
        st, wb3 = stg[bi % 2], wbs[bi % 2]
        if bi + 1 < nblk:
            load_block(w_ada, (bi + 1) * 256, 256, stg[(bi + 1) % 2])
        cast_block(st, wb3)
        if bi < 16:
            for sub in range(2):
                p = PS[sub]
                for k in range(16):
                    wp = wpart(wb3, k)
                    kb.op('pe', lambda e, k=k, wp=wp, sub=sub, p=p: e.matmul(p[:, 0:2], wp[:, k, sub * 128:(sub + 1) * 128], silb[:, k, :], start=(k == 0), stop=(k == 15)), [wp.t, silb], [p])
                blk = bi * 2 + sub
                kb.op('dve', lambda e, p=p, blk=blk: e.tensor_scalar(out=modT[:, blk, :], in0=p[:, 0:2], scalar1=badT[:, blk:blk + 1], scalar2=None, op0=ALU.add), [p, badT], [modT])
        else:
            p = PS[2 + bi % 2]
            for k in range(16):
                wp = wpart(wb3, k)
                kb.op('pe', lambda e, k=k, wp=wp, p=p: e.matmul(p[0:2, 0:256], silb[:, k, :], wp[:, k, :], start=(k == 0), stop=(k == 15)), [wp.t, silb], [p])
            kb.op('dve', lambda e, p=p, bi=bi: e.tensor_tensor(out=modrow[:], in0=p[0:2, 0:256], in1=badrow[:, bi * 256:(bi + 1) * 256], op=ALU.add), [p, badrow], [modrow])
            kb.dma('pool', mod_d[:, bi * 256:(bi + 1) * 256], modrow[:], [modrow], [mod_d], prim=modrow)
    for c in range(2):
        kb.op('dve', lambda e, c=c: e.scalar_tensor_tensor(out=A1[:, :, c], in0=modT[:, 16:32, c], scalar=1.0, in1=g1s[:], op0=ALU.add, op1=ALU.mult), [modT, g1s], [A1])
        kb.op('dve', lambda e, c=c: e.tensor_copy(out=B1[:, :, c], in_=modT[:, 0:16, c]), [modT], [B1])
    kb.barrier()
    ph.close()

    ph = ExitStack()
    hT = kb.sb("hT", [128, 16, NTS], BF16, ph)
    xt = [kb.sb("xt%d" % i, [128, D], F32, ph) for i in range(2)]
    xn = [kb.sb("xn%d" % i, [128, D], F32, ph) for i in range(2)]
    junk = kb.sb("junk", [128, D], BF16, ph)
    ssq = [kb.sb("ssq%d" % i, [128, 1], F32, ph) for i in range(2)]
    rstd = [kb.sb("rstd%d" % i, [128, 1], F32, ph) for i in range(2)]
    binT = kb.sb("binT", [128, 128], F32, ph)
    kb.dma('sp', binT[:], b_inT[:], [b_inT], [binT], prim=binT)
    stg = [kb.sb("stg%d" % i, [128, 16, 256], F32, ph) for i in range(2)]
    wbs = [[kb.sb("wb%d_%d" % (i, j), [128, KSPL[j][1] - KSPL[j][0], 256], BF16, ph) for j in range(3)] for i in range(2)]
    ev_bf = [kb.sb("evbf%d" % i, [128, 512], BF16, ph) for i in range(4)]
    ev_f = [kb.sb("evf%d" % i, [128, 512], F32, ph) for i in range(4)]
    sgt = [kb.sb("sgt%d" % i, [128, 512], F32, ph) for i in range(2)]
    vb = [kb.sb("vb%d" % i, [128, 256], F32, ph) for i in range(2)]
    ko = [kb.sb("ko%d" % i, [128, 128], F32, ph) for i in range(4)]
    cnt = {'ev': 0, 'ps': 0, 'ko': 0, 'x': 0, 'vb': 0, 'sg': 0}

    def rr(key, n):
        cnt[key] += 1
        return (cnt[key] - 1) % n

    def make_hT(src_ap, nrows, col0, grp):
        i = rr('x', 2)
        x, xnn, ss, rs = xt[i], xn[i], ssq[i], rstd[i]
        kb.dma('sp', x[0:nrows, :], src_ap, [], [x], prim=x)
        kb.op('act', lambda e: e.activation(out=junk[0:nrows, :], in_=x[0:nrows, :], func=AF.Square, accum_out=ss[0:nrows, :]), [x], [junk, ss])
        kb.op('dve', lambda e: e.tensor_scalar(out=ss[0:nrows, :], in0=ss[0:nrows, :], scalar1=1.0 / D, scalar2=1e-6, op0=ALU.mult, op1=ALU.add), [ss], [ss])
        kb.op('act', lambda e: e.activation(out=ss[0:nrows, :], in_=ss[0:nrows, :], func=AF.Sqrt), [ss], [ss])
        kb.op('dve', lambda e: e.reciprocal(out=rs[0:nrows, :], in_=ss[0:nrows, :]), [ss], [rs])
        kb.op('act', lambda e: e.activation(out=xnn[0:nrows, :], in_=x[0:nrows, :], func=AF.Copy, scale=rs[0:nrows, :]), [x, rs], [xnn])
        for kq in range(4):
            p = PS[4 + rr('ps', 4)]
            for kk in range(4):
                k = kq * 4 + kk
                kb.op('pe', lambda e, k=k, kk=kk, p=p: e.transpose(p[:, kk * 128:kk * 128 + nrows], xnn[0:nrows, k * 128:(k + 1) * 128], ident[0:nrows, 0:nrows]), [xnn, ident], [p])
            for kk in range(4):
                k = kq * 4 + kk
                kb.op('dve' if kk % 2 == 0 else 'pool' if False else 'dve', lambda e, k=k, kk=kk, p=p: e.tensor_scalar(out=hT[:, k, col0:col0 + nrows], in0=p[:, kk * 128:kk * 128 + nrows], scalar1=A1[:, k, grp:grp + 1], scalar2=B1[:, k, grp:grp + 1], op0=ALU.mult, op1=ALU.add), [p, A1, B1], [hT])

    QO, KO, VO, UAO, UBO, GAO, GBO = 0, 3072, 6144, 9216, 10752, 12288, 14336
    wq = {'i': 0}

    def stream(blocks, body):
        def issue(bi):
            st = stg[(wq['i'] + bi) % 2]
            for (c0, n, d0) in blocks[bi]:
                kb.dma('sp', st[:, :, d0:d0 + n], w_in[:, c0:c0 + n].rearrange("(k p) n -> p k n", p=128), [], [st], prim=st)
        issue(0)
        for bi in range(len(blocks)):
            if bi + 1 < len(blocks):
                issue(bi + 1)
            st, wb3 = stg[(wq['i'] + bi) % 2], wbs[(wq['i'] + bi) % 2]
            cast_block(st, wb3)
            body(bi, wb3)
        wq['i'] += len(blocks)

    def fm_mm(wb3, sub, tok0, ntok):
        p = PS[rr('ps', 4)]
        for k in range(16):
            wp = wpart(wb3, k)
            kb.op('pe', lambda e, k=k, wp=wp: e.matmul(p[:, 0:ntok], wp[:, k, sub * 128:(sub + 1) * 128], hT[:, k, tok0:tok0 + ntok], start=(k == 0), stop=(k == 15)), [wp.t, hT], [p])
        return p

    def k_block(g, h, wb3, sub, tokblocks, ctx0, out_rows):
        blk = (KO + (g * 8 + h) * 128) // 128
        for (c0, ntok, cc) in tokblocks:
            p = fm_mm(wb3, sub, c0, ntok)
            i = rr('ev', 4)
            eb, ef = ev_bf[i], ev_f[i]
            need = [(t0, r0) for (t0, r0) in out_rows if c0 <= t0 < c0 + ntok]
            if need:
                kb.op('dve', lambda e, p=p, ef=ef: e.tensor_scalar(out=ef[:, 0:ntok], in0=p[:, 0:ntok], scalar1=binT[:, blk:blk + 1], scalar2=None, op0=ALU.add), [p, binT], [ef])
                kb.op('act', lambda e, ef=ef, eb=eb: e.activation(out=eb[:, 0:ntok], in_=ef[:, 0:ntok], func=AF.Copy), [ef], [eb])
            else:
                kb.op('act', lambda e, p=p, eb=eb: e.activation(out=eb[:, 0:ntok], in_=p[:, 0:ntok], func=AF.Identity, bias=binT[:, blk:blk + 1]), [p, binT], [eb])
            kb.dma('pool', kT_d[g][h, :, cc:cc + ntok], eb[:, 0:ntok], [eb], [kT_d[g]], prim=eb)
            if need:
                for (t0, r0) in need:
                    pt = PS[4 + rr('ps', 4)]
                    kb.op('pe', lambda e, pt=pt, ef=ef, t0=t0: e.transpose(pt[:, 0:128], ef[:, t0 - c0:t0 - c0 + 128], ident[:]), [ef, ident], [pt])
                    kk = ko[rr('ko', 4)]
                    kb.op('act', lambda e, pt=pt, kk=kk: e.activation(out=kk[:], in_=pt[:, 0:128], func=AF.Copy), [pt], [kk])
                    if 'd' not in os.environ.get('KSKIP', ''):
                        kb.dma(os.environ.get('KSQ', 'pool'), kv_o[g][r0:r0 + 128, 0, h, :], kk[:], [kk], [kv_o[g]], prim=kk)

    def v_block(g, h0, wb3, toktiles, out_rows):
        c0 = VO + (g * 8 + h0) * 128
        b = vb[rr('vb', 2)]
        kb.dma('pool', b[:], b_in_row[:, c0:c0 + 256].partition_broadcast(128), [b_in_row], [b], prim=b)
        for (t0, cr) in toktiles:
            p = PS[rr('ps', 4)]
            for k in range(16):
                wp = wpart(wb3, k)
                kb.op('pe', lambda e, k=k, wp=wp, p=p: e.matmul(p[:, 0:256], hT[:, k, t0:t0 + 128], wp[:, k, :], start=(k == 0), stop=(k == 15)), [wp.t, hT], [p])
            i = rr('ev', 4)
            eb, ef = ev_bf[i], ev_f[i]
            kb.op('dve', lambda e, p=p, ef=ef: e.tensor_tensor(out=ef[:, 0:256], in0=p[:, 0:256], in1=b[:], op=ALU.add), [p, b], [ef])
            kb.op('act', lambda e, eb=eb, ef=ef: e.activation(out=eb[:, 0:256], in_=ef[:, 0:256], func=AF.Copy), [ef], [eb])
            kb.dma('pool', V_d[g][cr:cr + 128, h0:h0 + 2, :], eb[:, 0:256].rearrange("p (h d) -> p h d", h=2), [eb], [V_d[g]], prim=eb)
            for (tt0, r0) in out_rows:
                if tt0 == t0:
                    kb.dma('pool', kv_o[g][r0:r0 + 128, 1, h0:h0 + 2, :], ef[:, 0:256].rearrange("p (h d) -> p h d", h=2), [ef], [kv_o[g]], prim=ef)

    def u_block(j, wb3, tokblocks):
        ba, bb = (UAO // 128) + j, (UBO // 128) + j
        for (c0, ntok, dst, dc) in tokblocks:
            pa = fm_mm(wb3, 0, c0, ntok)
            pb = fm_mm(wb3, 1, c0, ntok)
            s = sgt[rr('sg', 2)]
            i = rr('ev', 4)
            ef = ev_f[i]
            kb.op('act', lambda e, pb=pb, s=s: e.activation(out=s[:, 0:ntok], in_=pb[:, 0:ntok], func=AF.Sigmoid, bias=binT[:, bb:bb + 1]), [pb, binT], [s])
            kb.op('dve', lambda e, pa=pa, s=s, ef=ef: e.scalar_tensor_tensor(out=ef[:, 0:ntok], in0=pa[:, 0:ntok], scalar=binT[:, ba:ba + 1], in1=s[:, 0:ntok], op0=ALU.add, op1=ALU.mult), [pa, s, binT], [ef])
            kb.dma('pool', dst[j * 128:(j + 1) * 128, dc:dc + ntok], ef[:, 0:ntok], [ef], [dst], prim=ef)

    for grp_i in range(2 if DBG in ('', 'A', 'B') else 0):
        if DBG == 'A' and grp_i == 1:
            break
        base = grp_i * 1024
        for t in range(8):
            make_hT(xh[base + t * 128: base + (t + 1) * 128, :], 128, t * 128, 0)
        blocks, kinds = [], []
        for hp in range(4):
            blocks.append([(KO + (2 * 8 + 2 * hp) * 128, 256, 0)]); kinds.append(('k', 2, 2 * hp))
        for hp in range(4):
            blocks.append([(VO + (2 * 8 + 2 * hp) * 128, 256, 0)]); kinds.append(('v', 2, 2 * hp))
        if grp_i == 1:
            for g in (1, 0):
                for hp in range(4):
                    blocks.append([(KO + (g * 8 + 2 * hp) * 128, 256, 0)]); kinds.append(('k', g, 2 * hp))
                for hp in range(4):
                    blocks.append([(VO + (g * 8 + 2 * hp) * 128, 256, 0)]); kinds.append(('v', g, 2 * hp))
            for j in range(12):
                blocks.append([(UAO + j * 128, 128, 0), (UBO + j * 128, 128, 128)]); kinds.append(('u', j, 0))

        def body(bi, wb3, kinds=kinds, base=base):
            kind, g, h0 = kinds[bi]
            if kind == 'k':
                for sub in range(2):
                    if g == 2:
                        tb = [(0, 512, base), (512, 512, base + 512)]
                    elif g == 1:
                        tb = [(512, 512, 0)]
                    else:
                        tb = [(896, 128, 0)]
                    k_block(g, h0 + sub, wb3, sub, tb, 0, [])
            elif kind == 'v':
                if g == 2:
                    tt = [(t * 128, base + t * 128) for t in range(8)]
                elif g == 1:
                    tt = [(512 + t * 128, t * 128) for t in range(4)]
                else:
                    tt = [(896, 0)]
                v_block(g, h0, wb3, tt, [])
            else:
                u_block(g, wb3, [(896, 128, u_d, 0)])
        stream(blocks, body)

    for t in range(8 if DBG in ('', 'C') else 0):
        make_hT(xh[HALO + t * 128: HALO + (t + 1) * 128, :], 128, t * 128, 0)
    if DBG in ('', 'C'):
        make_hT(xs[:, :], NS, NT, 1)
    blocks, kinds = [], []
    for g in range(3):
        for hp in range(4):
            blocks.append([(QO + (g * 8 + 2 * hp) * 128, 256, 0)]); kinds.append(('q', g, 2 * hp))
        for hp in range(4):
            blocks.append([(KO + (g * 8 + 2 * hp) * 128, 256, 0)]); kinds.append(('k', g, 2 * hp))
        for hp in range(4):
            blocks.append([(VO + (g * 8 + 2 * hp) * 128, 256, 0)]); kinds.append(('v', g, 2 * hp))
    for j in range(12):
        blocks.append([(UAO + j * 128, 128, 0), (UBO + j * 128, 128, 128)]); kinds.append(('u', j, 0))
    for j in range(16):
        blocks.append([(GAO + j * 128, 256, 0)] if False else [(GAO + 2 * j * 128, 256, 0)]); kinds.append(('g', 2 * j, 0))
    vs_sb = kb.sb("vs_sb", [NS, 256], F32, ph); vs_bf = kb.sb("vs_bf", [NS, 256], BF16, ph)
    sm_bf = kb.sb("sm_bf", [128, NS], BF16, ph); sm_f = kb.sb("sm_f", [128, NS], F32, ph); sm_t = kb.sb("sm_t", [NS, 128], F32, ph)

    def bodyC(bi, wb3):
        kind, g, h0 = kinds[bi]
        if kind == 'q':
            for sub in range(2):
                h = h0 + sub
                blk = (QO + (g * 8 + h) * 128) // 128
                for half in range(2):
                    p = fm_mm(wb3, sub, half * 512, 512)
                    eb = ev_bf[rr('ev', 4)]
                    kb.op('act', lambda e, p=p, eb=eb, blk=blk: e.activation(out=eb[:, :], in_=p[:, :], func=AF.Identity, bias=binT[:, blk:blk + 1]), [p, binT], [eb])
                    kb.dma('pool', qT_d[g, h, :, half * 512:(half + 1) * 512], eb[:, :], [eb], [qT_d], prim=eb)
                p = fm_mm(wb3, sub, NT, NS)
                kb.op('act', lambda e, p=p, blk=blk: e.activation(out=sm_bf[:, :], in_=p[:, 0:NS], func=AF.Identity, bias=binT[:, blk:blk + 1]), [p, binT], [sm_bf])
                kb.dma('pool', qsT_d[g, h, :, :], sm_bf[:, :], [sm_bf], [qsT_d], prim=sm_bf)
        elif kind == 'k':
            W = GH[g]
            nout = min(W, NT)
            outs = [(NT - nout + t * 128, t * 128) for t in range(nout // 128)]
            for sub in range(2):
                h = h0 + sub
                KS = os.environ.get('KSUB', 'os')
                k_block(g, h, wb3, sub, [(0, 512, W), (512, 512, W + 512)], 0, outs if 'o' in KS else [])
                if 's' not in KS:
                    continue
                blk = (KO + (g * 8 + h) * 128) // 128
                p = fm_mm(wb3, sub, NT, NS)
                kb.op('dve', lambda e, p=p, blk=blk: e.tensor_scalar(out=sm_f[:, :], in0=p[:, 0:NS], scalar1=binT[:, blk:blk + 1], scalar2=None, op0=ALU.add), [p, binT], [sm_f])
                kb.op('act', lambda e: e.activation(out=sm_bf[:, :], in_=sm_f[:, :], func=AF.Copy), [sm_f], [sm_bf])
                kb.dma('pool', ksT_d[g, h, :, :], sm_bf[:, :], [sm_bf], [ksT_d], prim=sm_bf)
                pt = PS[4 + rr('ps', 4)]
                kb.op('pe', lambda e, pt=pt: e.transpose(pt[0:NS, 0:128], sm_f[:, :], ident[:]), [sm_f, ident], [pt])
                kb.op('act', lambda e, pt=pt: e.activation(out=sm_t[:, :], in_=pt[0:NS, 0:128], func=AF.Copy), [pt], [sm_t])
                kb.dma('pool', kvs_o[g, :, 0, h, :], sm_t[:, :], [sm_t], [kvs_o], prim=sm_t)
        elif kind == 'v':
            W = GH[g]
            nout = min(W, NT)
            outs = [(NT - nout + t * 128, t * 128) for t in range(nout // 128)]
            v_block(g, h0, wb3, [(t * 128, W + t * 128) for t in range(8)], outs)
            c0 = VO + (g * 8 + h0) * 128
            b = vb[rr('vb', 2)]
            kb.dma('pool', b[:], b_in_row[:, c0:c0 + 256].partition_broadcast(128), [b_in_row], [b], prim=b)
            p = PS[rr('ps', 4)]
            for k in range(16):
                wp = wpart(wb3, k)
                kb.op('pe', lambda e, k=k, wp=wp, p=p: e.matmul(p[0:NS, 0:256], hT[:, k, NT:NTS], wp[:, k, :], start=(k == 0), stop=(k == 15)), [wp.t, hT], [p])
            kb.op('dve', lambda e, p=p, b=b: e.tensor_tensor(out=vs_sb[:, :], in0=p[0:NS, 0:256], in1=b[0:NS, :], op=ALU.add), [p, b], [vs_sb])
            kb.op('act', lambda e: e.activation(out=vs_bf[:, :], in_=vs_sb[:, :], func=AF.Copy), [vs_sb], [vs_bf])
            kb.dma('pool', Vs_d[g, :, h0:h0 + 2, :], vs_bf[:, :].rearrange("p (h d) -> p h d", h=2), [vs_bf], [Vs_d], prim=vs_bf)
            kb.dma('pool', kvs_o[g, :, 1, h0:h0 + 2, :], vs_sb[:, :].rearrange("p (h d) -> p h d", h=2), [vs_sb], [kvs_o], prim=vs_sb)
        elif kind == 'u':
            u_block(g, wb3, [(0, 512, u_d, 128), (512, 512, u_d, 128 + 512), (NT, NS, us_d, 0)])
        else:
            for sub in range(2):
                jb = g + sub
                blk = GAO // 128 + jb
                for (c0, ntok) in ((0, 512), (512, 512), (NT, NS)):
                    p = fm_mm(wb3, sub, c0, ntok)
                    ef = ev_f[rr('ev', 4)]
                    kb.op('act', lambda e, p=p, ef=ef, blk=blk, ntok=ntok: e.activation(out=ef[:, 0:ntok], in_=p[:, 0:ntok], func=AF.Sigmoid, bias=binT[:, blk:blk + 1]), [p, binT], [ef])
                    kb.dma('pool', sg_d[jb * 128:(jb + 1) * 128, c0:c0 + ntok], ef[:, 0:ntok], [ef], [sg_d], prim=ef)
    KK = os.environ.get('KKINDS', 'qkvug')
    sel = [i for i in range(len(blocks)) if kinds[i][0] in KK]
    blocks = [blocks[i] for i in sel]; kinds = [kinds[i] for i in sel]
    if DBG in ('', 'C'):
        stream(blocks, bodyC)
    kb.barrier()
    ph.close()


    mid = ExitStack()
    attn_oT = kb.sb("attn_oT", [128, 8, NTS], BF16, mid)
    SCALE = 128.0 ** -0.5
    if DBG in ('', 'C', 'ATT'):
        ph = ExitStack()
        M1 = kb.sb("M1", [128, 256], F32, ph); kb.dma('sp', M1[:], M1_d[:], [], [M1], prim=M1)
        kval = kb.sb("kval", [128, 53], F32, ph); kb.dma('sp', kval[:], kval_d[:], [], [kval], prim=kval)
        SMc = kb.sb("SMc", [128, 21, NS], F32, ph); kb.dma('sp', SMc[:], SMc_d[:], [], [SMc], prim=SMc)
        SMn = kb.sb("SMn", [NS, 3, NS], F32, ph); kb.dma('sp', SMn[:], SMn_d[:], [], [SMn], prim=SMn)
        onesb = kb.sb("onesb", [128, 128], BF16, ph)
        kb.op('dve', lambda e: e.memset(onesb[:], 1.0), [], [onesb])
        qt = [kb.sb("qt%d" % i, [128, 3, NT], BF16, ph) for i in range(2)]
        kt = [[kb.sb("kt%d_%d" % (i, g), [128, NCTX[g]], BF16, ph) for g in range(3)] for i in range(2)]
        vt1 = [kb.sb("vt1_%d" % i, [128, 9, 128], BF16, ph) for i in range(2)]
        vt2 = [kb.sb("vt2_%d" % i, [128, 4, 3, 128], BF16, ph) for i in range(2)]
        vt3 = [kb.sb("vt3_%d" % i, [128, 16, 2, 128], BF16, ph) for i in range(2)]
        Et = [kb.sb("Et%d" % i, [128, 256], F32, ph) for i in range(3)]
        Pt = [kb.sb("Pt%d" % i, [128, 256], BF16, ph) for i in range(3)]
        acc = kb.sb("acc", [128, NT], F32, ph); dacc = kb.sb("dacc", [128, NT], F32, ph)
        qs = [kb.sb("qs%d" % i, [128, 3, NS], BF16, ph) for i in range(2)]
        ksn = [kb.sb("ksn%d" % i, [128, 3, NS], BF16, ph) for i in range(2)]
        vsn = [kb.sb("vsn%d" % i, [NS, 3, 128], BF16, ph) for i in range(2)]
        ck = [kb.sb("ck%d" % i, [128, 128], F32, ph) for i in range(3)]
        cv = [kb.sb("cv%d" % i, [128, 128], F32, ph) for i in range(3)]
        ckT = [kb.sb("ckT%d" % i, [128, 128], BF16, ph) for i in range(3)]
        cvb = [kb.sb("cvb%d" % i, [128, 128], BF16, ph) for i in range(3)]
        Es = [kb.sb("Es%d" % i, [128, NS], F32, ph) for i in range(3)]
        Psm = [kb.sb("Psm%d" % i, [128, NS], BF16, ph) for i in range(3)]
        osn = kb.sb("osn", [128, NS], F32, ph); dsn = kb.sb("dsn", [128, NS], F32, ph)
        ac = {'s': 0, 'c': 0}

        def load_head(h, i):
            for g in range(3):
                kb.dma('sp', qt[i][:, g, :], qT_d[g, h, :, :], [qT_d], [qt[i]], prim=qt[i])
                kb.dma('sp', kt[i][g][:, :], kT_d[g][h, :, :], [kT_d[g]], [kt[i][g]], prim=kt[i][g])
            kb.dma('sp', vt1[i][:], V_d[0][:, h, :].rearrange("(j p) d -> p j d", p=128), [V_d[0]], [vt1[i]], prim=vt1[i])
            for r in range(4):
                kb.dma('sp', vt2[i][:, r, :, :], V_d[1][:, h, :].rearrange("(j p r) d -> p r j d", p=128, r=4)[:, r, :, :], [V_d[1]], [vt2[i]], prim=vt2[i])
            v3 = V_d[2][:, h, :].rearrange("(m r) d -> m r d", r=16)
            kb.dma('sp', vt3[i][:, :, 0, :], v3[0:128, :, :], [V_d[2]], [vt3[i]], prim=vt3[i])
            kb.dma('sp', vt3[i][0:64, :, 1, :], v3[128:192, :, :], [V_d[2]], [vt3[i]], prim=vt3[i])
            kb.dma('sp', qs[i][:], qsT_d[:, h, :, :].rearrange("g d q -> d g q"), [qsT_d], [qs[i]], prim=qs[i])
            kb.dma('sp', ksn[i][:], ksT_d[:, h, :, :].rearrange("g d q -> d g q"), [ksT_d], [ksn[i]], prim=ksn[i])
            kb.dma('sp', vsn[i][:], Vs_d[:, :, h, :].rearrange("g t d -> t g d"), [Vs_d], [vsn[i]], prim=vsn[i])

        def tile_attn(kT_ap, nk, q_ap, N, v_ap, kv_idx, m0, qstart, first):
            sp_ = PS[4 + ac['s'] % 3]; Ei = Et[ac['s'] % 3]; Pi = Pt[ac['s'] % 3]; ac['s'] += 1
            kb.op('pe', lambda e: e.matmul(sp_[0:nk, 0:N], kT_ap, q_ap, start=True, stop=True), kt_reads, [sp_])
            kb.op('act', lambda e: e.activation(out=Ei[0:nk, 0:N], in_=sp_[0:nk, 0:N], func=AF.Exp, scale=SCALE), [sp_], [Ei])
            kb.op('dve', lambda e: e.scalar_tensor_tensor(out=Pi[0:nk, 0:N], in0=Ei[0:nk, 0:N], scalar=kval[0:nk, kv_idx:kv_idx + 1], in1=M1[0:nk, m0:m0 + N], op0=ALU.mult, op1=ALU.mult), [Ei, kval, M1], [Pi])
            segs = []
            a, b = qstart, qstart + N
            if a < 512 and b > 512:
                segs = [(a, 512, 0), (512, b, 512 - a)]
            else:
                segs = [(a, b, 0)]
            for (qa, qb, po) in segs:
                bank = qa // 512
                st = first[bank]
                first[bank] = False
                n = qb - qa
                kb.op('pe', lambda e: e.matmul(PS[bank][:, qa - bank * 512:qb - bank * 512], v_ap, Pi[0:nk, po:po + n], start=st, stop=True, skip_group_check=True), [Pi] + v_reads, [PS[bank]])
                kb.op('pe', lambda e: e.matmul(PS[2 + bank][:, qa - bank * 512:qb - bank * 512], onesb[0:nk, :], Pi[0:nk, po:po + n], start=st, stop=True, skip_group_check=True), [Pi, onesb], [PS[2 + bank]])

        load_head(0, 0)
        for h in range(8):
            i = h % 2
            if h + 1 < 8:
                load_head(h + 1, (h + 1) % 2)
            kt_reads = [kt[i][0], kt[i][1], kt[i][2], qt[i]]
            v_reads = [vt1[i], vt2[i], vt3[i]]
            first = [True, True]
            for j in range(9):
                t0 = max(0, 128 * (j - 1)); t1 = min(NT, 128 * (j - 1) + 256)
                m0 = 128 if j == 0 else 0
                tile_attn(kt[i][0][:, 128 * j:128 * j + 128], 128, qt[i][:, 0, t0:t1], t1 - t0, vt1[i][:, j, :], j, m0, t0, first)
            for b in range(2):
                kb.op('act', lambda e, b=b: e.activation(out=acc[:, 512 * b:512 * b + 512], in_=PS[b][:, :], func=AF.Copy), [PS[b]], [acc])
                kb.op('dve', lambda e, b=b: e.tensor_copy(out=dacc[:, 512 * b:512 * b + 512], in_=PS[2 + b][:, :]), [PS[2 + b]], [dacc])
            first = [True, True]
            k2 = kt[i][1][:, :].rearrange("d (j p r) -> d j r p", p=128, r=4)
            q2 = qt[i][:, 1, :].rearrange("d (i r) -> d r i", r=4)
            for r in range(4):
                for j in range(3):
                    i0 = 0 if j < 2 else 128
                    N = 128 if j != 1 else 256
                    m0 = 128 if j == 0 else 0
                    tile_attn(k2[:, j, r, :], 128, q2[:, r, i0:i0 + N], N, vt2[i][:, r, j, :], 9 + r * 3 + j, m0, r * 256 + i0, first)
            for (A, Pb) in ((acc, 0), (dacc, 2)):
                Av = A[:, :].rearrange("p (i r) -> p r i", r=4)
                for b in range(2):
                    kb.op('dve', lambda e, b=b, Av=Av, Pb=Pb: e.tensor_tensor(out=Av[:, 2 * b:2 * b + 2, :], in0=Av[:, 2 * b:2 * b + 2, :], in1=PS[Pb + b][:, :].rearrange("p (r i) -> p r i", r=2), op=ALU.add), [A, PS[Pb + b]], [A])
            first = [True, True]
            k3 = kt[i][2][:, :].rearrange("d (m r) -> d r m", r=16)
            q3 = qt[i][:, 2, :].rearrange("d (i r) -> d r i", r=16)
            for r in range(16):
                tile_attn(k3[:, r, 0:128], 128, q3[:, r, :], 64, vt3[i][:, r, 0, :], 21 + 2 * r, 128, r * 64, first)
                tile_attn(k3[:, r, 128:192], 64, q3[:, r, :], 64, vt3[i][0:64, r, 1, :], 21 + 2 * r + 1, 0, r * 64, first)
            for (A, Pb) in ((acc, 0), (dacc, 2)):
                Av = A[:, :].rearrange("p (i r) -> p r i", r=16)
                for b in range(2):
                    kb.op('dve', lambda e, b=b, Av=Av, Pb=Pb: e.tensor_tensor(out=Av[:, 8 * b:8 * b + 8, :], in0=Av[:, 8 * b:8 * b + 8, :], in1=PS[Pb + b][:, :].rearrange("p (r i) -> p r i", r=8), op=ALU.add), [A, PS[Pb + b]], [A])
            kb.op('dve', lambda e: e.reciprocal(out=dacc[:, :], in_=dacc[:, :]), [dacc], [dacc])
            kb.op('dve', lambda e: e.tensor_tensor(out=attn_oT[:, h, 0:NT], in0=acc[:, :], in1=dacc[:, :], op=ALU.mult), [acc, dacc], [attn_oT])
            po, pd = PS[7], PS[7]
            firsts = [True]
            tiles = []
            for g, W in enumerate(GH):
                for j in range(W // 128):
                    tiles.append((g, j))
            ti = 0
            for (g, j) in tiles:
                c = ac['c'] % 3; ac['c'] += 1
                kb.dma('sp', ck[c][:], cache_d[g][128 * j:128 * j + 128, 0, h, :], [], [ck[c]], prim=ck[c])
                kb.dma('sp', cv[c][:], cache_d[g][128 * j:128 * j + 128, 1, h, :], [], [cv[c]], prim=cv[c])
                pt = PS[4 + ac['s'] % 3]; ac['s'] += 1
                kb.op('pe', lambda e, c=c, pt=pt: e.transpose(pt[:, 0:128], ck[c][:], ident[:]), [ck[c], ident], [pt])
                kb.op('act', lambda e, c=c, pt=pt: e.activation(out=ckT[c][:], in_=pt[:, 0:128], func=AF.Copy), [pt], [ckT[c]])
                kb.op('dve', lambda e, c=c: e.tensor_copy(out=cvb[c][:], in_=cv[c][:]), [cv[c]], [cvb[c]])
                sp_ = PS[4 + ac['s'] % 3]; Ei = Es[ac['s'] % 3]; Pi = Psm[ac['s'] % 3]; ac['s'] += 1
                kb.op('pe', lambda e, c=c, sp_=sp_, g=g: e.matmul(sp_[:, 0:NS], ckT[c][:], qs[i][:, g, :], start=True, stop=True), [ckT[c], qs[i]], [sp_])
                kb.op('act', lambda e, sp_=sp_, Ei=Ei: e.activation(out=Ei[:, :], in_=sp_[:, 0:NS], func=AF.Exp, scale=SCALE), [sp_], [Ei])
                kb.op('dve', lambda e, Ei=Ei, Pi=Pi, ti=ti: e.tensor_tensor(out=Pi[:, :], in0=Ei[:, :], in1=SMc[:, ti, :], op=ALU.mult), [Ei, SMc], [Pi])
                st = firsts[0]; firsts[0] = False
                kb.op('pe', lambda e, c=c, Pi=Pi, st=st: e.matmul(PS[7][:, 0:NS], cvb[c][:], Pi[:, :], start=st, stop=True, skip_group_check=True), [cvb[c], Pi], [PS[7]])
                kb.op('pe', lambda e, Pi=Pi, st=st: e.matmul(PS[7][:, 64:64 + NS], onesb[:, :], Pi[:, :], start=False, stop=True, skip_group_check=True), [onesb, Pi], [PS[7]])
                ti += 1
            for g in range(3):
                sp_ = PS[4 + ac['s'] % 3]; Ei = Es[ac['s'] % 3]; Pi = Psm[ac['s'] % 3]; ac['s'] += 1
                kb.op('pe', lambda e, sp_=sp_, g=g: e.matmul(sp_[0:NS, 0:NS], ksn[i][:, g, :], qs[i][:, g, :], start=True, stop=True), [ksn[i], qs[i]], [sp_])
                kb.op('act', lambda e, sp_=sp_, Ei=Ei: e.activation(out=Ei[0:NS, :], in_=sp_[0:NS, 0:NS], func=AF.Exp, scale=SCALE), [sp_], [Ei])
                kb.op('dve', lambda e, Ei=Ei, Pi=Pi, g=g: e.tensor_tensor(out=Pi[0:NS, :], in0=Ei[0:NS, :], in1=SMn[:, g, :], op=ALU.mult), [Ei, SMn], [Pi])
                kb.op('pe', lambda e, Pi=Pi, g=g: e.matmul(PS[7][:, 0:NS], vsn[i][:, g, :], Pi[0:NS, :], start=False, stop=True, skip_group_check=True), [vsn[i], Pi], [PS[7]])
                kb.op('pe', lambda e, Pi=Pi: e.matmul(PS[7][:, 64:64 + NS], onesb[0:NS, :], Pi[0:NS, :], start=False, stop=True, skip_group_check=True), [onesb, Pi], [PS[7]])
            kb.op('dve', lambda e: e.reciprocal(out=dsn[:, :], in_=PS[7][:, 64:64 + NS]), [PS[7]], [dsn])
            kb.op('dve', lambda e: e.tensor_tensor(out=attn_oT[:, h, NT:NTS], in0=PS[7][:, 0:NS], in1=dsn[:, :], op=ALU.mult), [PS[7], dsn], [attn_oT])
        if os.environ.get('KDUMPA'):
            adbg = kb.dram("attn_dbg", [128, 8, NTS], BF16, "ExternalOutput")
            kb.dma('pool', adbg[:], attn_oT[:], [attn_oT], [adbg], prim=attn_oT)
        kb.barrier()
        ph.close()


    conv_fT = kb.sb("conv_fT", [128, 12, NTS], BF16, mid)
    x1_d = kb.dram("x1_d", [NT + 128, D])
    if DBG in ('', 'CONV'):
        ph = ExitStack()
        wdw = kb.sb("wdw", [128, 12, 31], F32, ph); kb.dma('sp', wdw[:], w_dwT[:], [], [wdw], prim=wdw)
        cpar = kb.sb("cpar", [128, 3, 12], F32, ph); kb.dma('sp', cpar[:], cparT[:], [], [cpar], prim=cpar)
        hprev = kb.sb("hprev", [128, 1], F32, ph); kb.dma('sp', hprev[:], hprev_d[:], [], [hprev], prim=hprev)
        onesf = kb.sb("onesf", [128, 128], F32, ph); kb.op('dve', lambda e: e.memset(onesf[:], 1.0), [], [onesf])
        yc = kb.sb("yc", [128, 12, NTS], F32, ph)
        ub = [kb.sb("ub%d" % i, [128, 30 + NT], F32, ph) for i in range(2)]
        ubs = [kb.sb("ubs%d" % i, [128, 30 + NS], F32, ph) for i in range(2)]
        cpo = kb.sb("cpo", [32, 1536], F32, ph); cso = kb.sb("cso", [NS, 1536], F32, ph)
        sq = [kb.sb("sq%d" % i, [128, 512], F32, ph) for i in range(2)]
        mu = kb.sb("mu", [128, NTS], F32, ph); rsd = kb.sb("rsd", [128, NTS], F32, ph); tmpc = [kb.sb("tmpc%d" % i, [128, 512], F32, ph) for i in range(2)]
        kb.dma('pool', convs_o[0:22, :], st_d[8:30, :], [st_d], [convs_o], prim=hprev)
        for j in range(12):
            u, us_ = ub[j % 2], ubs[j % 2]
            kb.dma('sp', u[:, :], u_d[j * 128:(j + 1) * 128, 98:128 + NT], [u_d], [u], prim=u)
            kb.dma('sp', us_[:, 0:30], stT_d[j * 128:(j + 1) * 128, :], [stT_d], [us_], prim=us_)
            kb.dma('sp', us_[:, 30:30 + NS], us_d[j * 128:(j + 1) * 128, :], [us_d], [us_], prim=us_)
            kb.op('dve', lambda e, u=u: e.tensor_scalar(out=u[:, 0:30], in0=u[:, 0:30], scalar1=hprev[:, 0:1], scalar2=None, op0=ALU.mult), [u, hprev], [u])
            for (src, L, c0) in ((u, NT, 0), (us_, NS, NT)):
                kb.op('dve', lambda e, src=src, L=L, c0=c0: e.tensor_scalar(out=yc[:, j, c0:c0 + L], in0=src[:, 0:L], scalar1=wdw[:, j, 0:1], scalar2=cpar[:, 0, j:j + 1], op0=ALU.mult, op1=ALU.add), [src, wdw, cpar], [yc])
                for k in range(1, 31):
                    kb.op('dve', lambda e, src=src, L=L, c0=c0, k=k: e.scalar_tensor_tensor(out=yc[:, j, c0:c0 + L], in0=src[:, k:k + L], scalar=wdw[:, j, k:k + 1], in1=yc[:, j, c0:c0 + L], op0=ALU.mult, op1=ALU.add), [src, wdw], [yc])
            pt = PS[4 + j % 2]
            kb.op('pe', lambda e, pt=pt, u=u: e.transpose(pt[0:32, 0:128], u[:, 30 + NT - 32:30 + NT], ident[:]), [u, ident], [pt])
            kb.op('act', lambda e, pt=pt: e.activation(out=cpo[:, j * 128:(j + 1) * 128], in_=pt[0:32, 0:128], func=AF.Copy), [pt], [cpo])
            pt2 = PS[6 + j % 2]
            kb.op('pe', lambda e, pt2=pt2, us_=us_: e.transpose(pt2[0:NS, 0:128], us_[:, 30:30 + NS], ident[:]), [us_, ident], [pt2])
            kb.op('act', lambda e, pt2=pt2: e.activation(out=cso[:, j * 128:(j + 1) * 128], in_=pt2[0:NS, 0:128], func=AF.Copy), [pt2], [cso])
        kb.dma('pool', convp_o[:, :], cpo[2:32, :], [cpo], [convp_o], prim=cpo)
        kb.dma('pool', convs_o[22:30, :], cso[:, :], [cso], [convs_o], prim=cso)
        for (c0, n) in ((0, 512), (512, 512), (NT, NS)):
            p1, p2 = PS[0], PS[1]
            for j in range(12):
                s_ = sq[j % 2]
                kb.op('act', lambda e, s_=s_: e.activation(out=s_[:, 0:n], in_=yc[:, j, c0:c0 + n], func=AF.Square), [yc], [s_])
                kb.op('pe', lambda e: e.matmul(p1[:, 0:n], onesf[:, :], yc[:, j, c0:c0 + n], start=(j == 0), stop=(j == 11)), [onesf, yc], [p1])
                kb.op('pe', lambda e, s_=s_: e.matmul(p2[:, 0:n], onesf[:, :], s_[:, 0:n], start=(j == 0), stop=(j == 11)), [onesf, s_], [p2])
            t_ = tmpc[0]
            kb.op('dve', lambda e: e.tensor_scalar(out=mu[:, c0:c0 + n], in0=p1[:, 0:n], scalar1=1.0 / 1536, scalar2=None, op0=ALU.mult), [p1], [mu])
            kb.op('dve', lambda e: e.tensor_tensor(out=t_[:, 0:n], in0=mu[:, c0:c0 + n], in1=mu[:, c0:c0 + n], op=ALU.mult), [mu], [t_])
            kb.op('dve', lambda e: e.scalar_tensor_tensor(out=t_[:, 0:n], in0=p2[:, 0:n], scalar=1.0 / 1536, in1=t_[:, 0:n], op0=ALU.mult, op1=ALU.subtract), [p2, t_], [t_])
            kb.op('dve', lambda e: e.tensor_scalar(out=t_[:, 0:n], in0=t_[:, 0:n], scalar1=1e-6, scalar2=None, op0=ALU.add), [t_], [t_])
            kb.op('act', lambda e: e.activation(out=t_[:, 0:n], in_=t_[:, 0:n], func=AF.Sqrt), [t_], [t_])
            kb.op('dve', lambda e: e.reciprocal(out=rsd[:, c0:c0 + n], in_=t_[:, 0:n]), [t_], [rsd])
            for j in range(12):
                t2 = tmpc[1]
                kb.op('dve', lambda e, t2=t2: e.tensor_tensor(out=t2[:, 0:n], in0=yc[:, j, c0:c0 + n], in1=mu[:, c0:c0 + n], op=ALU.subtract), [yc, mu], [t2])
                kb.op('pool', lambda e, t2=t2: e.tensor_tensor(out=t2[:, 0:n], in0=t2[:, 0:n], in1=rsd[:, c0:c0 + n], op=ALU.mult), [t2, rsd], [t2])
                kb.op('dve', lambda e, t2=t2: e.tensor_scalar(out=t2[:, 0:n], in0=t2[:, 0:n], scalar1=cpar[:, 1, j:j + 1], scalar2=cpar[:, 2, j:j + 1], op0=ALU.mult, op1=ALU.add), [t2, cpar], [t2])
                kb.op('act', lambda e, t2=t2: e.activation(out=conv_fT[:, j, c0:c0 + n], in_=t2[:, 0:n], func=AF.Silu), [t2], [conv_fT])
        kb.barrier()
        ph.close()

    if DBG in ('', 'CONV'):
        ph = ExitStack()
        sT = kb.sb("sT", [128, 16, NTS], BF16, ph)
        bo = kb.sb("bo", [128, 2, 16], F32, ph); kb.dma('sp', bo[:], boT[:], [], [bo], prim=bo)
        stg = [kb.sb("stg%d" % i, [128, 16, 256], F32, ph) for i in range(2)]
        wbs = [[kb.sb("wb%d_%d" % (i, j), [128, KSPL[j][1] - KSPL[j][0], 256], BF16, ph) for j in range(3)] for i in range(2)]
        sga = [kb.sb("sga%d" % i, [128, 512], F32, ph) for i in range(2)]; sgb = [kb.sb("sgb%d" % i, [128, 512], F32, ph) for i in range(2)]
        t1 = [kb.sb("t1_%d" % i, [128, 512], F32, ph) for i in range(2)]; t2_ = [kb.sb("t2_%d" % i, [128, 512], F32, ph) for i in range(2)]
        mc = {'i': 0}
        seq = []
        for cb in range(8):
            seq.append(('a', cb)); seq.append(('c', cb))

        def issue(idx):
            kind, cb = seq[idx]
            st = stg[idx % 2]
            if kind == 'a':
                kb.dma('sp', st[:, 0:8, :], w_ao[:, cb * 256:(cb + 1) * 256].rearrange("(k p) n -> p k n", p=128), [], [st], prim=st)
            else:
                kb.dma('sp', st[:, 0:12, :], w_co[:, cb * 256:(cb + 1) * 256].rearrange("(k p) n -> p k n", p=128), [], [st], prim=st)
        issue(0)
        pa_t = {}
        for idx in range(len(seq)):
            if idx + 1 < len(seq):
                issue(idx + 1)
            kind, cb = seq[idx]
            st, wb3 = stg[idx % 2], wbs[idx % 2]
            nk = 8 if kind == 'a' else 12
            cast_block(st, wb3, nk=nk)
            src = attn_oT if kind == 'a' else conv_fT
            for sub in range(2):
                jb = cb * 2 + sub
                for ci, (c0, n) in enumerate(((0, 512), (512, 512), (NT, NS))):
                    if kind == 'a':
                        p = PS[(sub * 3 + ci) % 6]
                    else:
                        p = PS[6 + (sub * 3 + ci) % 2]
                    for k in range(nk):
                        wp = wpart(wb3, k)
                        kb.op('pe', lambda e, k=k, wp=wp, p=p: e.matmul(p[:, 0:n], wp[:, k, sub * 128:(sub + 1) * 128], src[:, k, c0:c0 + n], start=(k == 0), stop=(k == nk - 1)), [wp.t, src], [p])
                    if kind == 'a':
                        pa_t[(sub, ci)] = p
                    else:
                        m = mc['i'] % 2; mc['i'] += 1
                        pa = pa_t[(sub, ci)]
                        kb.dma('sp', sga[m][:, 0:n], sg_d[jb * 128:(jb + 1) * 128, c0:c0 + n], [sg_d], [sga[m]], prim=sga[m])
                        kb.dma('sp', sgb[m][:, 0:n], sg_d[2048 + jb * 128:2048 + (jb + 1) * 128, c0:c0 + n], [sg_d], [sgb[m]], prim=sgb[m])
                        kb.op('dve', lambda e, pa=pa, m=m: e.scalar_tensor_tensor(out=t1[m][:, 0:n], in0=pa[:, 0:n], scalar=bo[:, 0, jb:jb + 1], in1=sga[m][:, 0:n], op0=ALU.add, op1=ALU.mult), [pa, bo, sga[m]], [t1[m]])
                        kb.op('dve', lambda e, p=p, m=m: e.scalar_tensor_tensor(out=t2_[m][:, 0:n], in0=p[:, 0:n], scalar=bo[:, 1, jb:jb + 1], in1=sgb[m][:, 0:n], op0=ALU.add, op1=ALU.mult), [p, bo, sgb[m]], [t2_[m]])
                        kb.op('pool', lambda e, m=m: e.tensor_tensor(out=sT[:, jb, c0:c0 + n], in0=t1[m][:, 0:n], in1=t2_[m][:, 0:n], op=ALU.add), [t1[m], t2_[m]], [sT])
        g1bc = kb.sb("g1bc", [128, D], F32, ph); g1bs = kb.sb("g1bs", [NS, D], F32, ph)
        kb.dma('pool', g1bc[:], mod_d[0:1, 2 * D:3 * D].partition_broadcast(128), [mod_d], [g1bc], prim=g1bc)
        kb.dma('pool', g1bs[:], mod_d[1:2, 2 * D:3 * D].partition_broadcast(NS), [mod_d], [g1bs], prim=g1bs)
        xp_ = [kb.sb("xp%d" % i, [128, 256], F32, ph) for i in range(3)]
        xo_ = [kb.sb("xo%d" % i, [128, 256], F32, ph) for i in range(3)]
        wo_i = {'i': 0}

        def issue_o(cb):
            st = stg[cb % 2]
            kb.dma('sp', st[:, :, :], w_o[:, cb * 256:(cb + 1) * 256].rearrange("(k p) n -> p k n", p=128), [], [st], prim=st)
        issue_o(0)
        for cb in range(8):
            if cb + 1 < 8:
                issue_o(cb + 1)
            st, wb3 = stg[cb % 2], wbs[cb % 2]
            cast_block(st, wb3)
            for t in range(9):
                rows = 128 if t < 8 else NS
                tok0 = t * 128
                p = PS[t % 6]
                for k in range(16):
                    wp = wpart(wb3, k)
                    kb.op('pe', lambda e, k=k, wp=wp, p=p: e.matmul(p[0:rows, 0:256], sT[:, k, tok0:tok0 + rows], wp[:, k, :], start=(k == 0), stop=(k == 15)), [wp.t, sT], [p])
                m = wo_i['i'] % 3; wo_i['i'] += 1
                xsrc = xh[HALO + tok0:HALO + tok0 + rows, cb * 256:(cb + 1) * 256] if t < 8 else xs[:, cb * 256:(cb + 1) * 256]
                gb = g1bc if t < 8 else g1bs
                kb.dma('sp', xp_[m][0:rows, :], xsrc, [], [xp_[m]], prim=xp_[m])
                kb.op('dve', lambda e, p=p, m=m, gb=gb: e.tensor_tensor(out=xo_[m][0:rows, :], in0=p[0:rows, 0:256], in1=gb[0:rows, cb * 256:(cb + 1) * 256], op=ALU.mult), [p, gb], [xo_[m]])
                kb.op('pool', lambda e, m=m: e.tensor_tensor(out=xo_[m][0:rows, :], in0=xo_[m][0:rows, :], in1=xp_[m][0:rows, :], op=ALU.add), [xo_[m], xp_[m]], [xo_[m]])
                kb.dma('pool', x1_d[tok0:tok0 + rows, cb * 256:(cb + 1) * 256], xo_[m][0:rows, :], [xo_[m]], [x1_d], prim=xo_[m])
        kb.barrier()
        ph.close()


    mid.close()
    AX = mybir.AxisListType.X
    NE = int(os.environ.get('KNE', '32'))
    late = ExitStack()
    h2T = kb.sb("h2T", [128, 16, NTS], BF16, late)
    gateM = kb.sb("gateM", [128, 9, 32], F32, late)
    kb.op('dve', lambda e: e.memset(gateM[:], 0.0), [], [gateM])
    ph = ExitStack()
    A2 = [kb.sb("A2_%d" % r, [128 if r == 0 else NS, D], F32, ph) for r in range(2)]
    B2 = [kb.sb("B2_%d" % r, [128 if r == 0 else NS, D], F32, ph) for r in range(2)]
    g2bc = kb.sb("g2bc", [128, D], F32, ph)
    kb.dma('pool', g2bc[:], g2_row[:, :].partition_broadcast(128), [], [g2bc], prim=g2bc)
    for r in range(2):
        n = 128 if r == 0 else NS
        kb.dma('pool', A2[r][:], mod_d[r:r + 1, 4 * D:5 * D].partition_broadcast(n), [mod_d], [A2[r]], prim=A2[r])
        kb.dma('pool', B2[r][:], mod_d[r:r + 1, 3 * D:4 * D].partition_broadcast(n), [mod_d], [B2[r]], prim=B2[r])
        kb.op('dve', lambda e, r=r, n=n: e.scalar_tensor_tensor(out=A2[r][:], in0=A2[r][:], scalar=1.0, in1=g2bc[0:n, :], op0=ALU.add, op1=ALU.mult), [A2[r], g2bc], [A2[r]])
    wr = kb.sb("wr", [128, 16, 32], F32, ph); kb.dma('sp', wr[:], w_router[:, :].rearrange("(k p) e -> p k e", p=128), [], [wr], prim=wr)
    brbc = kb.sb("brbc", [128, 32], F32, ph); kb.dma('pool', brbc[:], b_router[:, :].partition_broadcast(128), [], [brbc], prim=brbc)
    x1t = [kb.sb("x1t%d" % i, [128, D], F32, ph) for i in range(2)]
    hn = [kb.sb("hn%d" % i, [128, D], F32, ph) for i in range(2)]
    h2f = [kb.sb("h2f%d" % i, [128, 16, 128], F32, ph) for i in range(2)]
    junk2 = kb.sb("junk2", [128, D], BF16, ph)
    sm = {n: kb.sb("sm_" + n, [128, w], F32, ph) for n, w in (("ss", 1), ("rs", 1), ("lg", 32), ("mx", 8), ("mk", 32), ("nm", 1), ("ex", 32), ("su", 1))}
    for t in range(9):
        rows = 128 if t < 8 else NS
        tok0 = t * 128
        r = 0 if t < 8 else 1
        x, h_, hf = x1t[t % 2], hn[t % 2], h2f[t % 2]
        ss, rs = sm["ss"], sm["rs"]
        kb.dma('sp', x[0:rows, :], x1_d[tok0:tok0 + rows, :], [x1_d], [x], prim=x)
        kb.op('act', lambda e: e.activation(out=junk2[0:rows, :], in_=x[0:rows, :], func=AF.Square, accum_out=ss[0:rows, :]), [x], [junk2, ss])
        kb.op('dve', lambda e: e.tensor_scalar(out=ss[0:rows, :], in0=ss[0:rows, :], scalar1=1.0 / D, scalar2=1e-6, op0=ALU.mult, op1=ALU.add), [ss], [ss])
        kb.op('act', lambda e: e.activation(out=ss[0:rows, :], in_=ss[0:rows, :], func=AF.Sqrt), [ss], [ss])
        kb.op('dve', lambda e: e.reciprocal(out=rs[0:rows, :], in_=ss[0:rows, :]), [ss], [rs])
        kb.op('act', lambda e: e.activation(out=h_[0:rows, :], in_=x[0:rows, :], func=AF.Copy, scale=rs[0:rows, :]), [x, rs], [h_])
        kb.op('dve', lambda e: e.tensor_tensor(out=h_[0:rows, :], in0=h_[0:rows, :], in1=A2[r][0:rows, :], op=ALU.mult), [h_, A2[r]], [h_])
        kb.op('pool', lambda e: e.tensor_tensor(out=h_[0:rows, :], in0=h_[0:rows, :], in1=B2[r][0:rows, :], op=ALU.add), [h_, B2[r]], [h_])
        for kq in range(4):
            p = PS[kq]
            for kk in range(4):
                k = kq * 4 + kk
                kb.op('pe', lambda e, k=k, kk=kk, p=p: e.transpose(p[:, kk * 128:kk * 128 + rows], h_[0:rows, k * 128:(k + 1) * 128], ident[0:rows, 0:rows]), [h_, ident], [p])
            pv = p[:, :].rearrange("p (k t) -> p k t", k=4)
            kb.op('act', lambda e, pv=pv, kq=kq: e.activation(out=h2T[:, 4 * kq:4 * kq + 4, tok0:tok0 + rows], in_=pv[:, :, 0:rows], func=AF.Copy), [p], [h2T])
            kb.op('dve', lambda e, pv=pv, kq=kq: e.tensor_copy(out=hf[:, 4 * kq:4 * kq + 4, 0:rows], in_=pv[:, :, 0:rows]), [p], [hf])
        pr = PS[4 + t % 2]
        for k in range(16):
            kb.op('pe', lambda e, k=k: e.matmul(pr[0:rows, 0:32], hf[:, k, 0:rows], wr[:, k, :], start=(k == 0), stop=(k == 15)), [hf, wr], [pr])
        lg, mx, mk, nm, ex, su = sm["lg"], sm["mx"], sm["mk"], sm["nm"], sm["ex"], sm["su"]
        kb.op('dve', lambda e: e.tensor_tensor(out=lg[0:rows, :], in0=pr[0:rows, 0:32], in1=brbc[0:rows, :], op=ALU.add), [pr, brbc], [lg])
        kb.op('dve', lambda e: e.max(out=mx[0:rows, :], in_=lg[0:rows, :]), [lg], [mx])
        kb.op('dve', lambda e: e.tensor_scalar(out=mk[0:rows, :], in0=lg[0:rows, :], scalar1=mx[0:rows, 3:4], scalar2=None, op0=ALU.is_ge), [lg, mx], [mk])
        kb.op('dve', lambda e: e.tensor_scalar(out=nm[0:rows, :], in0=mx[0:rows, 0:1], scalar1=-1.0, scalar2=None, op0=ALU.mult), [mx], [nm])
        kb.op('act', lambda e: e.activation(out=ex[0:rows, :], in_=lg[0:rows, :], func=AF.Exp, bias=nm[0:rows, :]), [lg, nm], [ex])
        kb.op('dve', lambda e: e.tensor_tensor(out=ex[0:rows, :], in0=ex[0:rows, :], in1=mk[0:rows, :], op=ALU.mult), [ex, mk], [ex])
        kb.op('dve', lambda e: e.reduce_sum(out=su[0:rows, :], in_=ex[0:rows, :], axis=AX), [ex], [su])
        kb.op('dve', lambda e: e.reciprocal(out=su[0:rows, :], in_=su[0:rows, :]), [su], [su])
        kb.op('dve', lambda e: e.tensor_scalar(out=gateM[0:rows, t, :], in0=ex[0:rows, :], scalar1=su[0:rows, 0:1], scalar2=None, op0=ALU.mult), [ex, su], [gateM])
    kb.barrier()
    ph.close()

    late2 = ExitStack()
    accM = kb.sb("accM", [128, 9, D], F32, late2)
    ph = ExitStack()
    pi = ExitStack()
    b2 = kb.sb("b2", [32, D], F32, pi); kb.dma('sp', b2[:], b_moe2[:, :], [], [b2], prim=b2)
    gT = kb.sb("gT", [32, 128], F32, pi)
    for t in range(9):
        pt = PS[6]
        kb.op('pe', lambda e, t=t, pt=pt: e.transpose(pt[0:32, 0:128], gateM[:, t, :], ident[:]), [gateM, ident], [pt])
        kb.op('act', lambda e, pt=pt: e.activation(out=gT[:, :], in_=pt[0:32, 0:128], func=AF.Copy), [pt], [gT])
        for c4 in range(4):
            p = PS[c4]
            kb.op('pe', lambda e, p=p, c4=c4: e.matmul(p[:, :], gT[:, :], b2[:, c4 * 512:(c4 + 1) * 512], start=True, stop=True), [gT, b2], [p])
            kb.op('act', lambda e, p=p, c4=c4, t=t: e.activation(out=accM[:, t, c4 * 512:(c4 + 1) * 512], in_=p[:, :], func=AF.Copy), [p], [accM])
    kb.barrier()
    pi.close()
    actT = kb.sb("actT", [128, 8, NTS], BF16, ph)
    b1 = kb.sb("b1", [128, 32, 32], F32, ph); kb.dma('sp', b1[:], b1T_d[:], [], [b1], prim=b1)
    USPL = [(0, 3, 'act'), (3, 6, 'dve'), (6, 8, 'pool')]
    stu = [kb.sb("stu%d" % i, [128, 8, 512], F32, ph) for i in range(2)]
    wbu = [[kb.sb("wbu%d_%d" % (i, j), [128, USPL[j][1] - USPL[j][0], 512], BF16, ph) for j in range(3)] for i in range(4)]
    gsT = kb.sb("gsT", [128, 4, NTS], BF16, ph)
    gt = kb.sb("gt", [128, 512], F32, ph); st_ = kb.sb("st_", [128, 512], F32, ph); lt = kb.sb("lt", [128, 512], F32, ph)
    CH = ((0, 512), (512, 512), (NT, NS))
    units = []
    for ex_ in range(NE):
        for hf in range(2):
            for jq in range(2):
                c = hf * 1024 + jq * 512
                for kh in range(2):
                    units.append(w_moe1[ex_, kh * 1024:(kh + 1) * 1024, c:c + 512])
                for kh in range(2):
                    units.append(w_moe1[ex_, kh * 1024:(kh + 1) * 1024, 2048 + c:2048 + c + 512])
            for cb in range(4):
                units.append(w_moe2[ex_, hf * 1024:(hf + 1) * 1024, cb * 512:(cb + 1) * 512])
    us = {'issued': 0, 'cast': 0, 'p': 0}

    def u_issue():
        i = us['issued']
        if i < len(units):
            kb.dma('sp', stu[i % 2][:, :, :], units[i].rearrange("(k p) n -> p k n", p=128), [], [stu[i % 2]], prim=stu[i % 2])
            us['issued'] += 1

    def u_next():
        i = us['cast']; us['cast'] += 1
        while us['issued'] < min(i + 2, len(units)):
            u_issue()
        st, w3 = stu[i % 2], wbu[i % 4]
        for j, (k0, k1, en) in enumerate(USPL):
            if en == 'act':
                kb.op('act', lambda e, j=j, k0=k0, k1=k1: e.activation(out=w3[j][:, :, :], in_=st[:, k0:k1, :], func=AF.Copy), [st], [w3[j]])
            else:
                kb.op(en, lambda e, j=j, k0=k0, k1=k1: e.tensor_copy(out=w3[j][:, :, :], in_=st[:, k0:k1, :]), [st], [w3[j]])
        return w3

    def upart(w3, k):
        for j, (k0, k1, en) in enumerate(USPL):
            if k0 <= k < k1:
                return w3[j], k - k0

    def nps():
        us['p'] += 1
        return PS[us['p'] % 6]

    def w1_mm(U0, U1, sub, c0, n):
        p = nps()
        for k in range(16):
            tl, kl = upart(U0 if k < 8 else U1, k % 8)
            kb.op('pe', lambda e, k=k, tl=tl, kl=kl: e.matmul(p[:, 0:n], tl[:, kl, sub * 128:(sub + 1) * 128], h2T[:, k, c0:c0 + n], start=(k == 0), stop=(k == 15)), [tl, h2T], [p])
        return p

    u_issue()
    for ex_ in range(NE):
        for hf in range(2):
            for jq in range(2):
                U0 = u_next(); U1 = u_next()
                for sub in range(4):
                    jb = hf * 8 + jq * 4 + sub
                    for (c0, n) in CH:
                        p = w1_mm(U0, U1, sub, c0, n)
                        kb.op('dve', lambda e, p=p: e.tensor_scalar(out=gt[:, 0:n], in0=p[:, 0:n], scalar1=b1[:, ex_, jb:jb + 1], scalar2=7.0, op0=ALU.add, op1=ALU.min), [p, b1], [gt])
                        kb.op('act', lambda e: e.activation(out=st_[:, 0:n], in_=gt[:, 0:n], func=AF.Sigmoid, scale=1.702), [gt], [st_])
                        kb.op('pool', lambda e: e.tensor_tensor(out=gsT[:, sub, c0:c0 + n], in0=gt[:, 0:n], in1=st_[:, 0:n], op=ALU.mult), [gt, st_], [gsT])
                U0 = u_next(); U1 = u_next()
                for sub in range(4):
                    jb = hf * 8 + jq * 4 + sub
                    for (c0, n) in CH:
                        p = w1_mm(U0, U1, sub, c0, n)
                        kb.op('dve', lambda e, p=p: e.tensor_scalar(out=lt[:, 0:n], in0=p[:, 0:n], scalar1=b1[:, ex_, 16 + jb:16 + jb + 1], scalar2=7.0, op0=ALU.add, op1=ALU.min), [p, b1], [lt])
                        kb.op('pool', lambda e: e.tensor_scalar(out=lt[:, 0:n], in0=lt[:, 0:n], scalar1=-7.0, scalar2=1.0, op0=ALU.max, op1=ALU.add), [lt], [lt])
                        kb.op('dve', lambda e: e.tensor_tensor(out=actT[:, jq * 4 + sub, c0:c0 + n], in0=gsT[:, sub, c0:c0 + n], in1=lt[:, 0:n], op=ALU.mult), [gsT, lt], [actT])
            for cb in range(4):
                U = u_next()
                for t in range(9):
                    rows = 128 if t < 8 else NS
                    tok0 = t * 128
                    p = nps()
                    for k in range(8):
                        tl, kl = upart(U, k)
                        kb.op('pe', lambda e, k=k, tl=tl, kl=kl, p=p: e.matmul(p[0:rows, 0:512], actT[:, k, tok0:tok0 + rows], tl[:, kl, :], start=(k == 0), stop=(k == 7)), [tl, actT], [p])
                    kb.op('dve', lambda e, p=p, t=t: e.scalar_tensor_tensor(out=accM[0:rows, t, cb * 512:(cb + 1) * 512], in0=p[0:rows, 0:512], scalar=gateM[0:rows, t, ex_:ex_ + 1], in1=accM[0:rows, t, cb * 512:(cb + 1) * 512], op0=ALU.mult, op1=ALU.add), [p, gateM, accM], [accM])
    kb.barrier()
    ph.close()

    ph = ExitStack()
    g5 = [kb.sb("g5_%d" % r, [128 if r == 0 else NS, D], F32, ph) for r in range(2)]
    gfbc = kb.sb("gfbc", [128, D], F32, ph)
    kb.dma('pool', gfbc[:], gf_row[:, :].partition_broadcast(128), [], [gfbc], prim=gfbc)
    for r in range(2):
        kb.dma('pool', g5[r][:], mod_d[r:r + 1, 5 * D:6 * D].partition_broadcast(128 if r == 0 else NS), [mod_d], [g5[r]], prim=g5[r])
    x1t = [kb.sb("x1f%d" % i, [128, D], F32, ph) for i in range(2)]
    yo = [kb.sb("yo%d" % i, [128, D], F32, ph) for i in range(2)]
    junk3 = kb.sb("junk3", [128, D], BF16, ph)
    ss2 = kb.sb("ss2", [128, 1], F32, ph); rs2 = kb.sb("rs2", [128, 1], F32, ph)
    for t in range(9):
        rows = 128 if t < 8 else NS
        tok0 = t * 128
        r = 0 if t < 8 else 1
        x, y = x1t[t % 2], yo[t % 2]
        kb.dma('sp', x[0:rows, :], x1_d[tok0:tok0 + rows, :], [x1_d], [x], prim=x)
        kb.op('dve', lambda e: e.tensor_tensor(out=y[0:rows, :], in0=accM[0:rows, t, :], in1=g5[r][0:rows, :], op=ALU.mult), [accM, g5[r]], [y])
        kb.op('pool', lambda e: e.tensor_tensor(out=y[0:rows, :], in0=y[0:rows, :], in1=x[0:rows, :], op=ALU.add), [y, x], [y])
        kb.op('act', lambda e: e.activation(out=junk3[0:rows, :], in_=y[0:rows, :], func=AF.Square, accum_out=ss2[0:rows, :]), [y], [junk3, ss2])
        kb.op('dve', lambda e: e.tensor_scalar(out=ss2[0:rows, :], in0=ss2[0:rows, :], scalar1=1.0 / D, scalar2=1e-6, op0=ALU.mult, op1=ALU.add), [ss2], [ss2])
        kb.op('act', lambda e: e.activation(out=ss2[0:rows, :], in_=ss2[0:rows, :], func=AF.Sqrt), [ss2], [ss2])
        kb.op('dve', lambda e: e.reciprocal(out=rs2[0:rows, :], in_=ss2[0:rows, :]), [ss2], [rs2])
        kb.op('act', lambda e: e.activation(out=y[0:rows, :], in_=y[0:rows, :], func=AF.Copy, scale=rs2[0:rows, :]), [y, rs2], [y])
        kb.op('dve', lambda e: e.tensor_tensor(out=y[0:rows, :], in0=y[0:rows, :], in1=gfbc[0:rows, :], op=ALU.mult), [y, gfbc], [y])
        if t < 8:
            kb.dma('pool', y_o[tok0:tok0 + 128, :], y[:, :], [y], [y_o], prim=y)
        else:
            kb.dma('pool', ys_o[:, :], y[0:NS, :], [y], [ys_o], prim=y)
    kb.finish()
    ph.close()
    late2.close()
    late.close()
    es.close()
    return nc


_NC = None
CORES = list(range(NCORES))


def kernel(**inp):
    global _NC
    f = lambda a: np.ascontiguousarray(np.asarray(a, dtype=np.float32))
    xp = f(inp['x_prompt']); xs = f(inp['x_sample'])
    shared = {
        "ident": np.eye(128, dtype=np.float32),
        "w_ada": f(inp['w_ada'][0]), "b_adaT": f(inp['b_ada'][0].reshape(96, 128).T), "b_ada_row": f(inp['b_ada'][0].reshape(1, -1)),
        "g1T": f(inp['g_norm1'][0].reshape(16, 128).T),
        "w_in": f(inp['w_in'][0]), "b_inT": f(inp['b_in'][0].reshape(128, 128).T), "b_in_row": f(inp['b_in'][0].reshape(1, -1)),
    }
    shared["w_dwT"] = f(inp['w_dw'][0].T.reshape(12, 128, 31).transpose(1, 0, 2))
    shared["cparT"] = f(np.stack([inp['b_dw'][0].reshape(12, 128).T, inp['g_ln_conv'][0].reshape(12, 128).T, inp['b_ln_conv'][0].reshape(12, 128).T], axis=1))
    shared["w_ao"] = f(inp['w_attn_out'][0]); shared["w_co"] = f(inp['w_conv_out'][0]); shared["w_o"] = f(inp['w_o'][0])
    shared["boT"] = f(np.stack([inp['b_attn_out'][0].reshape(16, 128).T, inp['b_conv_out'][0].reshape(16, 128).T], axis=1))
    shared["g2_row"] = f(inp['g_norm2'][0].reshape(1, -1)); shared["w_router"] = f(inp['w_router'][0]); shared["b_router"] = f(inp['b_router'][0].reshape(1, -1))
    shared["w_moe1"] = f(inp['w_moe1'][0]); shared["w_moe2"] = f(inp['w_moe2'][0]); shared["b_moe2"] = f(inp['b_moe2'][0]); shared["gf_row"] = f(inp['g_final'].reshape(1, -1))
    shared["b1T"] = f(inp['b_moe1'][0].reshape(32, 32, 128).transpose(2, 0, 1))
    pp = np.arange(128)[:, None]; nn = np.arange(256)[None, :]
    shared["M1"] = ((nn - pp >= 0) & (nn - pp <= 128)).astype(np.float32)
    SMc = np.zeros((128, 21, NS), np.float32); SMn = np.zeros((NS, 3, NS), np.float32)
    ti = 0
    for g, (W, dd) in enumerate(((128, 1), (512, 4), (2048, 16))):
        for j in range(W // 128):
            m = 128 * j + np.arange(128)[:, None]; ii = np.arange(NS)[None, :]
            SMc[:, ti, :] = (((ii - m) % dd == 0) & (m >= ii)).astype(np.float32)
            ti += 1
        i1 = np.arange(NS)[:, None]; ii = np.arange(NS)[None, :]
        SMn[:, g, :] = ((i1 <= ii) & ((ii - i1) % dd == 0)).astype(np.float32)
    shared["SMc"] = SMc; shared["SMn"] = SMn
    in_maps = []
    for c in CORES:
        b, q = c // 4, c % 4
        s = q * NT
        xh = np.zeros((HALO + NT, D), np.float32)
        lo = max(0, s - HALO)
        xh[HALO - (s - lo):] = xp[b, lo:s + NT]
        cT = np.stack([f(inp['c_prompt'][b]).reshape(16, 128).T, f(inp['c_sample'][c]).reshape(16, 128).T], axis=-1)
        m = dict(shared)
        kval = np.zeros((128, 53), np.float32)
        p1 = np.arange(128)
        for j in range(9):
            kval[:, j] = (s - 128 + 128 * j + p1 >= 0)
        for r in range(4):
            for j in range(3):
                kval[:, 9 + r * 3 + j] = (s - 512 + 512 * j + 4 * p1 + r >= 0)
        for r in range(16):
            kval[:, 21 + 2 * r] = (s - 2048 + 16 * p1 + r >= 0)
            kval[:, 21 + 2 * r + 1] = (s - 2048 + 16 * (128 + p1) + r >= 0)
        m.update({"xh": xh, "xs": f(xs[c]), "cT": f(cT), "kval": kval, "hprev": np.full((128, 1), 1.0 if q > 0 else 0.0, np.float32),
                  "stT": f(inp['state_conv'][0, c].T), "st": f(inp['state_conv'][0, c]),
                  "cache0": f(inp['cache_kv_w128'][0, c]), "cache1": f(inp['cache_kv_w512'][0, c]), "cache2": f(inp['cache_kv_w2048'][0, c])})
        in_maps.append(m)
    if _NC is None:
        _NC = build()
    res = run_bass_kernel_spmd(_NC, in_maps, core_ids=list(range(len(CORES)))).results
    B, S = 2, 4096
    y_p = np.zeros((B, S, D), np.float32); y_s = np.zeros((8, NS, D), np.float32)
    kvp = [np.zeros((1, B, w, 2, 8, 128), np.float32) for w in (128, 512, 2048)]
    kvs = [np.zeros((1, 8, NS, 2, 8, 128), np.float32) for _ in range(3)]
    conv_p = np.zeros((1, B, 30, 1536), np.float32); conv_s = np.zeros((1, 8, 30, 1536), np.float32)
    for ci, c in enumerate(CORES):
        b, q = c // 4, c % 4
        r = res[ci]
        y_p[b, q * NT:(q + 1) * NT] = r["y_o"]
        y_s[c] = r["ys_o"]
        if q == 3:
            kvp[0][0, b] = r["kv1_o"]; kvp[1][0, b] = r["kv2_o"]; conv_p[0, b] = r["convp_o"]
        if q >= 2:
            kvp[2][0, b, (q - 2) * NT:(q - 1) * NT] = r["kv3_o"]
        for g in range(3):
            kvs[g][0, c] = r["kvs_o"][g]
        conv_s[0, c] = r["convs_o"]
    return (y_p, y_s, kvp[0], kvp[1], kvp[2], conv_p, kvs[0], kvs[1], kvs[2], conv_s)
```

```python
import numpy as np
from contextlib import ExitStack
import concourse.bass as bass
import concourse.mybir as mybir
from concourse.bass_utils import run_bass_kernel_spmd

dt = mybir.dt
F32, BF16, I32, U32 = dt.float32, dt.bfloat16, dt.int32, dt.uint32
AF = mybir.ActivationFunctionType
ALU = mybir.AluOpType

D = 2048
NT = 1024
NS = 8
NTS = NT + NS
HALO = 2048
NCORES = 8
CAP = 256
import os
DBG = os.environ.get('KDBG', '')


class T:
    def __init__(self, h, name):
        self.h = h
        self.name = name
        self.w = {}
        self.r = {}
        self.dsem = {}
        self.excl = False
        self.nowaw = False

    def __getitem__(self, k):
        return self.h[k]


class KB:
    def __init__(self, nc, es):
        self.nc = nc
        self.es = es
        self.eng = dict(pe=nc.tensor, act=nc.scalar, dve=nc.vector, pool=nc.gpsimd, sp=nc.sync)
        self.sems = []
        self.semcnt = []
        self.esem = {}
        for k in self.eng:
            self.esem[k] = self.new_sem('e_' + k)
        self.cnt = {k: 0 for k in self.eng}
        self.waited = {k: {} for k in self.eng}
        self.dfree = {'sw': [self.new_sem('dsw%d' % i) for i in range(48)], 'hw': [self.new_sem('dhw%d' % i) for i in range(44)]}
        self.dused = []
        self.uid = 0

    def new_sem(self, name):
        s = self.es.enter_context(self.nc.semaphore(name))
        self.sems.append(s)
        self.semcnt.append(0)
        return len(self.sems) - 1

    def sb(self, name, shape, dtype=F32, es=None):
        self.uid += 1
        es = es or self.es
        return T(es.enter_context(self.nc.sbuf_tensor("%s_%d" % (name, self.uid), list(shape), dtype)), name)

    def ps(self, name, shape, dtype=F32, es=None):
        self.uid += 1
        es = es or self.es
        t = T(es.enter_context(self.nc.psum_tensor("%s_%d" % (name, self.uid), list(shape), dtype)), name)
        t.excl = True
        return t

    def dram(self, name, shape, dtype=F32, kind="Internal"):
        t = T(self.nc.dram_tensor(name, list(shape), dtype, kind=kind).ap(), name)
        t.nowaw = True
        return t

    def _wait(self, e, deps, skip=None):
        wd = self.waited[e]
        for si, v in deps.items():
            if si == skip or wd.get(si, 0) >= v:
                continue
            self.eng[e].wait_ge(self.sems[si], v)
            wd[si] = v

    @staticmethod
    def _merge(d, o):
        for k, v in o.items():
            if d.get(k, 0) < v:
                d[k] = v

    def _deps(self, reads, writes):
        deps = {}
        for t in reads:
            self._merge(deps, t.w)
            if t.excl:
                self._merge(deps, t.r)
        for t in writes:
            if not t.nowaw:
                self._merge(deps, t.w)
            self._merge(deps, t.r)
        return deps

    def _post(self, ev, reads, writes):
        for t in writes:
            if t.nowaw:
                self._merge(t.w, ev)
            else:
                t.w = dict(ev)
            t.r = {}
        for t in reads:
            if t not in writes:
                self._merge(t.r, ev)

    def op(self, e, fn, reads=(), writes=()):
        si = self.esem[e]
        self._wait(e, self._deps(reads, writes), skip=si if e == 'pe' else None)
        inst = fn(self.eng[e])
        self.cnt[e] += 1
        self.semcnt[si] = self.cnt[e]
        inst.then_inc(self.sems[si], 1)
        self._post({si: self.cnt[e]}, reads, writes)
        return inst

    def dma(self, q, out, in_, reads=(), writes=(), prim=None, fn=None):
        kind = 'sw' if q == 'pool' else 'hw'
        if kind not in prim.dsem:
            prim.dsem[kind] = self.dfree[kind].pop()
            if prim not in self.dused:
                self.dused.append(prim)
        si = prim.dsem[kind]
        self._wait(q, self._deps(reads, writes), skip=si if prim in writes else None)
        inst = self.eng[q].dma_start(out=out, in_=in_) if fn is None else fn(self.eng[q])
        self.semcnt[si] += 16
        inst.then_inc(self.sems[si], 16)
        self._post({si: self.semcnt[si]}, reads, writes)
        return inst

    def barrier(self, keep=()):
        allv = {si: v for si, v in enumerate(self.semcnt) if v > 0}
        for e in self.eng:
            self._wait(e, allv, skip=self.esem[e])
        rest = []
        for t in self.dused:
            if t in keep:
                rest.append(t)
            else:
                for kind, si in t.dsem.items():
                    self.dfree[kind].append(si)
                t.dsem = {}
        self.dused = rest

    def finish(self):
        allv = {si: v for si, v in enumerate(self.semcnt) if v > 0}
        for e in self.eng:
            self._wait(e, allv, skip=self.esem[e])


def build():
    nc = bass.Bass("TRN2", target_bir_lowering=False)
    es = ExitStack()
    kb = KB(nc, es)
    IN = lambda n, s, d=F32: kb.dram(n, s, d, "ExternalInput")
    OUT = lambda n, s, d=F32: kb.dram(n, s, d, "ExternalOutput")
    xh = IN("xh", [HALO + NT, D]); xs = IN("xs", [NS, D]); cT = IN("cT", [128, 16, 2])
    ident_d = IN("ident", [128, 128])
    w_ada = IN("w_ada", [D, 6 * D]); b_adaT = IN("b_adaT", [128, 96]); b_ada_row = IN("b_ada_row", [1, 6 * D])
    g1T = IN("g1T", [128, 16])
    M1_d = IN("M1", [128, 256]); kval_d = IN("kval", [128, 53]); SMc_d = IN("SMc", [128, 21, NS]); SMn_d = IN("SMn", [NS, 3, NS])
    cache_d = [IN("cache%d" % g, [w, 2, 8, 128]) for g, w in enumerate((128, 512, 2048))]
    w_dwT = IN("w_dwT", [128, 12, 31]); cparT = IN("cparT", [128, 3, 12]); hprev_d = IN("hprev", [128, 1])
    stT_d = IN("stT", [1536, 30]); st_d = IN("st", [30, 1536])
    w_ao = IN("w_ao", [1024, D]); w_co = IN("w_co", [1536, D]); boT = IN("boT", [128, 2, 16]); w_o = IN("w_o", [D, D])
    g2_row = IN("g2_row", [1, D]); w_router = IN("w_router", [D, 32]); b_router = IN("b_router", [1, 32])
    w_moe1 = IN("w_moe1", [32, D, 2 * D]); b1T_d = IN("b1T", [128, 32, 32]); w_moe2 = IN("w_moe2", [32, D, D]); b_moe2 = IN("b_moe2", [32, D]); gf_row = IN("gf_row", [1, D])
    w_in = IN("w_in", [D, 16384]); b_inT = IN("b_inT", [128, 128]); b_in_row = IN("b_in_row", [1, 16384])
    y_o = OUT("y_o", [NT, D]); ys_o = OUT("ys_o", [NS, D])
    kv_o = [OUT("kv1_o", [128, 2, 8, 128]), OUT("kv2_o", [512, 2, 8, 128]), OUT("kv3_o", [1024, 2, 8, 128])]
    kvs_o = OUT("kvs_o", [3, NS, 2, 8, 128])
    convp_o = OUT("convp_o", [30, 1536]); convs_o = OUT("convs_o", [30, 1536])
    GH = [128, 512, 2048]
    NCTX = [GH[g] + NT for g in range(3)]
    kT_d = [kb.dram("kT_d%d" % g, [8, 128, NCTX[g]], BF16) for g in range(3)]
    V_d = [kb.dram("V_d%d" % g, [NCTX[g], 8, 128], BF16) for g in range(3)]
    qT_d = kb.dram("qT_d", [3, 8, 128, NT], BF16)
    qsT_d = kb.dram("qsT_d", [3, 8, 128, NS], BF16); ksT_d = kb.dram("ksT_d", [3, 8, 128, NS], BF16)
    Vs_d = kb.dram("Vs_d", [3, NS, 8, 128], BF16)
    u_d = kb.dram("u_d", [1536, 128 + NT]); us_d = kb.dram("us_d", [1536, NS])
    sg_d = kb.dram("sg_d", [4096, NTS])
    mod_d = kb.dram("mod_d", [2, 6 * D])

    ident = kb.sb("ident", [128, 128]); kb.dma('sp', ident[:], ident_d[:], [ident_d], [ident], prim=ident)
    identb = kb.sb("identb", [128, 128], BF16)
    kb.op('dve', lambda e: e.tensor_copy(out=identb[:], in_=ident[:]), [ident], [identb])
    A1 = kb.sb("A1", [128, 16, 2]); B1 = kb.sb("B1", [128, 16, 2])
    PS = [kb.ps("ps%d" % i, [128, 512]) for i in range(8)]

    ph = ExitStack()
    sil = kb.sb("sil", [128, 16, 2], F32, ph); silb = kb.sb("silb", [128, 16, 2], BF16, ph)
    badT = kb.sb("badT", [128, 96], F32, ph); g1s = kb.sb("g1s", [128, 16], F32, ph)
    modT = kb.sb("modT", [128, 32, 2], F32, ph)
    kb.dma('sp', sil[:], cT[:], [cT], [sil], prim=sil)
    kb.dma('sp', badT[:], b_adaT[:], [b_adaT], [badT], prim=badT)
    kb.dma('sp', g1s[:], g1T[:], [g1T], [g1s], prim=g1s)
    kb.op('act', lambda e: e.activation(out=silb[:], in_=sil[:], func=AF.Silu), [sil], [silb])
    stg = [kb.sb("stg%d" % i, [128, 16, 256], F32, ph) for i in range(2)]
    KSPL = [(0, 6, 'act'), (6, 12, 'dve'), (12, 16, 'pool')]
    wbs = [[kb.sb("wb%d_%d" % (i, j), [128, KSPL[j][1] - KSPL[j][0], 256], BF16, ph) for j in range(3)] for i in range(2)]

    def cast_block(stage, wb3, nk=16, n=256):
        for j, (k0, k1, e) in enumerate(KSPL):
            k1 = min(k1, nk)
            if k0 >= k1:
                continue
            if e == 'act':
                kb.op('act', lambda en, k0=k0, k1=k1, j=j: en.activation(out=wb3[j][:, 0:k1 - k0, 0:n], in_=stage[:, k0:k1, 0:n], func=AF.Copy), [stage], [wb3[j]])
            else:
                kb.op(e, lambda en, k0=k0, k1=k1, j=j: en.tensor_copy(out=wb3[j][:, 0:k1 - k0, 0:n], in_=stage[:, k0:k1, 0:n]), [stage], [wb3[j]])

    class WP:
        def __init__(self, t, kl):
            self.t, self.kl = t, kl

        def __getitem__(self, key):
            p, k, c = key
            return self.t[p, self.kl, c]

    def wpart(wb3, k):
        for j, (k0, k1, e) in enumerate(KSPL):
            if k0 <= k < k1:
                return WP(wb3[j], k - k0)

    def load_block(w_ap2d, col0, n, stage, nk=16):
        kb.dma('sp', stage[:, 0:nk, 0:n], w_ap2d[:, col0:col0 + n].rearrange("(k p) n -> p k n", p=128), [], [stage], prim=stage)

    modrow = kb.sb("modrow", [2, 256], F32, ph); badrow = kb.sb("badrow", [2, 6 * D], F32, ph)
    kb.dma('pool', badrow[:], b_ada_row[:].partition_broadcast(2), [b_ada_row], [badrow], prim=badrow)
    nblk = 6 * D // 256
    load_block(w_ada, 0, 256, stg[0])
    for bi in range(nblk):
        st, wb3 = stg[bi % 2], wbs[bi % 2]
        if bi + 1 < nblk:
            load_block(w_ada, (bi + 1) * 256, 256, stg[(bi + 1) % 2])
        cast_block(st, wb3)
        if bi < 16:
            for sub in range(2):
                p = PS[sub]
                for k in range(16):
                    wp = wpart(wb3, k)
                    kb.op('pe', lambda e, k=k, wp=wp, sub=sub, p=p: e.matmul(p[:, 0:2], wp[:, k, sub * 128:(sub + 1) * 128], silb[:, k, :], start=(k == 0), stop=(k == 15)), [wp.t, silb], [p])
                blk = bi * 2 + sub
                kb.op('dve', lambda e, p=p, blk=blk: e.tensor_scalar(out=modT[:, blk, :], in0=p[:, 0:2], scalar1=badT[:, blk:blk + 1], scalar2=None, op0=ALU.add), [p, badT], [modT])
        else:
            p = PS[2 + bi % 2]
            for k in range(16):
                wp = wpart(wb3, k)
                kb.op('pe', lambda e, k=k, wp=wp, p=p: e.matmul(p[0:2, 0:256], silb[:, k, :], wp[:, k, :], start=(k == 0), stop=(k == 15)), [wp.t, silb], [p])
            kb.op('dve', lambda e, p=p, bi=bi: e.tensor_tensor(out=modrow[:], in0=p[0:2, 0:256], in1=badrow[:, bi * 256:(bi + 1) * 256], op=ALU.add), [p, badrow], [modrow])
            kb.dma('pool', mod_d[:, bi * 256:(bi + 1) * 256], modrow[:], [modrow], [mod_d], prim=modrow)
    for c in range(2):
        kb.op('dve', lambda e, c=c: e.scalar_tensor_tensor(out=A1[:, :, c], in0=modT[:, 16:32, c], scalar=1.0, in1=g1s[:], op0=ALU.add, op1=ALU.mult), [modT, g1s], [A1])
        kb.op('dve', lambda e, c=c: e.tensor_copy(out=B1[:, :, c], in_=modT[:, 0:16, c]), [modT], [B1])
    kb.barrier()
    ph.close()

    ph = ExitStack()
    hT = kb.sb("hT", [128, 16, NTS], BF16, ph)
    xt = [kb.sb("xt%d" % i, [128, D], F32, ph) for i in range(2)]
    xn = [kb.sb("xn%d" % i, [128, D], F32, ph) for i in range(2)]
    junk = kb.sb("junk", [128, D], BF16, ph)
    ssq = [kb.sb("ssq%d" % i, [128, 1], F32, ph) for i in range(2)]
    rstd = [kb.sb("rstd%d" % i, [128, 1], F32, ph) for i in range(2)]
    binT = kb.sb("binT", [128, 128], F32, ph)
    kb.dma('sp', binT[:], b_inT[:], [b_inT], [binT], prim=binT)
    stg = [kb.sb("stg%d" % i, [128, 16, 256], F32, ph) for i in range(2)]
    wbs = [[kb.sb("wb%d_%d" % (i, j), [128, KSPL[j][1] - KSPL[j][0], 256], BF16, ph) for j in range(3)] for i in range(2)]
    ev_bf = [kb.sb("evbf%d" % i, [128, 512], BF16, ph) for i in range(4)]
    ev_f = [kb.sb("evf%d" % i, [128, 512], F32, ph) for i in range(4)]
    sgt = [kb.sb("sgt%d" % i, [128, 512], F32, ph) for i in range(2)]
    vb = [kb.sb("vb%d" % i, [128, 256], F32, ph) for i in range(2)]
    ko = [kb.sb("ko%d" % i, [128, 128], F32, ph) for i in range(4)]
    cnt = {'ev': 0, 'ps': 0, 'ko': 0, 'x': 0, 'vb': 0, 'sg': 0}

    def rr(key, n):
        cnt[key] += 1
        return (cnt[key] - 1) % n

    def make_hT(src_ap, nrows, col0, grp):
        i = rr('x', 2)
        x, xnn, ss, rs = xt[i], xn[i], ssq[i], rstd[i]
        kb.dma('sp', x[0:nrows, :], src_ap, [], [x], prim=x)
        kb.op('act', lambda e: e.activation(out=junk[0:nrows, :], in_=x[0:nrows, :], func=AF.Square, accum_out=ss[0:nrows, :]), [x], [junk, ss])
        kb.op('dve', lambda e: e.tensor_scalar(out=ss[0:nrows, :], in0=ss[0:nrows, :], scalar1=1.0 / D, scalar2=1e-6, op0=ALU.mult, op1=ALU.add), [ss], [ss])
        kb.op('act', lambda e: e.activation(out=ss[0:nrows, :], in_=ss[0:nrows, :], func=AF.Sqrt), [ss], [ss])
        kb.op('dve', lambda e: e.reciprocal(out=rs[0:nrows, :], in_=ss[0:nrows, :]), [ss], [rs])
        kb.op('act', lambda e: e.activation(out=xnn[0:nrows, :], in_=x[0:nrows, :], func=AF.Copy, scale=rs[0:nrows, :]), [x, rs], [xnn])
        for kq in range(4):
            p = PS[4 + rr('ps', 4)]
            for kk in range(4):
                k = kq * 4 + kk
                kb.op('pe', lambda e, k=k, kk=kk, p=p: e.transpose(p[:, kk * 128:kk * 128 + nrows], xnn[0:nrows, k * 128:(k + 1) * 128], ident[0:nrows, 0:nrows]), [xnn, ident], [p])
            for kk in range(4):
                k = kq * 4 + kk
                kb.op('dve' if kk % 2 == 0 else 'pool' if False else 'dve', lambda e, k=k, kk=kk, p=p: e.tensor_scalar(out=hT[:, k, col0:col0 + nrows], in0=p[:, kk * 128:kk * 128 + nrows], scalar1=A1[:, k, grp:grp + 1], scalar2=B1[:, k, grp:grp + 1], op0=ALU.mult, op1=ALU.add), [p, A1, B1], [hT])

    QO, KO, VO, UAO, UBO, GAO, GBO = 0, 3072, 6144, 9216, 10752, 12288, 14336
    wq = {'i': 0}

    def stream(blocks, body):
        def issue(bi):
            st = stg[(wq['i'] + bi) % 2]
            for (c0, n, d0) in blocks[bi]:
                kb.dma('sp', st[:, :, d0:d0 + n], w_in[:, c0:c0 + n].rearrange("(k p) n -> p k n", p=128), [], [st], prim=st)
        issue(0)
        for bi in range(len(blocks)):
            if bi + 1 < len(blocks):
                issue(bi + 1)
            st, wb3 = stg[(wq['i'] + bi) % 2], wbs[(wq['i'] + bi) % 2]
            cast_block(st, wb3)
            body(bi, wb3)
        wq['i'] += len(blocks)

    def fm_mm(wb3, sub, tok0, ntok):
        p = PS[rr('ps', 4)]
        for k in range(16):
            wp = wpart(wb3, k)
            kb.op('pe', lambda e, k=k, wp=wp: e.matmul(p[:, 0:ntok], wp[:, k, sub * 128:(sub + 1) * 128], hT[:, k, tok0:tok0 + ntok], start=(k == 0), stop=(k == 15)), [wp.t, hT], [p])
        return p

    def k_block(g, h, wb3, sub, tokblocks, ctx0, out_rows):
        blk = (KO + (g * 8 + h) * 128) // 128
        for (c0, ntok, cc) in tokblocks:
            p = fm_mm(wb3, sub, c0, ntok)
            i = rr('ev', 4)
            eb, ef = ev_bf[i], ev_f[i]
            need = [(t0, r0) for (t0, r0) in out_rows if c0 <= t0 < c0 + ntok]
            if need:
                kb.op('dve', lambda e, p=p, ef=ef: e.tensor_scalar(out=ef[:, 0:ntok], in0=p[:, 0:ntok], scalar1=binT[:, blk:blk + 1], scalar2=None, op0=ALU.add), [p, binT], [ef])
                kb.op('act', lambda e, ef=ef, eb=eb: e.activation(out=eb[:, 0:ntok], in_=ef[:, 0:ntok], func=AF.Copy), [ef], [eb])
            else:
                kb.op('act', lambda e, p=p, eb=eb: e.activation(out=eb[:, 0:ntok], in_=p[:, 0:ntok], func=AF.Identity, bias=binT[:, blk:blk + 1]), [p, binT], [eb])
            kb.dma('pool', kT_d[g][h, :, cc:cc + ntok], eb[:, 0:ntok], [eb], [kT_d[g]], prim=eb)
            if need:
                for (t0, r0) in need:
                    pt = PS[4 + rr('ps', 4)]
                    kb.op('pe', lambda e, pt=pt, ef=ef, t0=t0: e.transpose(pt[:, 0:128], ef[:, t0 - c0:t0 - c0 + 128], ident[:]), [ef, ident], [pt])
                    kk = ko[rr('ko', 4)]
                    kb.op('act', lambda e, pt=pt, kk=kk: e.activation(out=kk[:], in_=pt[:, 0:128], func=AF.Copy), [pt], [kk])
                    if 'd' not in os.environ.get('KSKIP', ''):
                        kb.dma(os.environ.get('KSQ', 'pool'), kv_o[g][r0:r0 + 128, 0, h, :], kk[:], [kk], [kv_o[g]], prim=kk)

    def v_block(g, h0, wb3, toktiles, out_rows):
        c0 = VO + (g * 8 + h0) * 128
        b = vb[rr('vb', 2)]
        kb.dma('pool', b[:], b_in_row[:, c0:c0 + 256].partition_broadcast(128), [b_in_row], [b], prim=b)
        for (t0, cr) in toktiles:
            p = PS[rr('ps', 4)]
            for k in range(16):
                wp = wpart(wb3, k)
                kb.op('pe', lambda e, k=k, wp=wp, p=p: e.matmul(p[:, 0:256], hT[:, k, t0:t0 + 128], wp[:, k, :], start=(k == 0), stop=(k == 15)), [wp.t, hT], [p])
            i = rr('ev', 4)
            eb, ef = ev_bf[i], ev_f[i]
            kb.op('dve', lambda e, p=p, ef=ef: e.tensor_tensor(out=ef[:, 0:256], in0=p[:, 0:256], in1=b[:], op=ALU.add), [p, b], [ef])
            kb.op('act', lambda e, eb=eb, ef=ef: e.activation(out=eb[:, 0:256], in_=ef[:, 0:256], func=AF.Copy), [ef], [eb])
            kb.dma('pool', V_d[g][cr:cr + 128, h0:h0 + 2, :], eb[:, 0:256].rearrange("p (h d) -> p h d", h=2), [eb], [V_d[g]], prim=eb)
            for (tt0, r0) in out_rows:
                if tt0 == t0:
                    kb.dma('pool', kv_o[g][r0:r0 + 128, 1, h0:h0 + 2, :], ef[:, 0:256].rearrange("p (h d) -> p h d", h=2), [ef], [kv_o[g]], prim=ef)

    def u_block(j, wb3, tokblocks):
        ba, bb = (UAO // 128) + j, (UBO // 128) + j
        for (c0, ntok, dst, dc) in tokblocks:
            pa = fm_mm(wb3, 0, c0, ntok)
            pb = fm_mm(wb3, 1, c0, ntok)
            s = sgt[rr('sg', 2)]
            i = rr('ev', 4)
            ef = ev_f[i]
            kb.op('act', lambda e, pb=pb, s=s: e.activation(out=s[:, 0:ntok], in_=pb[:, 0:ntok], func=AF.Sigmoid, bias=binT[:, bb:bb + 1]), [pb, binT], [s])
            kb.op('dve', lambda e, pa=pa, s=s, ef=ef: e.scalar_tensor_tensor(out=ef[:, 0:ntok], in0=pa[:, 0:ntok], scalar=binT[:, ba:ba + 1], in1=s[:, 0:ntok], op0=ALU.add, op1=ALU.mult), [pa, s, binT], [ef])
            kb.dma('pool', dst[j * 128:(j + 1) * 128, dc:dc + ntok], ef[:, 0:ntok], [ef], [dst], prim=ef)

    for grp_i in range(2 if DBG in ('', 'A', 'B') else 0):
        if DBG == 'A' and grp_i == 1:
            break
        base = grp_i * 1024
        for t in range(8):
            make_hT(xh[base + t * 128: base + (t + 1) * 128, :], 128, t * 128, 0)
        blocks, kinds = [], []
        for hp in range(4):
            blocks.append([(KO + (2 * 8 + 2 * hp) * 128, 256, 0)]); kinds.append(('k', 2, 2 * hp))
        for hp in range(4):
            blocks.append([(VO + (2 * 8 + 2 * hp) * 128, 256, 0)]); kinds.append(('v', 2, 2 * hp))
        if grp_i == 1:
            for g in (1, 0):
                for hp in range(4):
                    blocks.append([(KO + (g * 8 + 2 * hp) * 128, 256, 0)]); kinds.append(('k', g, 2 * hp))
                for hp in range(4):
                    blocks.append([(VO + (g * 8 + 2 * hp) * 128, 256, 0)]); kinds.append(('v', g, 2 * hp))
            for j in range(12):
                blocks.append([(UAO + j * 128, 128, 0), (UBO + j * 128, 128, 128)]); kinds.append(('u', j, 0))

        def body(bi, wb3, kinds=kinds, base=base):
            kind, g, h0 = kinds[bi]
            if kind == 'k':
                for sub in range(2):
                    if g == 2:
                        tb = [(0, 512, base), (512, 512, base + 512)]
                    elif g == 1:
                        tb = [(512, 512, 0)]
                    else:
                        tb = [(896, 128, 0)]
                    k_block(g, h0 + sub, wb3, sub, tb, 0, [])
            elif kind == 'v':
                if g == 2:
                    tt = [(t * 128, base + t * 128) for t in range(8)]
                elif g == 1:
                    tt = [(512 + t * 128, t * 128) for t in range(4)]
                else:
                    tt = [(896, 0)]
                v_block(g, h0, wb3, tt, [])
            else:
                u_block(g, wb3, [(896, 128, u_d, 0)])
        stream(blocks, body)

    for t in range(8 if DBG in ('', 'C') else 0):
        make_hT(xh[HALO + t * 128: HALO + (t + 1) * 128, :], 128, t * 128, 0)
    if DBG in ('', 'C'):
        make_hT(xs[:, :], NS, NT, 1)
    blocks, kinds = [], []
    for g in range(3):
        for hp in range(4):
            blocks.append([(QO + (g * 8 + 2 * hp) * 128, 256, 0)]); kinds.append(('q', g, 2 * hp))
        for hp in range(4):
            blocks.append([(KO + (g * 8 + 2 * hp) * 128, 256, 0)]); kinds.append(('k', g, 2 * hp))
        for hp in range(4):
            blocks.append([(VO + (g * 8 + 2 * hp) * 128, 256, 0)]); kinds.append(('v', g, 2 * hp))
    for j in range(12):
        blocks.append([(UAO + j * 128, 128, 0), (UBO + j * 128, 128, 128)]); kinds.append(('u', j, 0))
    for j in range(16):
        blocks.append([(GAO + j * 128, 256, 0)] if False else [(GAO + 2 * j * 128, 256, 0)]); kinds.append(('g', 2 * j, 0))
    vs_sb = kb.sb("vs_sb", [NS, 256], F32, ph); vs_bf = kb.sb("vs_bf", [NS, 256], BF16, ph)
    sm_bf = kb.sb("sm_bf", [128, NS], BF16, ph); sm_f = kb.sb("sm_f", [128, NS], F32, ph); sm_t = kb.sb("sm_t", [NS, 128], F32, ph)

    def bodyC(bi, wb3):
        kind, g, h0 = kinds[bi]
        if kind == 'q':
            for sub in range(2):
                h = h0 + sub
                blk = (QO + (g * 8 + h) * 128) // 128
                for half in range(2):
                    p = fm_mm(wb3, sub, half * 512, 512)
                    eb = ev_bf[rr('ev', 4)]
                    kb.op('act', lambda e, p=p, eb=eb, blk=blk: e.activation(out=eb[:, :], in_=p[:, :], func=AF.Identity, bias=binT[:, blk:blk + 1]), [p, binT], [eb])
                    kb.dma('pool', qT_d[g, h, :, half * 512:(half + 1) * 512], eb[:, :], [eb], [qT_d], prim=eb)
                p = fm_mm(wb3, sub, NT, NS)
                kb.op('act', lambda e, p=p, blk=blk: e.activation(out=sm_bf[:, :], in_=p[:, 0:NS], func=AF.Identity, bias=binT[:, blk:blk + 1]), [p, binT], [sm_bf])
                kb.dma('pool', qsT_d[g, h, :, :], sm_bf[:, :], [sm_bf], [qsT_d], prim=sm_bf)
        elif kind == 'k':
            W = GH[g]
            nout = min(W, NT)
            outs = [(NT - nout + t * 128, t * 128) for t in range(nout // 128)]
            for sub in range(2):
                h = h0 + sub
                KS = os.environ.get('KSUB', 'os')
                k_block(g, h, wb3, sub, [(0, 512, W), (512, 512, W + 512)], 0, outs if 'o' in KS else [])
                if 's' not in KS:
                    continue
                blk = (KO + (g * 8 + h) * 128) // 128
                p = fm_mm(wb3, sub, NT, NS)
                kb.op('dve', lambda e, p=p, blk=blk: e.tensor_scalar(out=sm_f[:, :], in0=p[:, 0:NS], scalar1=binT[:, blk:blk + 1], scalar2=None, op0=ALU.add), [p, binT], [sm_f])
                kb.op('act', lambda e: e.activation(out=sm_bf[:, :], in_=sm_f[:, :], func=AF.Copy), [sm_f], [sm_bf])
                kb.dma('pool', ksT_d[g, h, :, :], sm_bf[:, :], [sm_bf], [ksT_d], prim=sm_bf)
                pt = PS[4 + rr('ps', 4)]
                kb.op('pe', lambda e, pt=pt: e.transpose(pt[0:NS, 0:128], sm_f[:, :], ident[:]), [sm_f, ident], [pt])
                kb.op('act', lambda e, pt=pt: e.activation(out=sm_t[:, :], in_=pt[0:NS, 0:128], func=AF.Copy), [pt], [sm_t])
                kb.dma('pool', kvs_o[g, :, 0, h, :], sm_t[:, :], [sm_t], [kvs_o], prim=sm_t)
        elif kind == 'v':
            W = GH[g]
            nout = min(W, NT)
            outs = [(NT - nout + t * 128, t * 128) for t in range(nout // 128)]
            v_block(g, h0, wb3, [(t * 128, W + t * 128) for t in range(8)], outs)
            c0 = VO + (g * 8 + h0) * 128
            b = vb[rr('vb', 2)]
            kb.dma('pool', b[:], b_in_row[:, c0:c0 + 256].partition_broadcast(128), [b_in_row], [b], prim=b)
            p = PS[rr('ps', 4)]
            for k in range(16):
                wp = wpart(wb3, k)
                kb.op('pe', lambda e, k=k, wp=wp, p=p: e.matmul(p[0:NS, 0:256], hT[:, k, NT:NTS], wp[:, k, :], start=(k == 0), stop=(k == 15)), [wp.t, hT], [p])
            kb.op('dve', lambda e, p=p, b=b: e.tensor_tensor(out=vs_sb[:, :], in0=p[0:NS, 0:256], in1=b[0:NS, :], op=ALU.add), [p, b], [vs_sb])
            kb.op('act', lambda e: e.activation(out=vs_bf[:, :], in_=vs_sb[:, :], func=AF.Copy), [vs_sb], [vs_bf])
            kb.dma('pool', Vs_d[g, :, h0:h0 + 2, :], vs_bf[:, :].rearrange("p (h d) -> p h d", h=2), [vs_bf], [Vs_d], prim=vs_bf)
            kb.dma('pool', kvs_o[g, :, 1, h0:h0 + 2, :], vs_sb[:, :].rearrange("p (h d) -> p h d", h=2), [vs_sb], [kvs_o], prim=vs_sb)
        elif kind == 'u':
            u_block(g, wb3, [(0, 512, u_d, 128), (512, 512, u_d, 128 + 512), (NT, NS, us_d, 0)])
        else:
            for sub in range(2):
                jb = g + sub
                blk = GAO // 128 + jb
                for (c0, ntok) in ((0, 512), (512, 512), (NT, NS)):
                    p = fm_mm(wb3, sub, c0, ntok)
                    ef = ev_f[rr('ev', 4)]
                    kb.op('act', lambda e, p=p, ef=ef, blk=blk, ntok=ntok: e.activation(out=ef[:, 0:ntok], in_=p[:, 0:ntok], func=AF.Sigmoid, bias=binT[:, blk:blk + 1]), [p, binT], [ef])
                    kb.dma('pool', sg_d[jb * 128:(jb + 1) * 128, c0:c0 + ntok], ef[:, 0:ntok], [ef], [sg_d], prim=ef)
    KK = os.environ.get('KKINDS', 'qkvug')
    sel = [i for i in range(len(blocks)) if kinds[i][0] in KK]
    blocks = [blocks[i] for i in sel]; kinds = [kinds[i] for i in sel]
    if DBG in ('', 'C'):
        stream(blocks, bodyC)
    kb.barrier()
    ph.close()


    mid = ExitStack()
    attn_oT = kb.sb("attn_oT", [128, 8, NTS], BF16, mid)
    SCALE = 128.0 ** -0.5
    if DBG in ('', 'C', 'ATT'):
        ph = ExitStack()
        M1 = kb.sb("M1", [128, 256], F32, ph); kb.dma('sp', M1[:], M1_d[:], [], [M1], prim=M1)
        kval = kb.sb("kval", [128, 53], F32, ph); kb.dma('sp', kval[:], kval_d[:], [], [kval], prim=kval)
        SMc = kb.sb("SMc", [128, 21, NS], F32, ph); kb.dma('sp', SMc[:], SMc_d[:], [], [SMc], prim=SMc)
        SMn = kb.sb("SMn", [NS, 3, NS], F32, ph); kb.dma('sp', SMn[:], SMn_d[:], [], [SMn], prim=SMn)
        onesb = kb.sb("onesb", [128, 128], BF16, ph)
        kb.op('dve', lambda e: e.memset(onesb[:], 1.0), [], [onesb])
        qt = [kb.sb("qt%d" % i, [128, 3, NT], BF16, ph) for i in range(2)]
        kt = [[kb.sb("kt%d_%d" % (i, g), [128, NCTX[g]], BF16, ph) for g in range(3)] for i in range(2)]
        vt1 = [kb.sb("vt1_%d" % i, [128, 9, 128], BF16, ph) for i in range(2)]
        vt2 = [kb.sb("vt2_%d" % i, [128, 4, 3, 128], BF16, ph) for i in range(2)]
        vt3 = [kb.sb("vt3_%d" % i, [128, 16, 2, 128], BF16, ph) for i in range(2)]
        Et = [kb.sb("Et%d" % i, [128, 256], F32, ph) for i in range(3)]
        Pt = [kb.sb("Pt%d" % i, [128, 256], BF16, ph) for i in range(3)]
        acc = kb.sb("acc", [128, NT], F32, ph); dacc = kb.sb("dacc", [128, NT], F32, ph)
        qs = [kb.sb("qs%d" % i, [128, 3, NS], BF16, ph) for i in range(2)]
        ksn = [kb.sb("ksn%d" % i, [128, 3, NS], BF16, ph) for i in range(2)]
        vsn = [kb.sb("vsn%d" % i, [NS, 3, 128], BF16, ph) for i in range(2)]
        ck = [kb.sb("ck%d" % i, [128, 128], F32, ph) for i in range(3)]
        cv = [kb.sb("cv%d" % i, [128, 128], F32, ph) for i in range(3)]
        ckT = [kb.sb("ckT%d" % i, [128, 128], BF16, ph) for i in range(3)]
        cvb = [kb.sb("cvb%d" % i, [128, 128], BF16, ph) for i in range(3)]
        Es = [kb.sb("Es%d" % i, [128, NS], F32, ph) for i in range(3)]
        Psm = [kb.sb("Psm%d" % i, [128, NS], BF16, ph) for i in range(3)]
        osn = kb.sb("osn", [128, NS], F32, ph); dsn = kb.sb("dsn", [128, NS], F32, ph)
        ac = {'s': 0, 'c': 0}

        def load_head(h, i):
            for g in range(3):
                kb.dma('sp', qt[i][:, g, :], qT_d[g, h, :, :], [qT_d], [qt[i]], prim=qt[i])
                kb.dma('sp', kt[i][g][:, :], kT_d[g][h, :, :], [kT_d[g]], [kt[i][g]], prim=kt[i][g])
            kb.dma('sp', vt1[i][:], V_d[0][:, h, :].rearrange("(j p) d -> p j d", p=128), [V_d[0]], [vt1[i]], prim=vt1[i])
            for r in range(4):
                kb.dma('sp', vt2[i][:, r, :, :], V_d[1][:, h, :].rearrange("(j p r) d -> p r j d", p=128, r=4)[:, r, :, :], [V_d[1]], [vt2[i]], prim=vt2[i])
            v3 = V_d[2][:, h, :].rearrange("(m r) d -> m r d", r=16)
            kb.dma('sp', vt3[i][:, :, 0, :], v3[0:128, :, :], [V_d[2]], [vt3[i]], prim=vt3[i])
            kb.dma('sp', vt3[i][0:64, :, 1, :], v3[128:192, :, :], [V_d[2]], [vt3[i]], prim=vt3[i])
            kb.dma('sp', qs[i][:], qsT_d[:, h, :, :].rearrange("g d q -> d g q"), [qsT_d], [qs[i]], prim=qs[i])
            kb.dma('sp', ksn[i][:], ksT_d[:, h, :, :].rearrange("g d q -> d g q"), [ksT_d], [ksn[i]], prim=ksn[i])
            kb.dma('sp', vsn[i][:], Vs_d[:, :, h, :].rearrange("g t d -> t g d"), [Vs_d], [vsn[i]], prim=vsn[i])

        def tile_attn(kT_ap, nk, q_ap, N, v_ap, kv_idx, m0, qstart, first):
            sp_ = PS[4 + ac['s'] % 3]; Ei = Et[ac['s'] % 3]; Pi = Pt[ac['s'] % 3]; ac['s'] += 1
            kb.op('pe', lambda e: e.matmul(sp_[0:nk, 0:N], kT_ap, q_ap, start=True, stop=True), kt_reads, [sp_])
            kb.op('act', lambda e: e.activation(out=Ei[0:nk, 0:N], in_=sp_[0:nk, 0:N], func=AF.Exp, scale=SCALE), [sp_], [Ei])
            kb.op('dve', lambda e: e.scalar_tensor_tensor(out=Pi[0:nk, 0:N], in0=Ei[0:nk, 0:N], scalar=kval[0:nk, kv_idx:kv_idx + 1], in1=M1[0:nk, m0:m0 + N], op0=ALU.mult, op1=ALU.mult), [Ei, kval, M1], [Pi])
            segs = []
            a, b = qstart, qstart + N
            if a < 512 and b > 512:
                segs = [(a, 512, 0), (512, b, 512 - a)]
            else:
                segs = [(a, b, 0)]
            for (qa, qb, po) in segs:
                bank = qa // 512
                st = first[bank]
                first[bank] = False
                n = qb - qa
                kb.op('pe', lambda e: e.matmul(PS[bank][:, qa - bank * 512:qb - bank * 512], v_ap, Pi[0:nk, po:po + n], start=st, stop=True, skip_group_check=True), [Pi] + v_reads, [PS[bank]])
                kb.op('pe', lambda e: e.matmul(PS[2 + bank][:, qa - bank * 512:qb - bank * 512], onesb[0:nk, :], Pi[0:nk, po:po + n], start=st, stop=True, skip_group_check=True), [Pi, onesb], [PS[2 + bank]])

        load_head(0, 0)
        for h in range(8):
            i = h % 2
            if h + 1 < 8:
                load_head(h + 1, (h + 1) % 2)
            kt_reads = [kt[i][0], kt[i][1], kt[i][2], qt[i]]
            v_reads = [vt1[i], vt2[i], vt3[i]]
            first = [True, True]
            for j in range(9):
                t0 = max(0, 128 * (j - 1)); t1 = min(NT, 128 * (j - 1) + 256)
                m0 = 128 if j == 0 else 0
                tile_attn(kt[i][0][:, 128 * j:128 * j + 128], 128, qt[i][:, 0, t0:t1], t1 - t0, vt1[i][:, j, :], j, m0, t0, first)
            for b in range(2):
                kb.op('act', lambda e, b=b: e.activation(out=acc[:, 512 * b:512 * b + 512], in_=PS[b][:, :], func=AF.Copy), [PS[b]], [acc])
                kb.op('dve', lambda e, b=b: e.tensor_copy(out=dacc[:, 512 * b:512 * b + 512], in_=PS[2 + b][:, :]), [PS[2 + b]], [dacc])
            first = [True, True]
            k2 = kt[i][1][:, :].rearrange("d (j p r) -> d j r p", p=128, r=4)
            q2 = qt[i][:, 1, :].rearrange("d (i r) -> d r i", r=4)
            for r in range(4):
                for j in range(3):
                    i0 = 0 if j < 2 else 128
                    N = 128 if j != 1 else 256
                    m0 = 128 if j == 0 else 0
                    tile_attn(k2[:, j, r, :], 128, q2[:, r, i0:i0 + N], N, vt2[i][:, r, j, :], 9 + r * 3 + j, m0, r * 256 + i0, first)
            for (A, Pb) in ((acc, 0), (dacc, 2)):
                Av = A[:, :].rearrange("p (i r) -> p r i", r=4)
                for b in range(2):
                    kb.op('dve', lambda e, b=b, Av=Av, Pb=Pb: e.tensor_tensor(out=Av[:, 2 * b:2 * b + 2, :], in0=Av[:, 2 * b:2 * b + 2, :], in1=PS[Pb + b][:, :].rearrange("p (r i) -> p r i", r=2), op=ALU.add), [A, PS[Pb + b]], [A])
            first = [True, True]
            k3 = kt[i][2][:, :].rearrange("d (m r) -> d r m", r=16)
            q3 = qt[i][:, 2, :].rearrange("d (i r) -> d r i", r=16)
            for r in range(16):
                tile_attn(k3[:, r, 0:128], 128, q3[:, r, :], 64, vt3[i][:, r, 0, :], 21 + 2 * r, 128, r * 64, first)
                tile_attn(k3[:, r, 128:192], 64, q3[:, r, :], 64, vt3[i][0:64, r, 1, :], 21 + 2 * r + 1, 0, r * 64, first)
            for (A, Pb) in ((acc, 0), (dacc, 2)):
                Av = A[:, :].rearrange("p (i r) -> p r i", r=16)
                for b in range(2):
                    kb.op('dve', lambda e, b=b, Av=Av, Pb=Pb: e.tensor_tensor(out=Av[:, 8 * b:8 * b + 8, :], in0=Av[:, 8 * b:8 * b + 8, :], in1=PS[Pb + b][:, :].rearrange("p (r i) -> p r i", r=8), op=ALU.add), [A, PS[Pb + b]], [A])
            kb.op('dve', lambda e: e.reciprocal(out=dacc[:, :], in_=dacc[:, :]), [dacc], [dacc])
            kb.op('dve', lambda e: e.tensor_tensor(out=attn_oT[:, h, 0:NT], in0=acc[:, :], in1=dacc[:, :], op=ALU.mult), [acc, dacc], [attn_oT])
            po, pd = PS[7], PS[7]
            firsts = [True]
            tiles = []
            for g, W in enumerate(GH):
                for j in range(W // 128):
                    tiles.append((g, j))
            ti = 0
            for (g, j) in tiles:
                c = ac['c'] % 3; ac['c'] += 1
                kb.dma('sp', ck[c][:], cache_d[g][128 * j:128 * j + 128, 0, h, :], [], [ck[c]], prim=ck[c])
                kb.dma('sp', cv[c][:], cache_d[g][128 * j:128 * j + 128, 1, h, :], [], [cv[c]], prim=cv[c])
                pt = PS[4 + ac['s'] % 3]; ac['s'] += 1
                kb.op('pe', lambda e, c=c, pt=pt: e.transpose(pt[:, 0:128], ck[c][:], ident[:]), [ck[c], ident], [pt])
                kb.op('act', lambda e, c=c, pt=pt: e.activation(out=ckT[c][:], in_=pt[:, 0:128], func=AF.Copy), [pt], [ckT[c]])
                kb.op('dve', lambda e, c=c: e.tensor_copy(out=cvb[c][:], in_=cv[c][:]), [cv[c]], [cvb[c]])
                sp_ = PS[4 + ac['s'] % 3]; Ei = Es[ac['s'] % 3]; Pi = Psm[ac['s'] % 3]; ac['s'] += 1
                kb.op('pe', lambda e, c=c, sp_=sp_, g=g: e.matmul(sp_[:, 0:NS], ckT[c][:], qs[i][:, g, :], start=True, stop=True), [ckT[c], qs[i]], [sp_])
                kb.op('act', lambda e, sp_=sp_, Ei=Ei: e.activation(out=Ei[:, :], in_=sp_[:, 0:NS], func=AF.Exp, scale=SCALE), [sp_], [Ei])
                kb.op('dve', lambda e, Ei=Ei, Pi=Pi, ti=ti: e.tensor_tensor(out=Pi[:, :], in0=Ei[:, :], in1=SMc[:, ti, :], op=ALU.mult), [Ei, SMc], [Pi])
                st = firsts[0]; firsts[0] = False
                kb.op('pe', lambda e, c=c, Pi=Pi, st=st: e.matmul(PS[7][:, 0:NS], cvb[c][:], Pi[:, :], start=st, stop=True, skip_group_check=True), [cvb[c], Pi], [PS[7]])
                kb.op('pe', lambda e, Pi=Pi, st=st: e.matmul(PS[7][:, 64:64 + NS], onesb[:, :], Pi[:, :], start=False, stop=True, skip_group_check=True), [onesb, Pi], [PS[7]])
                ti += 1
            for g in range(3):
                sp_ = PS[4 + ac['s'] % 3]; Ei = Es[ac['s'] % 3]; Pi = Psm[ac['s'] % 3]; ac['s'] += 1
                kb.op('pe', lambda e, sp_=sp_, g=g: e.matmul(sp_[0:NS, 0:NS], ksn[i][:, g, :], qs[i][:, g, :], start=True, stop=True), [ksn[i], qs[i]], [sp_])
                kb.op('act', lambda e, sp_=sp_, Ei=Ei: e.activation(out=Ei[0:NS, :], in_=sp_[0:NS, 0:NS], func=AF.Exp, scale=SCALE), [sp_], [Ei])
                kb.op('dve', lambda e, Ei=Ei, Pi=Pi, g=g: e.tensor_tensor(out=Pi[0:NS, :], in0=Ei[0:NS, :], in1=SMn[:, g, :], op=ALU.mult), [Ei, SMn], [Pi])
                kb.op('pe', lambda e, Pi=Pi, g=g: e.matmul(PS[7][:, 0:NS], vsn[i][:, g, :], Pi[0:NS, :], start=False, stop=True, skip_group_check=True), [vsn[i], Pi], [PS[7]])
                kb.op('pe', lambda e, Pi=Pi: e.matmul(PS[7][:, 64:64 + NS], onesb[0:NS, :], Pi[0:NS, :], start=False, stop=True, skip_group_check=True), [onesb, Pi], [PS[7]])
            kb.op('dve', lambda e: e.reciprocal(out=dsn[:, :], in_=PS[7][:, 64:64 + NS]), [PS[7]], [dsn])
            kb.op('dve', lambda e: e.tensor_tensor(out=attn_oT[:, h, NT:NTS], in0=PS[7][:, 0:NS], in1=dsn[:, :], op=ALU.mult), [PS[7], dsn], [attn_oT])
        if os.environ.get('KDUMPA'):
            adbg = kb.dram("attn_dbg", [128, 8, NTS], BF16, "ExternalOutput")
            kb.dma('pool', adbg[:], attn_oT[:], [attn_oT], [adbg], prim=attn_oT)
        kb.barrier()
        ph.close()


    conv_fT = kb.sb("conv_fT", [128, 12, NTS], BF16, mid)
    x1_d = kb.dram("x1_d", [NT + 128, D])
    if DBG in ('', 'CONV'):
        ph = ExitStack()
        wdw = kb.sb("wdw", [128, 12, 31], F32, ph); kb.dma('sp', wdw[:], w_dwT[:], [], [wdw], prim=wdw)
        cpar = kb.sb("cpar", [128, 3, 12], F32, ph); kb.dma('sp', cpar[:], cparT[:], [], [cpar], prim=cpar)
        hprev = kb.sb("hprev", [128, 1], F32, ph); kb.dma('sp', hprev[:], hprev_d[:], [], [hprev], prim=hprev)
        onesf = kb.sb("onesf", [128, 128], F32, ph); kb.op('dve', lambda e: e.memset(onesf[:], 1.0), [], [onesf])
        yc = kb.sb("yc", [128, 12, NTS], F32, ph)
        ub = [kb.sb("ub%d" % i, [128, 30 + NT], F32, ph) for i in range(2)]
        ubs = [kb.sb("ubs%d" % i, [128, 30 + NS], F32, ph) for i in range(2)]
        cpo = kb.sb("cpo", [32, 1536], F32, ph); cso = kb.sb("cso", [NS, 1536], F32, ph)
        sq = [kb.sb("sq%d" % i, [128, 512], F32, ph) for i in range(2)]
        mu = kb.sb("mu", [128, NTS], F32, ph); rsd = kb.sb("rsd", [128, NTS], F32, ph); tmpc = [kb.sb("tmpc%d" % i, [128, 512], F32, ph) for i in range(2)]
        kb.dma('pool', convs_o[0:22, :], st_d[8:30, :], [st_d], [convs_o], prim=hprev)
        for j in range(12):
            u, us_ = ub[j % 2], ubs[j % 2]
            kb.dma('sp', u[:, :], u_d[j * 128:(j + 1) * 128, 98:128 + NT], [u_d], [u], prim=u)
            kb.dma('sp', us_[:, 0:30], stT_d[j * 128:(j + 1) * 128, :], [stT_d], [us_], prim=us_)
            kb.dma('sp', us_[:, 30:30 + NS], us_d[j * 128:(j + 1) * 128, :], [us_d], [us_], prim=us_)
            kb.op('dve', lambda e, u=u: e.tensor_scalar(out=u[:, 0:30], in0=u[:, 0:30], scalar1=hprev[:, 0:1], scalar2=None, op0=ALU.mult), [u, hprev], [u])
            for (src, L, c0) in ((u, NT, 0), (us_, NS, NT)):
                kb.op('dve', lambda e, src=src, L=L, c0=c0: e.tensor_scalar(out=yc[:, j, c0:c0 + L], in0=src[:, 0:L], scalar1=wdw[:, j, 0:1], scalar2=cpar[:, 0, j:j + 1], op0=ALU.mult, op1=ALU.add), [src, wdw, cpar], [yc])
                for k in range(1, 31):
                    kb.op('dve', lambda e, src=src, L=L, c0=c0, k=k: e.scalar_tensor_tensor(out=yc[:, j, c0:c0 + L], in0=src[:, k:k + L], scalar=wdw[:, j, k:k + 1], in1=yc[:, j, c0:c0 + L], op0=ALU.mult, op1=ALU.add), [src, wdw], [yc])
            pt = PS[4 + j % 2]
            kb.op('pe', lambda e, pt=pt, u=u: e.transpose(pt[0:32, 0:128], u[:, 30 + NT - 32:30 + NT], ident[:]), [u, ident], [pt])
            kb.op('act', lambda e, pt=pt: e.activation(out=cpo[:, j * 128:(j + 1) * 128], in_=pt[0:32, 0:128], func=AF.Copy), [pt], [cpo])
            pt2 = PS[6 + j % 2]
            kb.op('pe', lambda e, pt2=pt2, us_=us_: e.transpose(pt2[0:NS, 0:128], us_[:, 30:30 + NS], ident[:]), [us_, ident], [pt2])
            kb.op('act', lambda e, pt2=pt2: e.activation(out=cso[:, j * 128:(j + 1) * 128], in_=pt2[0:NS, 0:128], func=AF.Copy), [pt2], [cso])
        kb.dma('pool', convp_o[:, :], cpo[2:32, :], [cpo], [convp_o], prim=cpo)
        kb.dma('pool', convs_o[22:30, :], cso[:, :], [cso], [convs_o], prim=cso)
        for (c0, n) in ((0, 512), (512, 512), (NT, NS)):
            p1, p2 = PS[0], PS[1]
            for j in range(12):
                s_ = sq[j % 2]
                kb.op('act', lambda e, s_=s_: e.activation(out=s_[:, 0:n], in_=yc[:, j, c0:c0 + n], func=AF.Square), [yc], [s_])
                kb.op('pe', lambda e: e.matmul(p1[:, 0:n], onesf[:, :], yc[:, j, c0:c0 + n], start=(j == 0), stop=(j == 11)), [onesf, yc], [p1])
                kb.op('pe', lambda e, s_=s_: e.matmul(p2[:, 0:n], onesf[:, :], s_[:, 0:n], start=(j == 0), stop=(j == 11)), [onesf, s_], [p2])
            t_ = tmpc[0]
            kb.op('dve', lambda e: e.tensor_scalar(out=mu[:, c0:c0 + n], in0=p1[:, 0:n], scalar1=1.0 / 1536, scalar2=None, op0=ALU.mult), [p1], [mu])
            kb.op('dve', lambda e: e.tensor_tensor(out=t_[:, 0:n], in0=mu[:, c0:c0 + n], in1=mu[:, c0:c0 + n], op=ALU.mult), [mu], [t_])
            kb.op('dve', lambda e: e.scalar_tensor_tensor(out=t_[:, 0:n], in0=p2[:, 0:n], scalar=1.0 / 1536, in1=t_[:, 0:n], op0=ALU.mult, op1=ALU.subtract), [p2, t_], [t_])
            kb.op('dve', lambda e: e.tensor_scalar(out=t_[:, 0:n], in0=t_[:, 0:n], scalar1=1e-6, scalar2=None, op0=ALU.add), [t_], [t_])
            kb.op('act', lambda e: e.activation(out=t_[:, 0:n], in_=t_[:, 0:n], func=AF.Sqrt), [t_], [t_])
            kb.op('dve', lambda e: e.reciprocal(out=rsd[:, c0:c0 + n], in_=t_[:, 0:n]), [t_], [rsd])
            for j in range(12):
                t2 = tmpc[1]
                kb.op('dve', lambda e, t2=t2: e.tensor_tensor(out=t2[:, 0:n], in0=yc[:, j, c0:c0 + n], in1=mu[:, c0:c0 + n], op=ALU.subtract), [yc, mu], [t2])
                kb.op('pool', lambda e, t2=t2: e.tensor_tensor(out=t2[:, 0:n], in0=t2[:, 0:n], in1=rsd[:, c0:c0 + n], op=ALU.mult), [t2, rsd], [t2])
                kb.op('dve', lambda e, t2=t2: e.tensor_scalar(out=t2[:, 0:n], in0=t2[:, 0:n], scalar1=cpar[:, 1, j:j + 1], scalar2=cpar[:, 2, j:j + 1], op0=ALU.mult, op1=ALU.add), [t2, cpar], [t2])
                kb.op('act', lambda e, t2=t2: e.activation(out=conv_fT[:, j, c0:c0 + n], in_=t2[:, 0:n], func=AF.Silu), [t2], [conv_fT])
        kb.barrier()
        ph.close()

    if DBG in ('', 'CONV'):
        ph = ExitStack()
        sT = kb.sb("sT", [128, 16, NTS], BF16, ph)
        bo = kb.sb("bo", [128, 2, 16], F32, ph); kb.dma('sp', bo[:], boT[:], [], [bo], prim=bo)
        stg = [kb.sb("stg%d" % i, [128, 16, 256], F32, ph) for i in range(2)]
        wbs = [[kb.sb("wb%d_%d" % (i, j), [128, KSPL[j][1] - KSPL[j][0], 256], BF16, ph) for j in range(3)] for i in range(2)]
        sga = [kb.sb("sga%d" % i, [128, 512], F32, ph) for i in range(2)]; sgb = [kb.sb("sgb%d" % i, [128, 512], F32, ph) for i in range(2)]
        t1 = [kb.sb("t1_%d" % i, [128, 512], F32, ph) for i in range(2)]; t2_ = [kb.sb("t2_%d" % i, [128, 512], F32, ph) for i in range(2)]
        mc = {'i': 0}
        seq = []
        for cb in range(8):
            seq.append(('a', cb)); seq.append(('c', cb))

        def issue(idx):
            kind, cb = seq[idx]
            st = stg[idx % 2]
            if kind == 'a':
                kb.dma('sp', st[:, 0:8, :], w_ao[:, cb * 256:(cb + 1) * 256].rearrange("(k p) n -> p k n", p=128), [], [st], prim=st)
            else:
                kb.dma('sp', st[:, 0:12, :], w_co[:, cb * 256:(cb + 1) * 256].rearrange("(k p) n -> p k n", p=128), [], [st], prim=st)
        issue(0)
        pa_t = {}
        for idx in range(len(seq)):
            if idx + 1 < len(seq):
                issue(idx + 1)
            kind, cb = seq[idx]
            st, wb3 = stg[idx % 2], wbs[idx % 2]
            nk = 8 if kind == 'a' else 12
            cast_block(st, wb3, nk=nk)
            src = attn_oT if kind == 'a' else conv_fT
            for sub in range(2):
                jb = cb * 2 + sub
                for ci, (c0, n) in enumerate(((0, 512), (512, 512), (NT, NS))):
                    if kind == 'a':
                        p = PS[(sub * 3 + ci) % 6]
                    else:
                        p = PS[6 + (sub * 3 + ci) % 2]
                    for k in range(nk):
                        wp = wpart(wb3, k)
                        kb.op('pe', lambda e, k=k, wp=wp, p=p: e.matmul(p[:, 0:n], wp[:, k, sub * 128:(sub + 1) * 128], src[:, k, c0:c0 + n], start=(k == 0), stop=(k == nk - 1)), [wp.t, src], [p])
                    if kind == 'a':
                        pa_t[(sub, ci)] = p
                    else:
                        m = mc['i'] % 2; mc['i'] += 1
                        pa = pa_t[(sub, ci)]
                        kb.dma('sp', sga[m][:, 0:n], sg_d[jb * 128:(jb + 1) * 128, c0:c0 + n], [sg_d], [sga[m]], prim=sga[m])
                        kb.dma('sp', sgb[m][:, 0:n], sg_d[2048 + jb * 128:2048 + (jb + 1) * 128, c0:c0 + n], [sg_d], [sgb[m]], prim=sgb[m])
                        kb.op('dve', lambda e, pa=pa, m=m: e.scalar_tensor_tensor(out=t1[m][:, 0:n], in0=pa[:, 0:n], scalar=bo[:, 0, jb:jb + 1], in1=sga[m][:, 0:n], op0=ALU.add, op1=ALU.mult), [pa, bo, sga[m]], [t1[m]])
                        kb.op('dve', lambda e, p=p, m=m: e.scalar_tensor_tensor(out=t2_[m][:, 0:n], in0=p[:, 0:n], scalar=bo[:, 1, jb:jb + 1], in1=sgb[m][:, 0:n], op0=ALU.add, op1=ALU.mult), [p, bo, sgb[m]], [t2_[m]])
                        kb.op('pool', lambda e, m=m: e.tensor_tensor(out=sT[:, jb, c0:c0 + n], in0=t1[m][:, 0:n], in1=t2_[m][:, 0:n], op=ALU.add), [t1[m], t2_[m]], [sT])
        g1bc = kb.sb("g1bc", [128, D], F32, ph); g1bs = kb.sb("g1bs", [NS, D], F32, ph)
        kb.dma('pool', g1bc[:], mod_d[0:1, 2 * D:3 * D].partition_broadcast(128), [mod_d], [g1bc], prim=g1bc)
        kb.dma('pool', g1bs[:], mod_d[1:2, 2 * D:3 * D].partition_broadcast(NS), [mod_d], [g1bs], prim=g1bs)
        xp_ = [kb.sb("xp%d" % i, [128, 256], F32, ph) for i in range(3)]
        xo_ = [kb.sb("xo%d" % i, [128, 256], F32, ph) for i in range(3)]
        wo_i = {'i': 0}

        def issue_o(cb):
            st = stg[cb % 2]
            kb.dma('sp', st[:, :, :], w_o[:, cb * 256:(cb + 1) * 256].rearrange("(k p) n -> p k n", p=128), [], [st], prim=st)
        issue_o(0)
        for cb in range(8):
            if cb + 1 < 8:
                issue_o(cb + 1)
            st, wb3 = stg[cb % 2], wbs[cb % 2]
            cast_block(st, wb3)
            for t in range(9):
                rows = 128 if t < 8 else NS
                tok0 = t * 128
                p = PS[t % 6]
                for k in range(16):
                    wp = wpart(wb3, k)
                    kb.op('pe', lambda e, k=k, wp=wp, p=p: e.matmul(p[0:rows, 0:256], sT[:, k, tok0:tok0 + rows], wp[:, k, :], start=(k == 0), stop=(k == 15)), [wp.t, sT], [p])
                m = wo_i['i'] % 3; wo_i['i'] += 1
                xsrc = xh[HALO + tok0:HALO + tok0 + rows, cb * 256:(cb + 1) * 256] if t < 8 else xs[:, cb * 256:(cb + 1) * 256]
                gb = g1bc if t < 8 else g1bs
                kb.dma('sp', xp_[m][0:rows, :], xsrc, [], [xp_[m]], prim=xp_[m])
                kb.op('dve', lambda e, p=p, m=m, gb=gb: e.tensor_tensor(out=xo_[m][0:rows, :], in0=p[0:rows, 0:256], in1=gb[0:rows, cb * 256:(cb + 1) * 256], op=ALU.mult), [p, gb], [xo_[m]])
                kb.op('pool', lambda e, m=m: e.tensor_tensor(out=xo_[m][0:rows, :], in0=xo_[m][0:rows, :], in1=xp_[m][0:rows, :], op=ALU.add), [xo_[m], xp_[m]], [xo_[m]])
                kb.dma('pool', x1_d[tok0:tok0 + rows, cb * 256:(cb + 1) * 256], xo_[m][0:rows, :], [xo_[m]], [x1_d], prim=xo_[m])
        kb.barrier()
        ph.close()


    mid.close()
    AX = mybir.AxisListType.X
    NE = int(os.environ.get('KNE', '32'))
    late = ExitStack()
    h2T = kb.sb("h2T", [128, 16, NTS], BF16, late)
    gateM = kb.sb("gateM", [128, 9, 32], F32, late)
    kb.op('dve', lambda e: e.memset(gateM[:], 0.0), [], [gateM])
    ph = ExitStack()
    A2 = [kb.sb("A2_%d" % r, [128 if r == 0 else NS, D], F32, ph) for r in range(2)]
    B2 = [kb.sb("B2_%d" % r, [128 if r == 0 else NS, D], F32, ph) for r in range(2)]
    g2bc = kb.sb("g2bc", [128, D], F32, ph)
    kb.dma('pool', g2bc[:], g2_row[:, :].partition_broadcast(128), [], [g2bc], prim=g2bc)
    for r in range(2):
        n = 128 if r == 0 else NS
        kb.dma('pool', A2[r][:], mod_d[r:r + 1, 4 * D:5 * D].partition_broadcast(n), [mod_d], [A2[r]], prim=A2[r])
        kb.dma('pool', B2[r][:], mod_d[r:r + 1, 3 * D:4 * D].partition_broadcast(n), [mod_d], [B2[r]], prim=B2[r])
        kb.op('dve', lambda e, r=r, n=n: e.scalar_tensor_tensor(out=A2[r][:], in0=A2[r][:], scalar=1.0, in1=g2bc[0:n, :], op0=ALU.add, op1=ALU.mult), [A2[r], g2bc], [A2[r]])
    wr = kb.sb("wr", [128, 16, 32], F32, ph); kb.dma('sp', wr[:], w_router[:, :].rearrange("(k p) e -> p k e", p=128), [], [wr], prim=wr)
    brbc = kb.sb("brbc", [128, 32], F32, ph); kb.dma('pool', brbc[:], b_router[:, :].partition_broadcast(128), [], [brbc], prim=brbc)
    x1t = [kb.sb("x1t%d" % i, [128, D], F32, ph) for i in range(2)]
    hn = [kb.sb("hn%d" % i, [128, D], F32, ph) for i in range(2)]
    h2f = [kb.sb("h2f%d" % i, [128, 16, 128], F32, ph) for i in range(2)]
    junk2 = kb.sb("junk2", [128, D], BF16, ph)
    sm = {n: kb.sb("sm_" + n, [128, w], F32, ph) for n, w in (("ss", 1), ("rs", 1), ("lg", 32), ("mx", 8), ("mk", 32), ("nm", 1), ("ex", 32), ("su", 1))}
    for t in range(9):
        rows = 128 if t < 8 else NS
        tok0 = t * 128
        r = 0 if t < 8 else 1
        x, h_, hf = x1t[t % 2], hn[t % 2], h2f[t % 2]
        ss, rs = sm["ss"], sm["rs"]
        kb.dma('sp', x[0:rows, :], x1_d[tok0:tok0 + rows, :], [x1_d], [x], prim=x)
        kb.op('act', lambda e: e.activation(out=junk2[0:rows, :], in_=x[0:rows, :], func=AF.Square, accum_out=ss[0:rows, :]), [x], [junk2, ss])
        kb.op('dve', lambda e: e.tensor_scalar(out=ss[0:rows, :], in0=ss[0:rows, :], scalar1=1.0 / D, scalar2=1e-6, op0=ALU.mult, op1=ALU.add), [ss], [ss])
        kb.op('act', lambda e: e.activation(out=ss[0:rows, :], in_=ss[0:rows, :], func=AF.Sqrt), [ss], [ss])
        kb.op('dve', lambda e: e.reciprocal(out=rs[0:rows, :], in_=ss[0:rows, :]), [ss], [rs])
        kb.op('act', lambda e: e.activation(out=h_[0:rows, :], in_=x[0:rows, :], func=AF.Copy, scale=rs[0:rows, :]), [x, rs], [h_])
        kb.op('dve', lambda e: e.tensor_tensor(out=h_[0:rows, :], in0=h_[0:rows, :], in1=A2[r][0:rows, :], op=ALU.mult), [h_, A2[r]], [h_])
        kb.op('pool', lambda e: e.tensor_tensor(out=h_[0:rows, :], in0=h_[0:rows, :], in1=B2[r][0:rows, :], op=ALU.add), [h_, B2[r]], [h_])
        for kq in range(4):
            p = PS[kq]
            for kk in range(4):
                k = kq * 4 + kk
                kb.op('pe', lambda e, k=k, kk=kk, p=p: e.transpose(p[:, kk * 128:kk * 128 + rows], h_[0:rows, k * 128:(k + 1) * 128], ident[0:rows, 0:rows]), [h_, ident], [p])
            pv = p[:, :].rearrange("p (k t) -> p k t", k=4)
            kb.op('act', lambda e, pv=pv, kq=kq: e.activation(out=h2T[:, 4 * kq:4 * kq + 4, tok0:tok0 + rows], in_=pv[:, :, 0:rows], func=AF.Copy), [p], [h2T])
            kb.op('dve', lambda e, pv=pv, kq=kq: e.tensor_copy(out=hf[:, 4 * kq:4 * kq + 4, 0:rows], in_=pv[:, :, 0:rows]), [p], [hf])
        pr = PS[4 + t % 2]
        for k in range(16):
            kb.op('pe', lambda e, k=k: e.matmul(pr[0:rows, 0:32], hf[:, k, 0:rows], wr[:, k, :], start=(k == 0), stop=(k == 15)), [hf, wr], [pr])
        lg, mx, mk, nm, ex, su = sm["lg"], sm["mx"], sm["mk"], sm["nm"], sm["ex"], sm["su"]
        kb.op('dve', lambda e: e.tensor_tensor(out=lg[0:rows, :], in0=pr[0:rows, 0:32], in1=brbc[0:rows, :], op=ALU.add), [pr, brbc], [lg])
        kb.op('dve', lambda e: e.max(out=mx[0:rows, :], in_=lg[0:rows, :]), [lg], [mx])
        kb.op('dve', lambda e: e.tensor_scalar(out=mk[0:rows, :], in0=lg[0:rows, :], scalar1=mx[0:rows, 3:4], scalar2=None, op0=ALU.is_ge), [lg, mx], [mk])
        kb.op('dve', lambda e: e.tensor_scalar(out=nm[0:rows, :], in0=mx[0:rows, 0:1], scalar1=-1.0, scalar2=None, op0=ALU.mult), [mx], [nm])
        kb.op('act', lambda e: e.activation(out=ex[0:rows, :], in_=lg[0:rows, :], func=AF.Exp, bias=nm[0:rows, :]), [lg, nm], [ex])
        kb.op('dve', lambda e: e.tensor_tensor(out=ex[0:rows, :], in0=ex[0:rows, :], in1=mk[0:rows, :], op=ALU.mult), [ex, mk], [ex])
        kb.op('dve', lambda e: e.reduce_sum(out=su[0:rows, :], in_=ex[0:rows, :], axis=AX), [ex], [su])
        kb.op('dve', lambda e: e.reciprocal(out=su[0:rows, :], in_=su[0:rows, :]), [su], [su])
        kb.op('dve', lambda e: e.tensor_scalar(out=gateM[0:rows, t, :], in0=ex[0:rows, :], scalar1=su[0:rows, 0:1], scalar2=None, op0=ALU.mult), [ex, su], [gateM])
    kb.barrier()
    ph.close()

    late2 = ExitStack()
    accM = kb.sb("accM", [128, 9, D], F32, late2)
    ph = ExitStack()
    pi = ExitStack()
    b2 = kb.sb("b2", [32, D], F32, pi); kb.dma('sp', b2[:], b_moe2[:, :], [], [b2], prim=b2)
    gT = kb.sb("gT", [32, 128], F32, pi)
    for t in range(9):
        pt = PS[6]
        kb.op('pe', lambda e, t=t, pt=pt: e.transpose(pt[0:32, 0:128], gateM[:, t, :], ident[:]), [gateM, ident], [pt])
        kb.op('act', lambda e, pt=pt: e.activation(out=gT[:, :], in_=pt[0:32, 0:128], func=AF.Copy), [pt], [gT])
        for c4 in range(4):
            p = PS[c4]
            kb.op('pe', lambda e, p=p, c4=c4: e.matmul(p[:, :], gT[:, :], b2[:, c4 * 512:(c4 + 1) * 512], start=True, stop=True), [gT, b2], [p])
            kb.op('act', lambda e, p=p, c4=c4, t=t: e.activation(out=accM[:, t, c4 * 512:(c4 + 1) * 512], in_=p[:, :], func=AF.Copy), [p], [accM])
    kb.barrier()
    pi.close()
    actT = kb.sb("actT", [128, 8, NTS], BF16, ph)
    b1 = kb.sb("b1", [128, 32, 32], F32, ph); kb.dma('sp', b1[:], b1T_d[:], [], [b1], prim=b1)
    USPL = [(0, 3, 'act'), (3, 6, 'dve'), (6, 8, 'pool')]
    stu = [kb.sb("stu%d" % i, [128, 8, 512], F32, ph) for i in range(2)]
    wbu = [[kb.sb("wbu%d_%d" % (i, j), [128, USPL[j][1] - USPL[j][0], 512], BF16, ph) for j in range(3)] for i in range(4)]
    gsT = kb.sb("gsT", [128, 4, NTS], BF16, ph)
    gt = kb.sb("gt", [128, 512], F32, ph); st_ = kb.sb("st_", [128, 512], F32, ph); lt = kb.sb("lt", [128, 512], F32, ph)
    CH = ((0, 512), (512, 512), (NT, NS))
    units = []
    for ex_ in range(NE):
        for hf in range(2):
            for jq in range(2):
                c = hf * 1024 + jq * 512
                for kh in range(2):
                    units.append(w_moe1[ex_, kh * 1024:(kh + 1) * 1024, c:c + 512])
                for kh in range(2):
                    units.append(w_moe1[ex_, kh * 1024:(kh + 1) * 1024, 2048 + c:2048 + c + 512])
            for cb in range(4):
                units.append(w_moe2[ex_, hf * 1024:(hf + 1) * 1024, cb * 512:(cb + 1) * 512])
    us = {'issued': 0, 'cast': 0, 'p': 0}

    def u_issue():
        i = us['issued']
        if i < len(units):
            kb.dma('sp', stu[i % 2][:, :, :], units[i].rearrange("(k p) n -> p k n", p=128), [], [stu[i % 2]], prim=stu[i % 2])
            us['issued'] += 1

    def u_next():
        i = us['cast']; us['cast'] += 1
        while us['issued'] < min(i + 2, len(units)):
            u_issue()
        st, w3 = stu[i % 2], wbu[i % 4]
        for j, (k0, k1, en) in enumerate(USPL):
            if en == 'act':
                kb.op('act', lambda e, j=j, k0=k0, k1=k1: e.activation(out=w3[j][:, :, :], in_=st[:, k0:k1, :], func=AF.Copy), [st], [w3[j]])
            else:
                kb.op(en, lambda e, j=j, k0=k0, k1=k1: e.tensor_copy(out=w3[j][:, :, :], in_=st[:, k0:k1, :]), [st], [w3[j]])
        return w3

    def upart(w3, k):
        for j, (k0, k1, en) in enumerate(USPL):
            if k0 <= k < k1:
                return w3[j], k - k0

    def nps():
        us['p'] += 1
        return PS[us['p'] % 6]

    def w1_mm(U0, U1, sub, c0, n):
        p = nps()
        for k in range(16):
            tl, kl = upart(U0 if k < 8 else U1, k % 8)
            kb.op('pe', lambda e, k=k, tl=tl, kl=kl: e.matmul(p[:, 0:n], tl[:, kl, sub * 128:(sub + 1) * 128], h2T[:, k, c0:c0 + n], start=(k == 0), stop=(k == 15)), [tl, h2T], [p])
        return p

    u_issue()
    blocks = []
    for ex_ in range(NE):
        for hf in range(2):
            for jq in range(2):
                blocks.append(('g', ex_, hf, jq, 2)); blocks.append(('l', ex_, hf, jq, 2))
            for cb in range(4):
                blocks.append(('w', ex_, hf, cb, 1))
    pre = {}

    def ensure(bi):
        if bi < len(blocks) and bi not in pre:
            pre[bi] = [u_next() for _ in range(blocks[bi][4])]

    for bi, (kind, ex_, hf, x_, nu) in enumerate(blocks):
        ensure(bi)
        ensure(bi + 1)
        U = pre.pop(bi)
        if kind == 'g':
            jq = x_
            for sub in range(4):
                jb = hf * 8 + jq * 4 + sub
                for (c0, n) in CH:
                    p = w1_mm(U[0], U[1], sub, c0, n)
                    kb.op('dve', lambda e, p=p: e.tensor_scalar(out=gt[:, 0:n], in0=p[:, 0:n], scalar1=b1[:, ex_, jb:jb + 1], scalar2=7.0, op0=ALU.add, op1=ALU.min), [p, b1], [gt])
                    kb.op('act', lambda e: e.activation(out=st_[:, 0:n], in_=gt[:, 0:n], func=AF.Sigmoid, scale=1.702), [gt], [st_])
                    kb.op('pool', lambda e: e.tensor_tensor(out=gsT[:, sub, c0:c0 + n], in0=gt[:, 0:n], in1=st_[:, 0:n], op=ALU.mult), [gt, st_], [gsT])
        elif kind == 'l':
            jq = x_
            for sub in range(4):
                jb = hf * 8 + jq * 4 + sub
                for (c0, n) in CH:
                    p = w1_mm(U[0], U[1], sub, c0, n)
                    kb.op('dve', lambda e, p=p: e.tensor_scalar(out=lt[:, 0:n], in0=p[:, 0:n], scalar1=b1[:, ex_, 16 + jb:16 + jb + 1], scalar2=7.0, op0=ALU.add, op1=ALU.min), [p, b1], [lt])
                    kb.op('pool', lambda e: e.tensor_scalar(out=lt[:, 0:n], in0=lt[:, 0:n], scalar1=-7.0, scalar2=1.0, op0=ALU.max, op1=ALU.add), [lt], [lt])
                    kb.op('dve', lambda e: e.tensor_tensor(out=actT[:, jq * 4 + sub, c0:c0 + n], in0=gsT[:, sub, c0:c0 + n], in1=lt[:, 0:n], op=ALU.mult), [gsT, lt], [actT])
        else:
            cb = x_
            for t in range(9):
                rows = 128 if t < 8 else NS
                tok0 = t * 128
                p = nps()
                for k in range(8):
                    tl, kl = upart(U[0], k)
                    kb.op('pe', lambda e, k=k, tl=tl, kl=kl, p=p: e.matmul(p[0:rows, 0:512], actT[:, k, tok0:tok0 + rows], tl[:, kl, :], start=(k == 0), stop=(k == 7)), [tl, actT], [p])
                kb.op('dve', lambda e, p=p, t=t: e.scalar_tensor_tensor(out=accM[0:rows, t, cb * 512:(cb + 1) * 512], in0=p[0:rows, 0:512], scalar=gateM[0:rows, t, ex_:ex_ + 1], in1=accM[0:rows, t, cb * 512:(cb + 1) * 512], op0=ALU.mult, op1=ALU.add), [p, gateM, accM], [accM])
    kb.barrier()
    ph.close()

    ph = ExitStack()
    g5 = [kb.sb("g5_%d" % r, [128 if r == 0 else NS, D], F32, ph) for r in range(2)]
    gfbc = kb.sb("gfbc", [128, D], F32, ph)
    kb.dma('pool', gfbc[:], gf_row[:, :].partition_broadcast(128), [], [gfbc], prim=gfbc)
    for r in range(2):
        kb.dma('pool', g5[r][:], mod_d[r:r + 1, 5 * D:6 * D].partition_broadcast(128 if r == 0 else NS), [mod_d], [g5[r]], prim=g5[r])
    x1t = [kb.sb("x1f%d" % i, [128, D], F32, ph) for i in range(2)]
    yo = [kb.sb("yo%d" % i, [128, D], F32, ph) for i in range(2)]
    junk3 = kb.sb("junk3", [128, D], BF16, ph)
    ss2 = kb.sb("ss2", [128, 1], F32, ph); rs2 = kb.sb("rs2", [128, 1], F32, ph)
    for t in range(9):
        rows = 128 if t < 8 else NS
        tok0 = t * 128
        r = 0 if t < 8 else 1
        x, y = x1t[t % 2], yo[t % 2]
        kb.dma('sp', x[0:rows, :], x1_d[tok0:tok0 + rows, :], [x1_d], [x], prim=x)
        kb.op('dve', lambda e: e.tensor_tensor(out=y[0:rows, :], in0=accM[0:rows, t, :], in1=g5[r][0:rows, :], op=ALU.mult), [accM, g5[r]], [y])
        kb.op('pool', lambda e: e.tensor_tensor(out=y[0:rows, :], in0=y[0:rows, :], in1=x[0:rows, :], op=ALU.add), [y, x], [y])
        kb.op('act', lambda e: e.activation(out=junk3[0:rows, :], in_=y[0:rows, :], func=AF.Square, accum_out=ss2[0:rows, :]), [y], [junk3, ss2])
        kb.op('dve', lambda e: e.tensor_scalar(out=ss2[0:rows, :], in0=ss2[0:rows, :], scalar1=1.0 / D, scalar2=1e-6, op0=ALU.mult, op1=ALU.add), [ss2], [ss2])
        kb.op('act', lambda e: e.activation(out=ss2[0:rows, :], in_=ss2[0:rows, :], func=AF.Sqrt), [ss2], [ss2])
        kb.op('dve', lambda e: e.reciprocal(out=rs2[0:rows, :], in_=ss2[0:rows, :]), [ss2], [rs2])
        kb.op('act', lambda e: e.activation(out=y[0:rows, :], in_=y[0:rows, :], func=AF.Copy, scale=rs2[0:rows, :]), [y, rs2], [y])
        kb.op('dve', lambda e: e.tensor_tensor(out=y[0:rows, :], in0=y[0:rows, :], in1=gfbc[0:rows, :], op=ALU.mult), [y, gfbc], [y])
        if t < 8:
            kb.dma('pool', y_o[tok0:tok0 + 128, :], y[:, :], [y], [y_o], prim=y)
        else:
            kb.dma('pool', ys_o[:, :], y[0:NS, :], [y], [ys_o], prim=y)
    kb.finish()
    ph.close()
    late2.close()
    late.close()
    es.close()
    return nc


_NC = None
CORES = list(range(NCORES))


def kernel(**inp):
    global _NC
    f = lambda a: np.ascontiguousarray(np.asarray(a, dtype=np.float32))
    xp = f(inp['x_prompt']); xs = f(inp['x_sample'])
    shared = {
        "ident": np.eye(128, dtype=np.float32),
        "w_ada": f(inp['w_ada'][0]), "b_adaT": f(inp['b_ada'][0].reshape(96, 128).T), "b_ada_row": f(inp['b_ada'][0].reshape(1, -1)),
        "g1T": f(inp['g_norm1'][0].reshape(16, 128).T),
        "w_in": f(inp['w_in'][0]), "b_inT": f(inp['b_in'][0].reshape(128, 128).T), "b_in_row": f(inp['b_in'][0].reshape(1, -1)),
    }
    shared["w_dwT"] = f(inp['w_dw'][0].T.reshape(12, 128, 31).transpose(1, 0, 2))
    shared["cparT"] = f(np.stack([inp['b_dw'][0].reshape(12, 128).T, inp['g_ln_conv'][0].reshape(12, 128).T, inp['b_ln_conv'][0].reshape(12, 128).T], axis=1))
    shared["w_ao"] = f(inp['w_attn_out'][0]); shared["w_co"] = f(inp['w_conv_out'][0]); shared["w_o"] = f(inp['w_o'][0])
    shared["boT"] = f(np.stack([inp['b_attn_out'][0].reshape(16, 128).T, inp['b_conv_out'][0].reshape(16, 128).T], axis=1))
    shared["g2_row"] = f(inp['g_norm2'][0].reshape(1, -1)); shared["w_router"] = f(inp['w_router'][0]); shared["b_router"] = f(inp['b_router'][0].reshape(1, -1))
    shared["w_moe1"] = f(inp['w_moe1'][0]); shared["w_moe2"] = f(inp['w_moe2'][0]); shared["b_moe2"] = f(inp['b_moe2'][0]); shared["gf_row"] = f(inp['g_final'].reshape(1, -1))
    shared["b1T"] = f(inp['b_moe1'][0].reshape(32, 32, 128).transpose(2, 0, 1))
    pp = np.arange(128)[:, None]; nn = np.arange(256)[None, :]
    shared["M1"] = ((nn - pp >= 0) & (nn - pp <= 128)).astype(np.float32)
    SMc = np.zeros((128, 21, NS), np.float32); SMn = np.zeros((NS, 3, NS), np.float32)
    ti = 0
    for g, (W, dd) in enumerate(((128, 1), (512, 4), (2048, 16))):
        for j in range(W // 128):
            m = 128 * j + np.arange(128)[:, None]; ii = np.arange(NS)[None, :]
            SMc[:, ti, :] = (((ii - m) % dd == 0) & (m >= ii)).astype(np.float32)
            ti += 1
        i1 = np.arange(NS)[:, None]; ii = np.arange(NS)[None, :]
        SMn[:, g, :] = ((i1 <= ii) & ((ii - i1) % dd == 0)).astype(np.float32)
    shared["SMc"] = SMc; shared["SMn"] = SMn
    in_maps = []
    for c in CORES:
        b, q = c // 4, c % 4
        s = q * NT
        xh = np.zeros((HALO + NT, D), np.float32)
        lo = max(0, s - HALO)
        xh[HALO - (s - lo):] = xp[b, lo:s + NT]
        cT = np.stack([f(inp['c_prompt'][b]).reshape(16, 128).T, f(inp['c_sample'][c]).reshape(16, 128).T], axis=-1)
        m = dict(shared)
        kval = np.zeros((128, 53), np.float32)
        p1 = np.arange(128)
        for j in range(9):
            kval[:, j] = (s - 128 + 128 * j + p1 >= 0)
        for r in range(4):
            for j in range(3):
                kval[:, 9 + r * 3 + j] = (s - 512 + 512 * j + 4 * p1 + r >= 0)
        for r in range(16):
            kval[:, 21 + 2 * r] = (s - 2048 + 16 * p1 + r >= 0)
            kval[:, 21 + 2 * r + 1] = (s - 2048 + 16 * (128 + p1) + r >= 0)
        m.update({"xh": xh, "xs": f(xs[c]), "cT": f(cT), "kval": kval, "hprev": np.full((128, 1), 1.0 if q > 0 else 0.0, np.float32),
                  "stT": f(inp['state_conv'][0, c].T), "st": f(inp['state_conv'][0, c]),
                  "cache0": f(inp['cache_kv_w128'][0, c]), "cache1": f(inp['cache_kv_w512'][0, c]), "cache2": f(inp['cache_kv_w2048'][0, c])})
        in_maps.append(m)
    if _NC is None:
        _NC = build()
    res = run_bass_kernel_spmd(_NC, in_maps, core_ids=list(range(len(CORES)))).results
    B, S = 2, 4096
    y_p = np.zeros((B, S, D), np.float32); y_s = np.zeros((8, NS, D), np.float32)
    kvp = [np.zeros((1, B, w, 2, 8, 128), np.float32) for w in (128, 512, 2048)]
    kvs = [np.zeros((1, 8, NS, 2, 8, 128), np.float32) for _ in range(3)]
    conv_p = np.zeros((1, B, 30, 1536), np.float32); conv_s = np.zeros((1, 8, 30, 1536), np.float32)
    for ci, c in enumerate(CORES):
        b, q = c // 4, c % 4
        r = res[ci]
        y_p[b, q * NT:(q + 1) * NT] = r["y_o"]
        y_s[c] = r["ys_o"]
        if q == 3:
            kvp[0][0, b] = r["kv1_o"]; kvp[1][0, b] = r["kv2_o"]; conv_p[0, b] = r["convp_o"]
        if q >= 2:
            kvp[2][0, b, (q - 2) * NT:(q - 1) * NT] = r["kv3_o"]
        for g in range(3):
            kvs[g][0, c] = r["kvs_o"][g]
        conv_s[0, c] = r["convs_o"]
    return (y_p, y_s, kvp[0], kvp[1], kvp[2], conv_p, kvs[0], kvs[1], kvs[2], conv_s)
```

```python
import numpy as np
from contextlib import ExitStack
import concourse.bass as bass
import concourse.mybir as mybir
from concourse.bass_utils import run_bass_kernel_spmd

dt = mybir.dt
F32, BF16, I32, U32 = dt.float32, dt.bfloat16, dt.int32, dt.uint32
AF = mybir.ActivationFunctionType
ALU = mybir.AluOpType

D = 2048
NT = 1024
NS = 8
NTS = NT + NS
HALO = 2048
NCORES = 8
CAP = 256
import os
DBG = os.environ.get('KDBG', '')


class T:
    def __init__(self, h, name):
        self.h = h
        self.name = name
        self.w = {}
        self.r = {}
        self.dsem = {}
        self.excl = False
        self.nowaw = False

    def __getitem__(self, k):
        return self.h[k]


class KB:
    def __init__(self, nc, es):
        self.nc = nc
        self.es = es
        self.eng = dict(pe=nc.tensor, act=nc.scalar, dve=nc.vector, pool=nc.gpsimd, sp=nc.sync)
        self.sems = []
        self.semcnt = []
        self.esem = {}
        for k in self.eng:
            self.esem[k] = self.new_sem('e_' + k)
        self.cnt = {k: 0 for k in self.eng}
        self.waited = {k: {} for k in self.eng}
        self.dfree = {'sw': [self.new_sem('dsw%d' % i) for i in range(48)], 'hw': [self.new_sem('dhw%d' % i) for i in range(44)]}
        self.dused = []
        self.uid = 0

    def new_sem(self, name):
        s = self.es.enter_context(self.nc.semaphore(name))
        self.sems.append(s)
        self.semcnt.append(0)
        return len(self.sems) - 1

    def sb(self, name, shape, dtype=F32, es=None):
        self.uid += 1
        es = es or self.es
        return T(es.enter_context(self.nc.sbuf_tensor("%s_%d" % (name, self.uid), list(shape), dtype)), name)

    def ps(self, name, shape, dtype=F32, es=None):
        self.uid += 1
        es = es or self.es
        t = T(es.enter_context(self.nc.psum_tensor("%s_%d" % (name, self.uid), list(shape), dtype)), name)
        t.excl = True
        return t

    def dram(self, name, shape, dtype=F32, kind="Internal"):
        t = T(self.nc.dram_tensor(name, list(shape), dtype, kind=kind).ap(), name)
        t.nowaw = True
        return t

    def _wait(self, e, deps, skip=None):
        wd = self.waited[e]
        for si, v in deps.items():
            if si == skip or wd.get(si, 0) >= v:
                continue
            self.eng[e].wait_ge(self.sems[si], v)
            wd[si] = v

    @staticmethod
    def _merge(d, o):
        for k, v in o.items():
            if d.get(k, 0) < v:
                d[k] = v

    def _deps(self, reads, writes):
        deps = {}
        for t in reads:
            self._merge(deps, t.w)
            if t.excl:
                self._merge(deps, t.r)
        for t in writes:
            if not t.nowaw:
                self._merge(deps, t.w)
            self._merge(deps, t.r)
        return deps

    def _post(self, ev, reads, writes):
        for t in writes:
            if t.nowaw:
                self._merge(t.w, ev)
            else:
                t.w = dict(ev)
            t.r = {}
        for t in reads:
            if t not in writes:
                self._merge(t.r, ev)

    def op(self, e, fn, reads=(), writes=()):
        si = self.esem[e]
        self._wait(e, self._deps(reads, writes), skip=si if e == 'pe' else None)
        inst = fn(self.eng[e])
        self.cnt[e] += 1
        self.semcnt[si] = self.cnt[e]
        inst.then_inc(self.sems[si], 1)
        self._post({si: self.cnt[e]}, reads, writes)
        return inst

    def dma(self, q, out, in_, reads=(), writes=(), prim=None, fn=None):
        kind = 'sw' if q == 'pool' else 'hw'
        if kind not in prim.dsem:
            prim.dsem[kind] = self.dfree[kind].pop()
            if prim not in self.dused:
                self.dused.append(prim)
        si = prim.dsem[kind]
        self._wait(q, self._deps(reads, writes), skip=si if prim in writes else None)
        inst = self.eng[q].dma_start(out=out, in_=in_) if fn is None else fn(self.eng[q])
        self.semcnt[si] += 16
        inst.then_inc(self.sems[si], 16)
        self._post({si: self.semcnt[si]}, reads, writes)
        return inst

    def barrier(self, keep=()):
        allv = {si: v for si, v in enumerate(self.semcnt) if v > 0}
        for e in self.eng:
            self._wait(e, allv, skip=self.esem[e])
        rest = []
        for t in self.dused:
            if t in keep:
                rest.append(t)
            else:
                for kind, si in t.dsem.items():
                    self.dfree[kind].append(si)
                t.dsem = {}
        self.dused = rest

    def finish(self):
        allv = {si: v for si, v in enumerate(self.semcnt) if v > 0}
        for e in self.eng:
            self._wait(e, allv, skip=self.esem[e])


def build():
    nc = bass.Bass("TRN2", target_bir_lowering=False)
    es = ExitStack()
    kb = KB(nc, es)
    IN = lambda n, s, d=F32: kb.dram(n, s, d, "ExternalInput")
    OUT = lambda n, s, d=F32: kb.dram(n, s, d, "ExternalOutput")
    xh = IN("xh", [HALO + NT, D]); xs = IN("xs", [NS, D]); cT = IN("cT", [128, 16, 2])
    ident_d = IN("ident", [128, 128])
    w_ada = IN("w_ada", [D, 6 * D]); b_adaT = IN("b_adaT", [128, 96]); b_ada_row = IN("b_ada_row", [1, 6 * D])
    g1T = IN("g1T", [128, 16])
    M1_d = IN("M1", [128, 256]); kval_d = IN("kval", [128, 53]); SMc_d = IN("SMc", [128, 21, NS]); SMn_d = IN("SMn", [NS, 3, NS])
    cache_d = [IN("cache%d" % g, [w, 2, 8, 128]) for g, w in enumerate((128, 512, 2048))]
    w_dwT = IN("w_dwT", [128, 12, 31]); cparT = IN("cparT", [128, 3, 12]); hprev_d = IN("hprev", [128, 1])
    stT_d = IN("stT", [1536, 30]); st_d = IN("st", [30, 1536])
    w_ao = IN("w_ao", [1024, D]); w_co = IN("w_co", [1536, D]); boT = IN("boT", [128, 2, 16]); w_o = IN("w_o", [D, D])
    g2_row = IN("g2_row", [1, D]); w_router = IN("w_router", [D, 32]); b_router = IN("b_router", [1, 32])
    w_moe1 = IN("w_moe1", [32, D, 2 * D]); b1T_d = IN("b1T", [128, 32, 32]); w_moe2 = IN("w_moe2", [32, D, D]); b_moe2 = IN("b_moe2", [32, D]); gf_row = IN("gf_row", [1, D])
    w_in = IN("w_in", [D, 16384]); b_inT = IN("b_inT", [128, 128]); b_in_row = IN("b_in_row", [1, 16384])
    y_o = OUT("y_o", [NT, D]); ys_o = OUT("ys_o", [NS, D])
    kv_o = [OUT("kv1_o", [128, 2, 8, 128]), OUT("kv2_o", [512, 2, 8, 128]), OUT("kv3_o", [1024, 2, 8, 128])]
    kvs_o = OUT("kvs_o", [3, NS, 2, 8, 128])
    convp_o = OUT("convp_o", [30, 1536]); convs_o = OUT("convs_o", [30, 1536])
    GH = [128, 512, 2048]
    NCTX = [GH[g] + NT for g in range(3)]
    kT_d = [kb.dram("kT_d%d" % g, [8, 128, NCTX[g]], BF16) for g in range(3)]
    V_d = [kb.dram("V_d%d" % g, [NCTX[g], 8, 128], BF16) for g in range(3)]
    qT_d = kb.dram("qT_d", [3, 8, 128, NT], BF16)
    qsT_d = kb.dram("qsT_d", [3, 8, 128, NS], BF16); ksT_d = kb.dram("ksT_d", [3, 8, 128, NS], BF16)
    Vs_d = kb.dram("Vs_d", [3, NS, 8, 128], BF16)
    u_d = kb.dram("u_d", [1536, 128 + NT]); us_d = kb.dram("us_d", [1536, NS])
    sg_d = kb.dram("sg_d", [4096, NTS])
    mod_d = kb.dram("mod_d", [2, 6 * D])

    ident = kb.sb("ident", [128, 128]); kb.dma('sp', ident[:], ident_d[:], [ident_d], [ident], prim=ident)
    identb = kb.sb("identb", [128, 128], BF16)
    kb.op('dve', lambda e: e.tensor_copy(out=identb[:], in_=ident[:]), [ident], [identb])
    A1 = kb.sb("A1", [128, 16, 2]); B1 = kb.sb("B1", [128, 16, 2])
    PS = [kb.ps("ps%d" % i, [128, 512]) for i in range(8)]

    ph = ExitStack()
    sil = kb.sb("sil", [128, 16, 2], F32, ph); silb = kb.sb("silb", [128, 16, 2], BF16, ph)
    badT = kb.sb("badT", [128, 96], F32, ph); g1s = kb.sb("g1s", [128, 16], F32, ph)
    modT = kb.sb("modT", [128, 32, 2], F32, ph)
    kb.dma('sp', sil[:], cT[:], [cT], [sil], prim=sil)
    kb.dma('sp', badT[:], b_adaT[:], [b_adaT], [badT], prim=badT)
    kb.dma('sp', g1s[:], g1T[:], [g1T], [g1s], prim=g1s)
    kb.op('act', lambda e: e.activation(out=silb[:], in_=sil[:], func=AF.Silu), [sil], [silb])
    stg = [kb.sb("stg%d" % i, [128, 16, 256], F32, ph) for i in range(2)]
    KSPL = [(0, 6, 'act'), (6, 12, 'dve'), (12, 16, 'pool')]
    wbs = [[kb.sb("wb%d_%d" % (i, j), [128, KSPL[j][1] - KSPL[j][0], 256], BF16, ph) for j in range(3)] for i in range(2)]

    def cast_block(stage, wb3, nk=16, n=256):
        for j, (k0, k1, e) in enumerate(KSPL):
            k1 = min(k1, nk)
            if k0 >= k1:
                continue
            if e == 'act':
                kb.op('act', lambda en, k0=k0, k1=k1, j=j: en.activation(out=wb3[j][:, 0:k1 - k0, 0:n], in_=stage[:, k0:k1, 0:n], func=AF.Copy), [stage], [wb3[j]])
            else:
                kb.op(e, lambda en, k0=k0, k1=k1, j=j: en.tensor_copy(out=wb3[j][:, 0:k1 - k0, 0:n], in_=stage[:, k0:k1, 0:n]), [stage], [wb3[j]])

    class WP:
        def __init__(self, t, kl):
            self.t, self.kl = t, kl

        def __getitem__(self, key):
            p, k, c = key
            return self.t[p, self.kl, c]

    def wpart(wb3, k):
        for j, (k0, k1, e) in enumerate(KSPL):
            if k0 <= k < k1:
                return WP(wb3[j], k - k0)

    def load_block(w_ap2d, col0, n, stage, nk=16):
        kb.dma('sp', stage[:, 0:nk, 0:n], w_ap2d[:, col0:col0 + n].rearrange("(k p) n -> p k n", p=128), [], [stage], prim=stage)

    modrow = kb.sb("modrow", [2, 256], F32, ph); badrow = kb.sb("badrow", [2, 6 * D], F32, ph)
    kb.dma('pool', badrow[:], b_ada_row[:].partition_broadcast(2), [b_ada_row], [badrow], prim=badrow)
    nblk = 6 * D // 256
    load_block(w_ada, 0, 256, stg[0])
    for bi in range(nblk):
        st, wb3 = stg[bi % 2], wbs[bi % 2]
        if bi + 1 < nblk:
            load_block(w_ada, (bi + 1) * 256, 256, stg[(bi + 1) % 2])
        cast_block(st, wb3)
        if bi < 16:
            for sub in range(2):
                p = PS[sub]
                for k in range(16):
                    wp = wpart(wb3, k)
                    kb.op('pe', lambda e, k=k, wp=wp, sub=sub, p=p: e.matmul(p[:, 0:2], wp[:, k, sub * 128:(sub + 1) * 128], silb[:, k, :], start=(k == 0), stop=(k == 15)), [wp.t, silb], [p])
                blk = bi * 2 + sub
                kb.op('dve', lambda e, p=p, blk=blk: e.tensor_scalar(out=modT[:, blk, :], in0=p[:, 0:2], scalar1=badT[:, blk:blk + 1], scalar2=None, op0=ALU.add), [p, badT], [modT])
        else:
            p = PS[2 + bi % 2]
            for k in range(16):
                wp = wpart(wb3, k)
                kb.op('pe', lambda e, k=k, wp=wp, p=p: e.matmul(p[0:2, 0:256], silb[:, k, :], wp[:, k, :], start=(k == 0), stop=(k == 15)), [wp.t, silb], [p])
            kb.op('dve', lambda e, p=p, bi=bi: e.tensor_tensor(out=modrow[:], in0=p[0:2, 0:256], in1=badrow[:, bi * 256:(bi + 1) * 256], op=ALU.add), [p, badrow], [modrow])
            kb.dma('pool', mod_d[:, bi * 256:(bi + 1) * 256], modrow[:], [modrow], [mod_d], prim=modrow)
    for c in range(2):
        kb.op('dve', lambda e, c=c: e.scalar_tensor_tensor(out=A1[:, :, c], in0=modT[:, 16:32, c], scalar=1.0, in1=g1s[:], op0=ALU.add, op1=ALU.mult), [modT, g1s], [A1])
        kb.op('dve', lambda e, c=c: e.tensor_copy(out=B1[:, :, c], in_=modT[:, 0:16, c]), [modT], [B1])
    kb.barrier()
    ph.close()

    ph = ExitStack()
    hT = kb.sb("hT", [128, 16, NTS], BF16, ph)
    xt = [kb.sb("xt%d" % i, [128, D], F32, ph) for i in range(2)]
    xn = [kb.sb("xn%d" % i, [128, D], F32, ph) for i in range(2)]
    junk = kb.sb("junk", [128, D], BF16, ph)
    ssq = [kb.sb("ssq%d" % i, [128, 1], F32, ph) for i in range(2)]
    rstd = [kb.sb("rstd%d" % i, [128, 1], F32, ph) for i in range(2)]
    binT = kb.sb("binT", [128, 128], F32, ph)
    kb.dma('sp', binT[:], b_inT[:], [b_inT], [binT], prim=binT)
    stg = [kb.sb("stg%d" % i, [128, 16, 256], F32, ph) for i in range(2)]
    wbs = [[kb.sb("wb%d_%d" % (i, j), [128, KSPL[j][1] - KSPL[j][0], 256], BF16, ph) for j in range(3)] for i in range(2)]
    ev_bf = [kb.sb("evbf%d" % i, [128, 512], BF16, ph) for i in range(4)]
    ev_f = [kb.sb("evf%d" % i, [128, 512], F32, ph) for i in range(4)]
    sgt = [kb.sb("sgt%d" % i, [128, 512], F32, ph) for i in range(2)]
    vb = [kb.sb("vb%d" % i, [128, 256], F32, ph) for i in range(2)]
    ko = [kb.sb("ko%d" % i, [128, 128], F32, ph) for i in range(4)]
    cnt = {'ev': 0, 'ps': 0, 'ko': 0, 'x': 0, 'vb': 0, 'sg': 0}

    def rr(key, n):
        cnt[key] += 1
        return (cnt[key] - 1) % n

    def make_hT(src_ap, nrows, col0, grp):
        i = rr('x', 2)
        x, xnn, ss, rs = xt[i], xn[i], ssq[i], rstd[i]
        kb.dma('sp', x[0:nrows, :], src_ap, [], [x], prim=x)
        kb.op('act', lambda e: e.activation(out=junk[0:nrows, :], in_=x[0:nrows, :], func=AF.Square, accum_out=ss[0:nrows, :]), [x], [junk, ss])
        kb.op('dve', lambda e: e.tensor_scalar(out=ss[0:nrows, :], in0=ss[0:nrows, :], scalar1=1.0 / D, scalar2=1e-6, op0=ALU.mult, op1=ALU.add), [ss], [ss])
        kb.op('act', lambda e: e.activation(out=ss[0:nrows, :], in_=ss[0:nrows, :], func=AF.Sqrt), [ss], [ss])
        kb.op('dve', lambda e: e.reciprocal(out=rs[0:nrows, :], in_=ss[0:nrows, :]), [ss], [rs])
        kb.op('act', lambda e: e.activation(out=xnn[0:nrows, :], in_=x[0:nrows, :], func=AF.Copy, scale=rs[0:nrows, :]), [x, rs], [xnn])
        for kq in range(4):
            p = PS[4 + rr('ps', 4)]
            for kk in range(4):
                k = kq * 4 + kk
                kb.op('pe', lambda e, k=k, kk=kk, p=p: e.transpose(p[:, kk * 128:kk * 128 + nrows], xnn[0:nrows, k * 128:(k + 1) * 128], ident[0:nrows, 0:nrows]), [xnn, ident], [p])
            for kk in range(4):
                k = kq * 4 + kk
                kb.op('dve' if kk % 2 == 0 else 'pool' if False else 'dve', lambda e, k=k, kk=kk, p=p: e.tensor_scalar(out=hT[:, k, col0:col0 + nrows], in0=p[:, kk * 128:kk * 128 + nrows], scalar1=A1[:, k, grp:grp + 1], scalar2=B1[:, k, grp:grp + 1], op0=ALU.mult, op1=ALU.add), [p, A1, B1], [hT])

    QO, KO, VO, UAO, UBO, GAO, GBO = 0, 3072, 6144, 9216, 10752, 12288, 14336
    wq = {'i': 0}

    def stream(blocks, body):
        def issue(bi):
            st = stg[(wq['i'] + bi) % 2]
            for (c0, n, d0) in blocks[bi]:
                kb.dma('sp', st[:, :, d0:d0 + n], w_in[:, c0:c0 + n].rearrange("(k p) n -> p k n", p=128), [], [st], prim=st)
        issue(0)
        for bi in range(len(blocks)):
            if bi + 1 < len(blocks):
                issue(bi + 1)
            st, wb3 = stg[(wq['i'] + bi) % 2], wbs[(wq['i'] + bi) % 2]
            cast_block(st, wb3)
            body(bi, wb3)
        wq['i'] += len(blocks)

    def fm_mm(wb3, sub, tok0, ntok):
        p = PS[rr('ps', 4)]
        for k in range(16):
            wp = wpart(wb3, k)
            kb.op('pe', lambda e, k=k, wp=wp: e.matmul(p[:, 0:ntok], wp[:, k, sub * 128:(sub + 1) * 128], hT[:, k, tok0:tok0 + ntok], start=(k == 0), stop=(k == 15)), [wp.t, hT], [p])
        return p

    def k_block(g, h, wb3, sub, tokblocks, ctx0, out_rows):
        blk = (KO + (g * 8 + h) * 128) // 128
        for (c0, ntok, cc) in tokblocks:
            p = fm_mm(wb3, sub, c0, ntok)
            i = rr('ev', 4)
            eb, ef = ev_bf[i], ev_f[i]
            need = [(t0, r0) for (t0, r0) in out_rows if c0 <= t0 < c0 + ntok]
            if need:
                kb.op('dve', lambda e, p=p, ef=ef: e.tensor_scalar(out=ef[:, 0:ntok], in0=p[:, 0:ntok], scalar1=binT[:, blk:blk + 1], scalar2=None, op0=ALU.add), [p, binT], [ef])
                kb.op('act', lambda e, ef=ef, eb=eb: e.activation(out=eb[:, 0:ntok], in_=ef[:, 0:ntok], func=AF.Copy), [ef], [eb])
            else:
                kb.op('act', lambda e, p=p, eb=eb: e.activation(out=eb[:, 0:ntok], in_=p[:, 0:ntok], func=AF.Identity, bias=binT[:, blk:blk + 1]), [p, binT], [eb])
            kb.dma('pool', kT_d[g][h, :, cc:cc + ntok], eb[:, 0:ntok], [eb], [kT_d[g]], prim=eb)
            if need:
                for (t0, r0) in need:
                    pt = PS[4 + rr('ps', 4)]
                    kb.op('pe', lambda e, pt=pt, ef=ef, t0=t0: e.transpose(pt[:, 0:128], ef[:, t0 - c0:t0 - c0 + 128], ident[:]), [ef, ident], [pt])
                    kk = ko[rr('ko', 4)]
                    kb.op('act', lambda e, pt=pt, kk=kk: e.activation(out=kk[:], in_=pt[:, 0:128], func=AF.Copy), [pt], [kk])
                    if 'd' not in os.environ.get('KSKIP', ''):
                        kb.dma(os.environ.get('KSQ', 'pool'), kv_o[g][r0:r0 + 128, 0, h, :], kk[:], [kk], [kv_o[g]], prim=kk)

    def v_block(g, h0, wb3, toktiles, out_rows):
        c0 = VO + (g * 8 + h0) * 128
        b = vb[rr('vb', 2)]
        kb.dma('pool', b[:], b_in_row[:, c0:c0 + 256].partition_broadcast(128), [b_in_row], [b], prim=b)
        for (t0, cr) in toktiles:
            p = PS[rr('ps', 4)]
            for k in range(16):
                wp = wpart(wb3, k)
                kb.op('pe', lambda e, k=k, wp=wp, p=p: e.matmul(p[:, 0:256], hT[:, k, t0:t0 + 128], wp[:, k, :], start=(k == 0), stop=(k == 15)), [wp.t, hT], [p])
            i = rr('ev', 4)
            eb, ef = ev_bf[i], ev_f[i]
            kb.op('dve', lambda e, p=p, ef=ef: e.tensor_tensor(out=ef[:, 0:256], in0=p[:, 0:256], in1=b[:], op=ALU.add), [p, b], [ef])
            kb.op('act', lambda e, eb=eb, ef=ef: e.activation(out=eb[:, 0:256], in_=ef[:, 0:256], func=AF.Copy), [ef], [eb])
            kb.dma('pool', V_d[g][cr:cr + 128, h0:h0 + 2, :], eb[:, 0:256].rearrange("p (h d) -> p h d", h=2), [eb], [V_d[g]], prim=eb)
            for (tt0, r0) in out_rows:
                if tt0 == t0:
                    kb.dma('pool', kv_o[g][r0:r0 + 128, 1, h0:h0 + 2, :], ef[:, 0:256].rearrange("p (h d) -> p h d", h=2), [ef], [kv_o[g]], prim=ef)

    def u_block(j, wb3, tokblocks):
        ba, bb = (UAO // 128) + j, (UBO // 128) + j
        for (c0, ntok, dst, dc) in tokblocks:
            pa = fm_mm(wb3, 0, c0, ntok)
            pb = fm_mm(wb3, 1, c0, ntok)
            s = sgt[rr('sg', 2)]
            i = rr('ev', 4)
            ef = ev_f[i]
            kb.op('act', lambda e, pb=pb, s=s: e.activation(out=s[:, 0:ntok], in_=pb[:, 0:ntok], func=AF.Sigmoid, bias=binT[:, bb:bb + 1]), [pb, binT], [s])
            kb.op('dve', lambda e, pa=pa, s=s, ef=ef: e.scalar_tensor_tensor(out=ef[:, 0:ntok], in0=pa[:, 0:ntok], scalar=binT[:, ba:ba + 1], in1=s[:, 0:ntok], op0=ALU.add, op1=ALU.mult), [pa, s, binT], [ef])
            kb.dma('pool', dst[j * 128:(j + 1) * 128, dc:dc + ntok], ef[:, 0:ntok], [ef], [dst], prim=ef)

    for grp_i in range(2 if DBG in ('', 'A', 'B') else 0):
        if DBG == 'A' and grp_i == 1:
            break
        base = grp_i * 1024
        for t in range(8):
            make_hT(xh[base + t * 128: base + (t + 1) * 128, :], 128, t * 128, 0)
        blocks, kinds = [], []
        for hp in range(4):
            blocks.append([(KO + (2 * 8 + 2 * hp) * 128, 256, 0)]); kinds.append(('k', 2, 2 * hp))
        for hp in range(4):
            blocks.append([(VO + (2 * 8 + 2 * hp) * 128, 256, 0)]); kinds.append(('v', 2, 2 * hp))
        if grp_i == 1:
            for g in (1, 0):
                for hp in range(4):
                    blocks.append([(KO + (g * 8 + 2 * hp) * 128, 256, 0)]); kinds.append(('k', g, 2 * hp))
                for hp in range(4):
                    blocks.append([(VO + (g * 8 + 2 * hp) * 128, 256, 0)]); kinds.append(('v', g, 2 * hp))
            for j in range(12):
                blocks.append([(UAO + j * 128, 128, 0), (UBO + j * 128, 128, 128)]); kinds.append(('u', j, 0))

        def body(bi, wb3, kinds=kinds, base=base):
            kind, g, h0 = kinds[bi]
            if kind == 'k':
                for sub in range(2):
                    if g == 2:
                        tb = [(0, 512, base), (512, 512, base + 512)]
                    elif g == 1:
                        tb = [(512, 512, 0)]
                    else:
                        tb = [(896, 128, 0)]
                    k_block(g, h0 + sub, wb3, sub, tb, 0, [])
            elif kind == 'v':
                if g == 2:
                    tt = [(t * 128, base + t * 128) for t in range(8)]
                elif g == 1:
                    tt = [(512 + t * 128, t * 128) for t in range(4)]
                else:
                    tt = [(896, 0)]
                v_block(g, h0, wb3, tt, [])
            else:
                u_block(g, wb3, [(896, 128, u_d, 0)])
        stream(blocks, body)

    for t in range(8 if DBG in ('', 'C') else 0):
        make_hT(xh[HALO + t * 128: HALO + (t + 1) * 128, :], 128, t * 128, 0)
    if DBG in ('', 'C'):
        make_hT(xs[:, :], NS, NT, 1)
    blocks, kinds = [], []
    for g in range(3):
        for hp in range(4):
            blocks.append([(QO + (g * 8 + 2 * hp) * 128, 256, 0)]); kinds.append(('q', g, 2 * hp))
        for hp in range(4):
            blocks.append([(KO + (g * 8 + 2 * hp) * 128, 256, 0)]); kinds.append(('k', g, 2 * hp))
        for hp in range(4):
            blocks.append([(VO + (g * 8 + 2 * hp) * 128, 256, 0)]); kinds.append(('v', g, 2 * hp))
    for j in range(12):
        blocks.append([(UAO + j * 128, 128, 0), (UBO + j * 128, 128, 128)]); kinds.append(('u', j, 0))
    for j in range(16):
        blocks.append([(GAO + j * 128, 256, 0)] if False else [(GAO + 2 * j * 128, 256, 0)]); kinds.append(('g', 2 * j, 0))
    vs_sb = kb.sb("vs_sb", [NS, 256], F32, ph); vs_bf = kb.sb("vs_bf", [NS, 256], BF16, ph)
    sm_bf = kb.sb("sm_bf", [128, NS], BF16, ph); sm_f = kb.sb("sm_f", [128, NS], F32, ph); sm_t = kb.sb("sm_t", [NS, 128], F32, ph)

    def bodyC(bi, wb3):
        kind, g, h0 = kinds[bi]
        if kind == 'q':
            for sub in range(2):
                h = h0 + sub
                blk = (QO + (g * 8 + h) * 128) // 128
                for half in range(2):
                    p = fm_mm(wb3, sub, half * 512, 512)
                    eb = ev_bf[rr('ev', 4)]
                    kb.op('act', lambda e, p=p, eb=eb, blk=blk: e.activation(out=eb[:, :], in_=p[:, :], func=AF.Identity, bias=binT[:, blk:blk + 1]), [p, binT], [eb])
                    kb.dma('pool', qT_d[g, h, :, half * 512:(half + 1) * 512], eb[:, :], [eb], [qT_d], prim=eb)
                p = fm_mm(wb3, sub, NT, NS)
                kb.op('act', lambda e, p=p, blk=blk: e.activation(out=sm_bf[:, :], in_=p[:, 0:NS], func=AF.Identity, bias=binT[:, blk:blk + 1]), [p, binT], [sm_bf])
                kb.dma('pool', qsT_d[g, h, :, :], sm_bf[:, :], [sm_bf], [qsT_d], prim=sm_bf)
        elif kind == 'k':
            W = GH[g]
            nout = min(W, NT)
            outs = [(NT - nout + t * 128, t * 128) for t in range(nout // 128)]
            for sub in range(2):
                h = h0 + sub
                KS = os.environ.get('KSUB', 'os')
                k_block(g, h, wb3, sub, [(0, 512, W), (512, 512, W + 512)], 0, outs if 'o' in KS else [])
                if 's' not in KS:
                    continue
                blk = (KO + (g * 8 + h) * 128) // 128
                p = fm_mm(wb3, sub, NT, NS)
                kb.op('dve', lambda e, p=p, blk=blk: e.tensor_scalar(out=sm_f[:, :], in0=p[:, 0:NS], scalar1=binT[:, blk:blk + 1], scalar2=None, op0=ALU.add), [p, binT], [sm_f])
                kb.op('act', lambda e: e.activation(out=sm_bf[:, :], in_=sm_f[:, :], func=AF.Copy), [sm_f], [sm_bf])
                kb.dma('pool', ksT_d[g, h, :, :], sm_bf[:, :], [sm_bf], [ksT_d], prim=sm_bf)
                pt = PS[4 + rr('ps', 4)]
                kb.op('pe', lambda e, pt=pt: e.transpose(pt[0:NS, 0:128], sm_f[:, :], ident[:]), [sm_f, ident], [pt])
                kb.op('act', lambda e, pt=pt: e.activation(out=sm_t[:, :], in_=pt[0:NS, 0:128], func=AF.Copy), [pt], [sm_t])
                kb.dma('pool', kvs_o[g, :, 0, h, :], sm_t[:, :], [sm_t], [kvs_o], prim=sm_t)
        elif kind == 'v':
            W = GH[g]
            nout = min(W, NT)
            outs = [(NT - nout + t * 128, t * 128) for t in range(nout // 128)]
            v_block(g, h0, wb3, [(t * 128, W + t * 128) for t in range(8)], outs)
            c0 = VO + (g * 8 + h0) * 128
            b = vb[rr('vb', 2)]
            kb.dma('pool', b[:], b_in_row[:, c0:c0 + 256].partition_broadcast(128), [b_in_row], [b], prim=b)
            p = PS[rr('ps', 4)]
            for k in range(16):
                wp = wpart(wb3, k)
                kb.op('pe', lambda e, k=k, wp=wp, p=p: e.matmul(p[0:NS, 0:256], hT[:, k, NT:NTS], wp[:, k, :], start=(k == 0), stop=(k == 15)), [wp.t, hT], [p])
            kb.op('dve', lambda e, p=p, b=b: e.tensor_tensor(out=vs_sb[:, :], in0=p[0:NS, 0:256], in1=b[0:NS, :], op=ALU.add), [p, b], [vs_sb])
            kb.op('act', lambda e: e.activation(out=vs_bf[:, :], in_=vs_sb[:, :], func=AF.Copy), [vs_sb], [vs_bf])
            kb.dma('pool', Vs_d[g, :, h0:h0 + 2, :], vs_bf[:, :].rearrange("p (h d) -> p h d", h=2), [vs_bf], [Vs_d], prim=vs_bf)
            kb.dma('pool', kvs_o[g, :, 1, h0:h0 + 2, :], vs_sb[:, :].rearrange("p (h d) -> p h d", h=2), [vs_sb], [kvs_o], prim=vs_sb)
        elif kind == 'u':
            u_block(g, wb3, [(0, 512, u_d, 128), (512, 512, u_d, 128 + 512), (NT, NS, us_d, 0)])
        else:
            for sub in range(2):
                jb = g + sub
                blk = GAO // 128 + jb
                for (c0, ntok) in ((0, 512), (512, 512), (NT, NS)):
                    p = fm_mm(wb3, sub, c0, ntok)
                    ef = ev_f[rr('ev', 4)]
                    kb.op('act', lambda e, p=p, ef=ef, blk=blk, ntok=ntok: e.activation(out=ef[:, 0:ntok], in_=p[:, 0:ntok], func=AF.Sigmoid, bias=binT[:, blk:blk + 1]), [p, binT], [ef])
                    kb.dma('pool', sg_d[jb * 128:(jb + 1) * 128, c0:c0 + ntok], ef[:, 0:ntok], [ef], [sg_d], prim=ef)
    KK = os.environ.get('KKINDS', 'qkvug')
    sel = [i for i in range(len(blocks)) if kinds[i][0] in KK]
    blocks = [blocks[i] for i in sel]; kinds = [kinds[i] for i in sel]
    if DBG in ('', 'C'):
        stream(blocks, bodyC)
    kb.barrier()
    ph.close()


    mid = ExitStack()
    attn_oT = kb.sb("attn_oT", [128, 8, NTS], BF16, mid)
    SCALE = 128.0 ** -0.5
    if DBG in ('', 'C', 'ATT'):
        ph = ExitStack()
        M1 = kb.sb("M1", [128, 256], F32, ph); kb.dma('sp', M1[:], M1_d[:], [], [M1], prim=M1)
        kval = kb.sb("kval", [128, 53], F32, ph); kb.dma('sp', kval[:], kval_d[:], [], [kval], prim=kval)
        SMc = kb.sb("SMc", [128, 21, NS], F32, ph); kb.dma('sp', SMc[:], SMc_d[:], [], [SMc], prim=SMc)
        SMn = kb.sb("SMn", [NS, 3, NS], F32, ph); kb.dma('sp', SMn[:], SMn_d[:], [], [SMn], prim=SMn)
        onesb = kb.sb("onesb", [128, 128], BF16, ph)
        kb.op('dve', lambda e: e.memset(onesb[:], 1.0), [], [onesb])
        qt = [kb.sb("qt%d" % i, [128, 3, NT], BF16, ph) for i in range(2)]
        kt = [[kb.sb("kt%d_%d" % (i, g), [128, NCTX[g]], BF16, ph) for g in range(3)] for i in range(2)]
        vt1 = [kb.sb("vt1_%d" % i, [128, 9, 128], BF16, ph) for i in range(2)]
        vt2 = [kb.sb("vt2_%d" % i, [128, 4, 3, 128], BF16, ph) for i in range(2)]
        vt3 = [kb.sb("vt3_%d" % i, [128, 16, 2, 128], BF16, ph) for i in range(2)]
        Et = [kb.sb("Et%d" % i, [128, 256], F32, ph) for i in range(3)]
        Pt = [kb.sb("Pt%d" % i, [128, 256], BF16, ph) for i in range(3)]
        acc = kb.sb("acc", [128, NT], F32, ph); dacc = kb.sb("dacc", [128, NT], F32, ph)
        qs = [kb.sb("qs%d" % i, [128, 3, NS], BF16, ph) for i in range(2)]
        ksn = [kb.sb("ksn%d" % i, [128, 3, NS], BF16, ph) for i in range(2)]
        vsn = [kb.sb("vsn%d" % i, [NS, 3, 128], BF16, ph) for i in range(2)]
        ck = [kb.sb("ck%d" % i, [128, 128], F32, ph) for i in range(3)]
        cv = [kb.sb("cv%d" % i, [128, 128], F32, ph) for i in range(3)]
        ckT = [kb.sb("ckT%d" % i, [128, 128], BF16, ph) for i in range(3)]
        cvb = [kb.sb("cvb%d" % i, [128, 128], BF16, ph) for i in range(3)]
        Es = [kb.sb("Es%d" % i, [128, NS], F32, ph) for i in range(3)]
        Psm = [kb.sb("Psm%d" % i, [128, NS], BF16, ph) for i in range(3)]
        osn = kb.sb("osn", [128, NS], F32, ph); dsn = kb.sb("dsn", [128, NS], F32, ph)
        ac = {'s': 0, 'c': 0}

        def load_head(h, i):
            for g in range(3):
                kb.dma('sp', qt[i][:, g, :], qT_d[g, h, :, :], [qT_d], [qt[i]], prim=qt[i])
                kb.dma('sp', kt[i][g][:, :], kT_d[g][h, :, :], [kT_d[g]], [kt[i][g]], prim=kt[i][g])
            kb.dma('sp', vt1[i][:], V_d[0][:, h, :].rearrange("(j p) d -> p j d", p=128), [V_d[0]], [vt1[i]], prim=vt1[i])
            for r in range(4):
                kb.dma('sp', vt2[i][:, r, :, :], V_d[1][:, h, :].rearrange("(j p r) d -> p r j d", p=128, r=4)[:, r, :, :], [V_d[1]], [vt2[i]], prim=vt2[i])
            v3 = V_d[2][:, h, :].rearrange("(m r) d -> m r d", r=16)
            kb.dma('sp', vt3[i][:, :, 0, :], v3[0:128, :, :], [V_d[2]], [vt3[i]], prim=vt3[i])
            kb.dma('sp', vt3[i][0:64, :, 1, :], v3[128:192, :, :], [V_d[2]], [vt3[i]], prim=vt3[i])
            kb.dma('sp', qs[i][:], qsT_d[:, h, :, :].rearrange("g d q -> d g q"), [qsT_d], [qs[i]], prim=qs[i])
            kb.dma('sp', ksn[i][:], ksT_d[:, h, :, :].rearrange("g d q -> d g q"), [ksT_d], [ksn[i]], prim=ksn[i])
            kb.dma('sp', vsn[i][:], Vs_d[:, :, h, :].rearrange("g t d -> t g d"), [Vs_d], [vsn[i]], prim=vsn[i])

        def tile_attn(kT_ap, nk, q_ap, N, v_ap, kv_idx, m0, qstart, first):
            sp_ = PS[4 + ac['s'] % 3]; Ei = Et[ac['s'] % 3]; Pi = Pt[ac['s'] % 3]; ac['s'] += 1
            kb.op('pe', lambda e: e.matmul(sp_[0:nk, 0:N], kT_ap, q_ap, start=True, stop=True), kt_reads, [sp_])
            kb.op('act', lambda e: e.activation(out=Ei[0:nk, 0:N], in_=sp_[0:nk, 0:N], func=AF.Exp, scale=SCALE), [sp_], [Ei])
            kb.op('dve', lambda e: e.scalar_tensor_tensor(out=Pi[0:nk, 0:N], in0=Ei[0:nk, 0:N], scalar=kval[0:nk, kv_idx:kv_idx + 1], in1=M1[0:nk, m0:m0 + N], op0=ALU.mult, op1=ALU.mult), [Ei, kval, M1], [Pi])
            segs = []
            a, b = qstart, qstart + N
            if a < 512 and b > 512:
                segs = [(a, 512, 0), (512, b, 512 - a)]
            else:
                segs = [(a, b, 0)]
            for (qa, qb, po) in segs:
                bank = qa // 512
                st = first[bank]
                first[bank] = False
                n = qb - qa
                kb.op('pe', lambda e: e.matmul(PS[bank][:, qa - bank * 512:qb - bank * 512], v_ap, Pi[0:nk, po:po + n], start=st, stop=True, skip_group_check=True), [Pi] + v_reads, [PS[bank]])
                kb.op('pe', lambda e: e.matmul(PS[2 + bank][:, qa - bank * 512:qb - bank * 512], onesb[0:nk, :], Pi[0:nk, po:po + n], start=st, stop=True, skip_group_check=True), [Pi, onesb], [PS[2 + bank]])

        load_head(0, 0)
        for h in range(8):
            i = h % 2
            if h + 1 < 8:
                load_head(h + 1, (h + 1) % 2)
            kt_reads = [kt[i][0], kt[i][1], kt[i][2], qt[i]]
            v_reads = [vt1[i], vt2[i], vt3[i]]
            first = [True, True]
            for j in range(9):
                t0 = max(0, 128 * (j - 1)); t1 = min(NT, 128 * (j - 1) + 256)
                m0 = 128 if j == 0 else 0
                tile_attn(kt[i][0][:, 128 * j:128 * j + 128], 128, qt[i][:, 0, t0:t1], t1 - t0, vt1[i][:, j, :], j, m0, t0, first)
            for b in range(2):
                kb.op('act', lambda e, b=b: e.activation(out=acc[:, 512 * b:512 * b + 512], in_=PS[b][:, :], func=AF.Copy), [PS[b]], [acc])
                kb.op('dve', lambda e, b=b: e.tensor_copy(out=dacc[:, 512 * b:512 * b + 512], in_=PS[2 + b][:, :]), [PS[2 + b]], [dacc])
            first = [True, True]
            k2 = kt[i][1][:, :].rearrange("d (j p r) -> d j r p", p=128, r=4)
            q2 = qt[i][:, 1, :].rearrange("d (i r) -> d r i", r=4)
            for r in range(4):
                for j in range(3):
                    i0 = 0 if j < 2 else 128
                    N = 128 if j != 1 else 256
                    m0 = 128 if j == 0 else 0
                    tile_attn(k2[:, j, r, :], 128, q2[:, r, i0:i0 + N], N, vt2[i][:, r, j, :], 9 + r * 3 + j, m0, r * 256 + i0, first)
            for (A, Pb) in ((acc, 0), (dacc, 2)):
                Av = A[:, :].rearrange("p (i r) -> p r i", r=4)
                for b in range(2):
                    kb.op('dve', lambda e, b=b, Av=Av, Pb=Pb: e.tensor_tensor(out=Av[:, 2 * b:2 * b + 2, :], in0=Av[:, 2 * b:2 * b + 2, :], in1=PS[Pb + b][:, :].rearrange("p (r i) -> p r i", r=2), op=ALU.add), [A, PS[Pb + b]], [A])
            first = [True, True]
            k3 = kt[i][2][:, :].rearrange("d (m r) -> d r m", r=16)
            q3 = qt[i][:, 2, :].rearrange("d (i r) -> d r i", r=16)
            for r in range(16):
                tile_attn(k3[:, r, 0:128], 128, q3[:, r, :], 64, vt3[i][:, r, 0, :], 21 + 2 * r, 128, r * 64, first)
                tile_attn(k3[:, r, 128:192], 64, q3[:, r, :], 64, vt3[i][0:64, r, 1, :], 21 + 2 * r + 1, 0, r * 64, first)
            for (A, Pb) in ((acc, 0), (dacc, 2)):
                Av = A[:, :].rearrange("p (i r) -> p r i", r=16)
                for b in range(2):
                    kb.op('dve', lambda e, b=b, Av=Av, Pb=Pb: e.tensor_tensor(out=Av[:, 8 * b:8 * b + 8, :], in0=Av[:, 8 * b:8 * b + 8, :], in1=PS[Pb + b][:, :].rearrange("p (r i) -> p r i", r=8), op=ALU.add), [A, PS[Pb + b]], [A])
            kb.op('dve', lambda e: e.reciprocal(out=dacc[:, :], in_=dacc[:, :]), [dacc], [dacc])
            kb.op('dve', lambda e: e.tensor_tensor(out=attn_oT[:, h, 0:NT], in0=acc[:, :], in1=dacc[:, :], op=ALU.mult), [acc, dacc], [attn_oT])
            po, pd = PS[7], PS[7]
            firsts = [True]
            tiles = []
            for g, W in enumerate(GH):
                for j in range(W // 128):
                    tiles.append((g, j))
            ti = 0
            for (g, j) in tiles:
                c = ac['c'] % 3; ac['c'] += 1
                kb.dma('sp', ck[c][:], cache_d[g][128 * j:128 * j + 128, 0, h, :], [], [ck[c]], prim=ck[c])
                kb.dma('sp', cv[c][:], cache_d[g][128 * j:128 * j + 128, 1, h, :], [], [cv[c]], prim=cv[c])
                pt = PS[4 + ac['s'] % 3]; ac['s'] += 1
                kb.op('pe', lambda e, c=c, pt=pt: e.transpose(pt[:, 0:128], ck[c][:], ident[:]), [ck[c], ident], [pt])
                kb.op('act', lambda e, c=c, pt=pt: e.activation(out=ckT[c][:], in_=pt[:, 0:128], func=AF.Copy), [pt], [ckT[c]])
                kb.op('dve', lambda e, c=c: e.tensor_copy(out=cvb[c][:], in_=cv[c][:]), [cv[c]], [cvb[c]])
                sp_ = PS[4 + ac['s'] % 3]; Ei = Es[ac['s'] % 3]; Pi = Psm[ac['s'] % 3]; ac['s'] += 1
                kb.op('pe', lambda e, c=c, sp_=sp_, g=g: e.matmul(sp_[:, 0:NS], ckT[c][:], qs[i][:, g, :], start=True, stop=True), [ckT[c], qs[i]], [sp_])
                kb.op('act', lambda e, sp_=sp_, Ei=Ei: e.activation(out=Ei[:, :], in_=sp_[:, 0:NS], func=AF.Exp, scale=SCALE), [sp_], [Ei])
                kb.op('dve', lambda e, Ei=Ei, Pi=Pi, ti=ti: e.tensor_tensor(out=Pi[:, :], in0=Ei[:, :], in1=SMc[:, ti, :], op=ALU.mult), [Ei, SMc], [Pi])
                st = firsts[0]; firsts[0] = False
                kb.op('pe', lambda e, c=c, Pi=Pi, st=st: e.matmul(PS[7][:, 0:NS], cvb[c][:], Pi[:, :], start=st, stop=True, skip_group_check=True), [cvb[c], Pi], [PS[7]])
                kb.op('pe', lambda e, Pi=Pi, st=st: e.matmul(PS[7][:, 64:64 + NS], onesb[:, :], Pi[:, :], start=False, stop=True, skip_group_check=True), [onesb, Pi], [PS[7]])
                ti += 1
            for g in range(3):
                sp_ = PS[4 + ac['s'] % 3]; Ei = Es[ac['s'] % 3]; Pi = Psm[ac['s'] % 3]; ac['s'] += 1
                kb.op('pe', lambda e, sp_=sp_, g=g: e.matmul(sp_[0:NS, 0:NS], ksn[i][:, g, :], qs[i][:, g, :], start=True, stop=True), [ksn[i], qs[i]], [sp_])
                kb.op('act', lambda e, sp_=sp_, Ei=Ei: e.activation(out=Ei[0:NS, :], in_=sp_[0:NS, 0:NS], func=AF.Exp, scale=SCALE), [sp_], [Ei])
                kb.op('dve', lambda e, Ei=Ei, Pi=Pi, g=g: e.tensor_tensor(out=Pi[0:NS, :], in0=Ei[0:NS, :], in1=SMn[:, g, :], op=ALU.mult), [Ei, SMn], [Pi])
                kb.op('pe', lambda e, Pi=Pi, g=g: e.matmul(PS[7][:, 0:NS], vsn[i][:, g, :], Pi[0:NS, :], start=False, stop=True, skip_group_check=True), [vsn[i], Pi], [PS[7]])
                kb.op('pe', lambda e, Pi=Pi: e.matmul(PS[7][:, 64:64 + NS], onesb[0:NS, :], Pi[0:NS, :], start=False, stop=True, skip_group_check=True), [onesb, Pi], [PS[7]])
            kb.op('dve', lambda e: e.reciprocal(out=dsn[:, :], in_=PS[7][:, 64:64 + NS]), [PS[7]], [dsn])
            kb.op('dve', lambda e: e.tensor_tensor(out=attn_oT[:, h, NT:NTS], in0=PS[7][:, 0:NS], in1=dsn[:, :], op=ALU.mult), [PS[7], dsn], [attn_oT])
        if os.environ.get('KDUMPA'):
            adbg = kb.dram("attn_dbg", [128, 8, NTS], BF16, "ExternalOutput")
            kb.dma('pool', adbg[:], attn_oT[:], [attn_oT], [adbg], prim=attn_oT)
        kb.barrier()
        ph.close()


    conv_fT = kb.sb("conv_fT", [128, 12, NTS], BF16, mid)
    x1_d = kb.dram("x1_d", [NT + 128, D])
    if DBG in ('', 'CONV'):
        ph = ExitStack()
        wdw = kb.sb("wdw", [128, 12, 31], F32, ph); kb.dma('sp', wdw[:], w_dwT[:], [], [wdw], prim=wdw)
        cpar = kb.sb("cpar", [128, 3, 12], F32, ph); kb.dma('sp', cpar[:], cparT[:], [], [cpar], prim=cpar)
        hprev = kb.sb("hprev", [128, 1], F32, ph); kb.dma('sp', hprev[:], hprev_d[:], [], [hprev], prim=hprev)
        onesf = kb.sb("onesf", [128, 128], F32, ph); kb.op('dve', lambda e: e.memset(onesf[:], 1.0), [], [onesf])
        yc = kb.sb("yc", [128, 12, NTS], F32, ph)
        ub = [kb.sb("ub%d" % i, [128, 30 + NT], F32, ph) for i in range(2)]
        ubs = [kb.sb("ubs%d" % i, [128, 30 + NS], F32, ph) for i in range(2)]
        cpo = kb.sb("cpo", [32, 1536], F32, ph); cso = kb.sb("cso", [NS, 1536], F32, ph)
        sq = [kb.sb("sq%d" % i, [128, 512], F32, ph) for i in range(2)]
        mu = kb.sb("mu", [128, NTS], F32, ph); rsd = kb.sb("rsd", [128, NTS], F32, ph); tmpc = [kb.sb("tmpc%d" % i, [128, 512], F32, ph) for i in range(2)]
        kb.dma('pool', convs_o[0:22, :], st_d[8:30, :], [st_d], [convs_o], prim=hprev)
        for j in range(12):
            u, us_ = ub[j % 2], ubs[j % 2]
            kb.dma('sp', u[:, :], u_d[j * 128:(j + 1) * 128, 98:128 + NT], [u_d], [u], prim=u)
            kb.dma('sp', us_[:, 0:30], stT_d[j * 128:(j + 1) * 128, :], [stT_d], [us_], prim=us_)
            kb.dma('sp', us_[:, 30:30 + NS], us_d[j * 128:(j + 1) * 128, :], [us_d], [us_], prim=us_)
            kb.op('dve', lambda e, u=u: e.tensor_scalar(out=u[:, 0:30], in0=u[:, 0:30], scalar1=hprev[:, 0:1], scalar2=None, op0=ALU.mult), [u, hprev], [u])
            for (src, L, c0) in ((u, NT, 0), (us_, NS, NT)):
                kb.op('dve', lambda e, src=src, L=L, c0=c0: e.tensor_scalar(out=yc[:, j, c0:c0 + L], in0=src[:, 0:L], scalar1=wdw[:, j, 0:1], scalar2=cpar[:, 0, j:j + 1], op0=ALU.mult, op1=ALU.add), [src, wdw, cpar], [yc])
                for k in range(1, 31):
                    kb.op('dve', lambda e, src=src, L=L, c0=c0, k=k: e.scalar_tensor_tensor(out=yc[:, j, c0:c0 + L], in0=src[:, k:k + L], scalar=wdw[:, j, k:k + 1], in1=yc[:, j, c0:c0 + L], op0=ALU.mult, op1=ALU.add), [src, wdw], [yc])
            pt = PS[4 + j % 2]
            kb.op('pe', lambda e, pt=pt, u=u: e.transpose(pt[0:32, 0:128], u[:, 30 + NT - 32:30 + NT], ident[:]), [u, ident], [pt])
            kb.op('act', lambda e, pt=pt: e.activation(out=cpo[:, j * 128:(j + 1) * 128], in_=pt[0:32, 0:128], func=AF.Copy), [pt], [cpo])
            pt2 = PS[6 + j % 2]
            kb.op('pe', lambda e, pt2=pt2, us_=us_: e.transpose(pt2[0:NS, 0:128], us_[:, 30:30 + NS], ident[:]), [us_, ident], [pt2])
            kb.op('act', lambda e, pt2=pt2: e.activation(out=cso[:, j * 128:(j + 1) * 128], in_=pt2[0:NS, 0:128], func=AF.Copy), [pt2], [cso])
        kb.dma('pool', convp_o[:, :], cpo[2:32, :], [cpo], [convp_o], prim=cpo)
        kb.dma('pool', convs_o[22:30, :], cso[:, :], [cso], [convs_o], prim=cso)
        for (c0, n) in ((0, 512), (512, 512), (NT, NS)):
            p1, p2 = PS[0], PS[1]
            for j in range(12):
                s_ = sq[j % 2]
                kb.op('act', lambda e, s_=s_: e.activation(out=s_[:, 0:n], in_=yc[:, j, c0:c0 + n], func=AF.Square), [yc], [s_])
                kb.op('pe', lambda e: e.matmul(p1[:, 0:n], onesf[:, :], yc[:, j, c0:c0 + n], start=(j == 0), stop=(j == 11)), [onesf, yc], [p1])
                kb.op('pe', lambda e, s_=s_: e.matmul(p2[:, 0:n], onesf[:, :], s_[:, 0:n], start=(j == 0), stop=(j == 11)), [onesf, s_], [p2])
            t_ = tmpc[0]
            kb.op('dve', lambda e: e.tensor_scalar(out=mu[:, c0:c0 + n], in0=p1[:, 0:n], scalar1=1.0 / 1536, scalar2=None, op0=ALU.mult), [p1], [mu])
            kb.op('dve', lambda e: e.tensor_tensor(out=t_[:, 0:n], in0=mu[:, c0:c0 + n], in1=mu[:, c0:c0 + n], op=ALU.mult), [mu], [t_])
            kb.op('dve', lambda e: e.scalar_tensor_tensor(out=t_[:, 0:n], in0=p2[:, 0:n], scalar=1.0 / 1536, in1=t_[:, 0:n], op0=ALU.mult, op1=ALU.subtract), [p2, t_], [t_])
            kb.op('dve', lambda e: e.tensor_scalar(out=t_[:, 0:n], in0=t_[:, 0:n], scalar1=1e-6, scalar2=None, op0=ALU.add), [t_], [t_])
            kb.op('act', lambda e: e.activation(out=t_[:, 0:n], in_=t_[:, 0:n], func=AF.Sqrt), [t_], [t_])
            kb.op('dve', lambda e: e.reciprocal(out=rsd[:, c0:c0 + n], in_=t_[:, 0:n]), [t_], [rsd])
            for j in range(12):
                t2 = tmpc[1]
                kb.op('dve', lambda e, t2=t2: e.tensor_tensor(out=t2[:, 0:n], in0=yc[:, j, c0:c0 + n], in1=mu[:, c0:c0 + n], op=ALU.subtract), [yc, mu], [t2])
                kb.op('pool', lambda e, t2=t2: e.tensor_tensor(out=t2[:, 0:n], in0=t2[:, 0:n], in1=rsd[:, c0:c0 + n], op=ALU.mult), [t2, rsd], [t2])
                kb.op('dve', lambda e, t2=t2: e.tensor_scalar(out=t2[:, 0:n], in0=t2[:, 0:n], scalar1=cpar[:, 1, j:j + 1], scalar2=cpar[:, 2, j:j + 1], op0=ALU.mult, op1=ALU.add), [t2, cpar], [t2])
                kb.op('act', lambda e, t2=t2: e.activation(out=conv_fT[:, j, c0:c0 + n], in_=t2[:, 0:n], func=AF.Silu), [t2], [conv_fT])
        kb.barrier()
        ph.close()

    if DBG in ('', 'CONV'):
        ph = ExitStack()
        sT = kb.sb("sT", [128, 16, NTS], BF16, ph)
        bo = kb.sb("bo", [128, 2, 16], F32, ph); kb.dma('sp', bo[:], boT[:], [], [bo], prim=bo)
        stg = [kb.sb("stg%d" % i, [128, 16, 256], F32, ph) for i in range(2)]
        wbs = [[kb.sb("wb%d_%d" % (i, j), [128, KSPL[j][1] - KSPL[j][0], 256], BF16, ph) for j in range(3)] for i in range(2)]
        sga = [kb.sb("sga%d" % i, [128, 512], F32, ph) for i in range(2)]; sgb = [kb.sb("sgb%d" % i, [128, 512], F32, ph) for i in range(2)]
        t1 = [kb.sb("t1_%d" % i, [128, 512], F32, ph) for i in range(2)]; t2_ = [kb.sb("t2_%d" % i, [128, 512], F32, ph) for i in range(2)]
        mc = {'i': 0}
        seq = []
        for cb in range(8):
            seq.append(('a', cb)); seq.append(('c', cb))

        def issue(idx):
            kind, cb = seq[idx]
            st = stg[idx % 2]
            if kind == 'a':
                kb.dma('sp', st[:, 0:8, :], w_ao[:, cb * 256:(cb + 1) * 256].rearrange("(k p) n -> p k n", p=128), [], [st], prim=st)
            else:
                kb.dma('sp', st[:, 0:12, :], w_co[:, cb * 256:(cb + 1) * 256].rearrange("(k p) n -> p k n", p=128), [], [st], prim=st)
        issue(0)
        pa_t = {}
        for idx in range(len(seq)):
            if idx + 1 < len(seq):
                issue(idx + 1)
            kind, cb = seq[idx]
            st, wb3 = stg[idx % 2], wbs[idx % 2]
            nk = 8 if kind == 'a' else 12
            cast_block(st, wb3, nk=nk)
            src = attn_oT if kind == 'a' else conv_fT
            for sub in range(2):
                jb = cb * 2 + sub
                for ci, (c0, n) in enumerate(((0, 512), (512, 512), (NT, NS))):
                    if kind == 'a':
                        p = PS[(sub * 3 + ci) % 6]
                    else:
                        p = PS[6 + (sub * 3 + ci) % 2]
                    for k in range(nk):
                        wp = wpart(wb3, k)
                        kb.op('pe', lambda e, k=k, wp=wp, p=p: e.matmul(p[:, 0:n], wp[:, k, sub * 128:(sub + 1) * 128], src[:, k, c0:c0 + n], start=(k == 0), stop=(k == nk - 1)), [wp.t, src], [p])
                    if kind == 'a':
                        pa_t[(sub, ci)] = p
                    else:
                        m = mc['i'] % 2; mc['i'] += 1
                        pa = pa_t[(sub, ci)]
                        kb.dma('sp', sga[m][:, 0:n], sg_d[jb * 128:(jb + 1) * 128, c0:c0 + n], [sg_d], [sga[m]], prim=sga[m])
                        kb.dma('sp', sgb[m][:, 0:n], sg_d[2048 + jb * 128:2048 + (jb + 1) * 128, c0:c0 + n], [sg_d], [sgb[m]], prim=sgb[m])
                        kb.op('dve', lambda e, pa=pa, m=m: e.scalar_tensor_tensor(out=t1[m][:, 0:n], in0=pa[:, 0:n], scalar=bo[:, 0, jb:jb + 1], in1=sga[m][:, 0:n], op0=ALU.add, op1=ALU.mult), [pa, bo, sga[m]], [t1[m]])
                        kb.op('dve', lambda e, p=p, m=m: e.scalar_tensor_tensor(out=t2_[m][:, 0:n], in0=p[:, 0:n], scalar=bo[:, 1, jb:jb + 1], in1=sgb[m][:, 0:n], op0=ALU.add, op1=ALU.mult), [p, bo, sgb[m]], [t2_[m]])
                        kb.op('pool', lambda e, m=m: e.tensor_tensor(out=sT[:, jb, c0:c0 + n], in0=t1[m][:, 0:n], in1=t2_[m][:, 0:n], op=ALU.add), [t1[m], t2_[m]], [sT])
        g1bc = kb.sb("g1bc", [128, D], F32, ph); g1bs = kb.sb("g1bs", [NS, D], F32, ph)
        kb.dma('pool', g1bc[:], mod_d[0:1, 2 * D:3 * D].partition_broadcast(128), [mod_d], [g1bc], prim=g1bc)
        kb.dma('pool', g1bs[:], mod_d[1:2, 2 * D:3 * D].partition_broadcast(NS), [mod_d], [g1bs], prim=g1bs)
        xp_ = [kb.sb("xp%d" % i, [128, 256], F32, ph) for i in range(3)]
        xo_ = [kb.sb("xo%d" % i, [128, 256], F32, ph) for i in range(3)]
        wo_i = {'i': 0}

        def issue_o(cb):
            st = stg[cb % 2]
            kb.dma('sp', st[:, :, :], w_o[:, cb * 256:(cb + 1) * 256].rearrange("(k p) n -> p k n", p=128), [], [st], prim=st)
        issue_o(0)
        for cb in range(8):
            if cb + 1 < 8:
                issue_o(cb + 1)
            st, wb3 = stg[cb % 2], wbs[cb % 2]
            cast_block(st, wb3)
            for t in range(9):
                rows = 128 if t < 8 else NS
                tok0 = t * 128
                p = PS[t % 6]
                for k in range(16):
                    wp = wpart(wb3, k)
                    kb.op('pe', lambda e, k=k, wp=wp, p=p: e.matmul(p[0:rows, 0:256], sT[:, k, tok0:tok0 + rows], wp[:, k, :], start=(k == 0), stop=(k == 15)), [wp.t, sT], [p])
                m = wo_i['i'] % 3; wo_i['i'] += 1
                xsrc = xh[HALO + tok0:HALO + tok0 + rows, cb * 256:(cb + 1) * 256] if t < 8 else xs[:, cb * 256:(cb + 1) * 256]
                gb = g1bc if t < 8 else g1bs
                kb.dma('sp', xp_[m][0:rows, :], xsrc, [], [xp_[m]], prim=xp_[m])
                kb.op('dve', lambda e, p=p, m=m, gb=gb: e.tensor_tensor(out=xo_[m][0:rows, :], in0=p[0:rows, 0:256], in1=gb[0:rows, cb * 256:(cb + 1) * 256], op=ALU.mult), [p, gb], [xo_[m]])
                kb.op('pool', lambda e, m=m: e.tensor_tensor(out=xo_[m][0:rows, :], in0=xo_[m][0:rows, :], in1=xp_[m][0:rows, :], op=ALU.add), [xo_[m], xp_[m]], [xo_[m]])
                kb.dma('pool', x1_d[tok0:tok0 + rows, cb * 256:(cb + 1) * 256], xo_[m][0:rows, :], [xo_[m]], [x1_d], prim=xo_[m])
        kb.barrier()
        ph.close()


    mid.close()
    AX = mybir.AxisListType.X
    NE = int(os.environ.get('KNE', '32'))
    late = ExitStack()
    h2T = kb.sb("h2T", [128, 16, NTS], BF16, late)
    gateM = kb.sb("gateM", [128, 9, 32], F32, late)
    kb.op('dve', lambda e: e.memset(gateM[:], 0.0), [], [gateM])
    ph = ExitStack()
    A2 = [kb.sb("A2_%d" % r, [128 if r == 0 else NS, D], F32, ph) for r in range(2)]
    B2 = [kb.sb("B2_%d" % r, [128 if r == 0 else NS, D], F32, ph) for r in range(2)]
    g2bc = kb.sb("g2bc", [128, D], F32, ph)
    kb.dma('pool', g2bc[:], g2_row[:, :].partition_broadcast(128), [], [g2bc], prim=g2bc)
    for r in range(2):
        n = 128 if r == 0 else NS
        kb.dma('pool', A2[r][:], mod_d[r:r + 1, 4 * D:5 * D].partition_broadcast(n), [mod_d], [A2[r]], prim=A2[r])
        kb.dma('pool', B2[r][:], mod_d[r:r + 1, 3 * D:4 * D].partition_broadcast(n), [mod_d], [B2[r]], prim=B2[r])
        kb.op('dve', lambda e, r=r, n=n: e.scalar_tensor_tensor(out=A2[r][:], in0=A2[r][:], scalar=1.0, in1=g2bc[0:n, :], op0=ALU.add, op1=ALU.mult), [A2[r], g2bc], [A2[r]])
    wr = kb.sb("wr", [128, 16, 32], F32, ph); kb.dma('sp', wr[:], w_router[:, :].rearrange("(k p) e -> p k e", p=128), [], [wr], prim=wr)
    brbc = kb.sb("brbc", [128, 32], F32, ph); kb.dma('pool', brbc[:], b_router[:, :].partition_broadcast(128), [], [brbc], prim=brbc)
    x1t = [kb.sb("x1t%d" % i, [128, D], F32, ph) for i in range(2)]
    hn = [kb.sb("hn%d" % i, [128, D], F32, ph) for i in range(2)]
    h2f = [kb.sb("h2f%d" % i, [128, 16, 128], F32, ph) for i in range(2)]
    junk2 = kb.sb("junk2", [128, D], BF16, ph)
    sm = {n: kb.sb("sm_" + n, [128, w], F32, ph) for n, w in (("ss", 1), ("rs", 1), ("lg", 32), ("mx", 8), ("mk", 32), ("nm", 1), ("ex", 32), ("su", 1))}
    for t in range(9):
        rows = 128 if t < 8 else NS
        tok0 = t * 128
        r = 0 if t < 8 else 1
        x, h_, hf = x1t[t % 2], hn[t % 2], h2f[t % 2]
        ss, rs = sm["ss"], sm["rs"]
        kb.dma('sp', x[0:rows, :], x1_d[tok0:tok0 + rows, :], [x1_d], [x], prim=x)
        kb.op('act', lambda e: e.activation(out=junk2[0:rows, :], in_=x[0:rows, :], func=AF.Square, accum_out=ss[0:rows, :]), [x], [junk2, ss])
        kb.op('dve', lambda e: e.tensor_scalar(out=ss[0:rows, :], in0=ss[0:rows, :], scalar1=1.0 / D, scalar2=1e-6, op0=ALU.mult, op1=ALU.add), [ss], [ss])
        kb.op('act', lambda e: e.activation(out=ss[0:rows, :], in_=ss[0:rows, :], func=AF.Sqrt), [ss], [ss])
        kb.op('dve', lambda e: e.reciprocal(out=rs[0:rows, :], in_=ss[0:rows, :]), [ss], [rs])
        kb.op('act', lambda e: e.activation(out=h_[0:rows, :], in_=x[0:rows, :], func=AF.Copy, scale=rs[0:rows, :]), [x, rs], [h_])
        kb.op('dve', lambda e: e.tensor_tensor(out=h_[0:rows, :], in0=h_[0:rows, :], in1=A2[r][0:rows, :], op=ALU.mult), [h_, A2[r]], [h_])
        kb.op('pool', lambda e: e.tensor_tensor(out=h_[0:rows, :], in0=h_[0:rows, :], in1=B2[r][0:rows, :], op=ALU.add), [h_, B2[r]], [h_])
        for kq in range(4):
            p = PS[kq]
            for kk in range(4):
                k = kq * 4 + kk
                kb.op('pe', lambda e, k=k, kk=kk, p=p: e.transpose(p[:, kk * 128:kk * 128 + rows], h_[0:rows, k * 128:(k + 1) * 128], ident[0:rows, 0:rows]), [h_, ident], [p])
            pv = p[:, :].rearrange("p (k t) -> p k t", k=4)
            kb.op('act', lambda e, pv=pv, kq=kq: e.activation(out=h2T[:, 4 * kq:4 * kq + 4, tok0:tok0 + rows], in_=pv[:, :, 0:rows], func=AF.Copy), [p], [h2T])
            kb.op('dve', lambda e, pv=pv, kq=kq: e.tensor_copy(out=hf[:, 4 * kq:4 * kq + 4, 0:rows], in_=pv[:, :, 0:rows]), [p], [hf])
        pr = PS[4 + t % 2]
        for k in range(16):
            kb.op('pe', lambda e, k=k: e.matmul(pr[0:rows, 0:32], hf[:, k, 0:rows], wr[:, k, :], start=(k == 0), stop=(k == 15)), [hf, wr], [pr])
        lg, mx, mk, nm, ex, su = sm["lg"], sm["mx"], sm["mk"], sm["nm"], sm["ex"], sm["su"]
        kb.op('dve', lambda e: e.tensor_tensor(out=lg[0:rows, :], in0=pr[0:rows, 0:32], in1=brbc[0:rows, :], op=ALU.add), [pr, brbc], [lg])
        kb.op('dve', lambda e: e.max(out=mx[0:rows, :], in_=lg[0:rows, :]), [lg], [mx])
        kb.op('dve', lambda e: e.tensor_scalar(out=mk[0:rows, :], in0=lg[0:rows, :], scalar1=mx[0:rows, 3:4], scalar2=None, op0=ALU.is_ge), [lg, mx], [mk])
        kb.op('dve', lambda e: e.tensor_scalar(out=nm[0:rows, :], in0=mx[0:rows, 0:1], scalar1=-1.0, scalar2=None, op0=ALU.mult), [mx], [nm])
        kb.op('act', lambda e: e.activation(out=ex[0:rows, :], in_=lg[0:rows, :], func=AF.Exp, bias=nm[0:rows, :]), [lg, nm], [ex])
        kb.op('dve', lambda e: e.tensor_tensor(out=ex[0:rows, :], in0=ex[0:rows, :], in1=mk[0:rows, :], op=ALU.mult), [ex, mk], [ex])
        kb.op('dve', lambda e: e.reduce_sum(out=su[0:rows, :], in_=ex[0:rows, :], axis=AX), [ex], [su])
        kb.op('dve', lambda e: e.reciprocal(out=su[0:rows, :], in_=su[0:rows, :]), [su], [su])
        kb.op('dve', lambda e: e.tensor_scalar(out=gateM[0:rows, t, :], in0=ex[0:rows, :], scalar1=su[0:rows, 0:1], scalar2=None, op0=ALU.mult), [ex, su], [gateM])
    kb.barrier()
    ph.close()

    late2 = ExitStack()
    accM = kb.sb("accM", [128, 9, D], F32, late2)
    ph = ExitStack()
    pi = ExitStack()
    b2 = kb.sb("b2", [32, D], F32, pi); kb.dma('sp', b2[:], b_moe2[:, :], [], [b2], prim=b2)
    gT = kb.sb("gT", [32, 128], F32, pi)
    for t in range(9):
        pt = PS[6]
        kb.op('pe', lambda e, t=t, pt=pt: e.transpose(pt[0:32, 0:128], gateM[:, t, :], ident[:]), [gateM, ident], [pt])
        kb.op('act', lambda e, pt=pt: e.activation(out=gT[:, :], in_=pt[0:32, 0:128], func=AF.Copy), [pt], [gT])
        for c4 in range(4):
            p = PS[c4]
            kb.op('pe', lambda e, p=p, c4=c4: e.matmul(p[:, :], gT[:, :], b2[:, c4 * 512:(c4 + 1) * 512], start=True, stop=True), [gT, b2], [p])
            kb.op('act', lambda e, p=p, c4=c4, t=t: e.activation(out=accM[:, t, c4 * 512:(c4 + 1) * 512], in_=p[:, :], func=AF.Copy), [p], [accM])
    kb.barrier()
    pi.close()
    actT = kb.sb("actT", [128, 8, NTS], BF16, ph)
    b1 = kb.sb("b1", [128, 32, 32], F32, ph); kb.dma('sp', b1[:], b1T_d[:], [], [b1], prim=b1)
    USPL = [(0, 3, 'act'), (3, 6, 'dve'), (6, 8, 'pool')]
    stu = [kb.sb("stu%d" % i, [128, 8, 512], F32, ph) for i in range(2)]
    wbu = [[kb.sb("wbu%d_%d" % (i, j), [128, USPL[j][1] - USPL[j][0], 512], BF16, ph) for j in range(3)] for i in range(4)]
    gsT = [kb.sb("gsT%d" % i, [128, NTS], BF16, ph) for i in range(4)]
    gtr = [kb.sb("gt%d" % i, [128, 512], F32, ph) for i in range(2)]
    ltr = [kb.sb("lt%d" % i, [128, 512], F32, ph) for i in range(2)]
    ri = {'g': 0, 'l': 0}
    kb.op('dve', lambda e: e.tensor_scalar(out=gateM[:], in0=gateM[:], scalar1=1.0 / 1.702, scalar2=None, op0=ALU.mult), [gateM], [gateM])
    CH = ((0, 512), (512, 512), (NT, NS))
    units = []
    for ex_ in range(NE):
        for hf in range(2):
            for jq in range(2):
                c = hf * 1024 + jq * 512
                for kh in range(2):
                    units.append(w_moe1[ex_, kh * 1024:(kh + 1) * 1024, c:c + 512])
                for kh in range(2):
                    units.append(w_moe1[ex_, kh * 1024:(kh + 1) * 1024, 2048 + c:2048 + c + 512])
            for cb in range(4):
                units.append(w_moe2[ex_, hf * 1024:(hf + 1) * 1024, cb * 512:(cb + 1) * 512])
    us = {'issued': 0, 'cast': 0, 'p': 0}

    def u_issue():
        i = us['issued']
        if i < len(units):
            kb.dma('sp', stu[i % 2][:, :, :], units[i].rearrange("(k p) n -> p k n", p=128), [], [stu[i % 2]], prim=stu[i % 2])
            us['issued'] += 1

    def u_next():
        i = us['cast']; us['cast'] += 1
        while us['issued'] < min(i + 2, len(units)):
            u_issue()
        st, w3 = stu[i % 2], wbu[i % 4]
        for j, (k0, k1, en) in enumerate(USPL):
            if en == 'act':
                kb.op('act', lambda e, j=j, k0=k0, k1=k1: e.activation(out=w3[j][:, :, :], in_=st[:, k0:k1, :], func=AF.Copy), [st], [w3[j]])
            else:
                kb.op(en, lambda e, j=j, k0=k0, k1=k1: e.tensor_copy(out=w3[j][:, :, :], in_=st[:, k0:k1, :]), [st], [w3[j]])
        return w3

    def upart(w3, k):
        for j, (k0, k1, en) in enumerate(USPL):
            if k0 <= k < k1:
                return w3[j], k - k0

    def nps():
        us['p'] += 1
        return PS[us['p'] % 6]

    def w1_mm(U0, U1, sub, c0, n):
        p = nps()
        for k in range(16):
            tl, kl = upart(U0 if k < 8 else U1, k % 8)
            kb.op('pe', lambda e, k=k, tl=tl, kl=kl: e.matmul(p[:, 0:n], tl[:, kl, sub * 128:(sub + 1) * 128], h2T[:, k, c0:c0 + n], start=(k == 0), stop=(k == 15)), [tl, h2T], [p])
        return p

    u_issue()
    blocks = []
    for ex_ in range(NE):
        for hf in range(2):
            for jq in range(2):
                blocks.append(('g', ex_, hf, jq, 2)); blocks.append(('l', ex_, hf, jq, 2))
            for cb in range(4):
                blocks.append(('w', ex_, hf, cb, 1))
    pre = {}

    def ensure(bi):
        if bi < len(blocks) and bi not in pre:
            pre[bi] = [u_next() for _ in range(blocks[bi][4])]

    for bi, (kind, ex_, hf, x_, nu) in enumerate(blocks):
        ensure(bi)
        ensure(bi + 1)
        U = pre.pop(bi)
        if kind == 'g':
            jq = x_
            for sub in range(4):
                jb = hf * 8 + jq * 4 + sub
                for (c0, n) in CH:
                    p = w1_mm(U[0], U[1], sub, c0, n)
                    g_ = gtr[ri['g'] % 2]; ri['g'] += 1
                    kb.op('dve', lambda e, p=p, g_=g_: e.tensor_scalar(out=g_[:, 0:n], in0=p[:, 0:n], scalar1=b1[:, ex_, jb:jb + 1], scalar2=7.0, op0=ALU.add, op1=ALU.min), [p, b1], [g_])
                    kb.op('act', lambda e, g_=g_: e.activation(out=gsT[sub][:, c0:c0 + n], in_=g_[:, 0:n], func=AF.Silu, scale=1.702), [g_], [gsT[sub]])
        elif kind == 'l':
            jq = x_
            for sub in range(4):
                jb = hf * 8 + jq * 4 + sub
                for (c0, n) in CH:
                    p = w1_mm(U[0], U[1], sub, c0, n)
                    l_ = ltr[ri['l'] % 2]; ri['l'] += 1
                    kb.op('act', lambda e, p=p, l_=l_: e.activation(out=l_[:, 0:n], in_=p[:, 0:n], func=AF.Identity, bias=b1[:, ex_, 16 + jb:16 + jb + 1]), [p, b1], [l_])
                    kb.op('dve', lambda e, l_=l_: e.tensor_scalar(out=l_[:, 0:n], in0=l_[:, 0:n], scalar1=7.0, scalar2=-7.0, op0=ALU.min, op1=ALU.max), [l_], [l_])
                    kb.op('dve', lambda e, l_=l_: e.scalar_tensor_tensor(out=actT[:, jq * 4 + sub, c0:c0 + n], in0=l_[:, 0:n], scalar=1.0, in1=gsT[sub][:, c0:c0 + n], op0=ALU.add, op1=ALU.mult), [l_, gsT[sub]], [actT])
        else:
            cb = x_
            for t in range(9):
                rows = 128 if t < 8 else NS
                tok0 = t * 128
                p = nps()
                for k in range(8):
                    tl, kl = upart(U[0], k)
                    kb.op('pe', lambda e, k=k, tl=tl, kl=kl, p=p: e.matmul(p[0:rows, 0:512], actT[:, k, tok0:tok0 + rows], tl[:, kl, :], start=(k == 0), stop=(k == 7)), [tl, actT], [p])
                kb.op('dve', lambda e, p=p, t=t: e.scalar_tensor_tensor(out=accM[0:rows, t, cb * 512:(cb + 1) * 512], in0=p[0:rows, 0:512], scalar=gateM[0:rows, t, ex_:ex_ + 1], in1=accM[0:rows, t, cb * 512:(cb + 1) * 512], op0=ALU.mult, op1=ALU.add), [p, gateM, accM], [accM])
    kb.barrier()
    ph.close()

    ph = ExitStack()
    g5 = [kb.sb("g5_%d" % r, [128 if r == 0 else NS, D], F32, ph) for r in range(2)]
    gfbc = kb.sb("gfbc", [128, D], F32, ph)
    kb.dma('pool', gfbc[:], gf_row[:, :].partition_broadcast(128), [], [gfbc], prim=gfbc)
    for r in range(2):
        kb.dma('pool', g5[r][:], mod_d[r:r + 1, 5 * D:6 * D].partition_broadcast(128 if r == 0 else NS), [mod_d], [g5[r]], prim=g5[r])
    x1t = [kb.sb("x1f%d" % i, [128, D], F32, ph) for i in range(2)]
    yo = [kb.sb("yo%d" % i, [128, D], F32, ph) for i in range(2)]
    junk3 = kb.sb("junk3", [128, D], BF16, ph)
    ss2 = kb.sb("ss2", [128, 1], F32, ph); rs2 = kb.sb("rs2", [128, 1], F32, ph)
    for t in range(9):
        rows = 128 if t < 8 else NS
        tok0 = t * 128
        r = 0 if t < 8 else 1
        x, y = x1t[t % 2], yo[t % 2]
        kb.dma('sp', x[0:rows, :], x1_d[tok0:tok0 + rows, :], [x1_d], [x], prim=x)
        kb.op('dve', lambda e: e.tensor_tensor(out=y[0:rows, :], in0=accM[0:rows, t, :], in1=g5[r][0:rows, :], op=ALU.mult), [accM, g5[r]], [y])
        kb.op('pool', lambda e: e.tensor_tensor(out=y[0:rows, :], in0=y[0:rows, :], in1=x[0:rows, :], op=ALU.add), [y, x], [y])
        kb.op('act', lambda e: e.activation(out=junk3[0:rows, :], in_=y[0:rows, :], func=AF.Square, accum_out=ss2[0:rows, :]), [y], [junk3, ss2])
        kb.op('dve', lambda e: e.tensor_scalar(out=ss2[0:rows, :], in0=ss2[0:rows, :], scalar1=1.0 / D, scalar2=1e-6, op0=ALU.mult, op1=ALU.add), [ss2], [ss2])
        kb.op('act', lambda e: e.activation(out=ss2[0:rows, :], in_=ss2[0:rows, :], func=AF.Sqrt), [ss2], [ss2])
        kb.op('dve', lambda e: e.reciprocal(out=rs2[0:rows, :], in_=ss2[0:rows, :]), [ss2], [rs2])
        kb.op('act', lambda e: e.activation(out=y[0:rows, :], in_=y[0:rows, :], func=AF.Copy, scale=rs2[0:rows, :]), [y, rs2], [y])
        kb.op('dve', lambda e: e.tensor_tensor(out=y[0:rows, :], in0=y[0:rows, :], in1=gfbc[0:rows, :], op=ALU.mult), [y, gfbc], [y])
        if t < 8:
            kb.dma('pool', y_o[tok0:tok0 + 128, :], y[:, :], [y], [y_o], prim=y)
        else:
            kb.dma('pool', ys_o[:, :], y[0:NS, :], [y], [ys_o], prim=y)
    kb.finish()
    ph.close()
    late2.close()
    late.close()
    es.close()
    return nc


_NC = None
CORES = list(range(NCORES))


def kernel(**inp):
    global _NC
    f = lambda a: np.ascontiguousarray(np.asarray(a, dtype=np.float32))
    xp = f(inp['x_prompt']); xs = f(inp['x_sample'])
    shared = {
        "ident": np.eye(128, dtype=np.float32),
        "w_ada": f(inp['w_ada'][0]), "b_adaT": f(inp['b_ada'][0].reshape(96, 128).T), "b_ada_row": f(inp['b_ada'][0].reshape(1, -1)),
        "g1T": f(inp['g_norm1'][0].reshape(16, 128).T),
        "w_in": f(inp['w_in'][0]), "b_inT": f(inp['b_in'][0].reshape(128, 128).T), "b_in_row": f(inp['b_in'][0].reshape(1, -1)),
    }
    shared["w_dwT"] = f(inp['w_dw'][0].T.reshape(12, 128, 31).transpose(1, 0, 2))
    shared["cparT"] = f(np.stack([inp['b_dw'][0].reshape(12, 128).T, inp['g_ln_conv'][0].reshape(12, 128).T, inp['b_ln_conv'][0].reshape(12, 128).T], axis=1))
    shared["w_ao"] = f(inp['w_attn_out'][0]); shared["w_co"] = f(inp['w_conv_out'][0]); shared["w_o"] = f(inp['w_o'][0])
    shared["boT"] = f(np.stack([inp['b_attn_out'][0].reshape(16, 128).T, inp['b_conv_out'][0].reshape(16, 128).T], axis=1))
    shared["g2_row"] = f(inp['g_norm2'][0].reshape(1, -1)); shared["w_router"] = f(inp['w_router'][0]); shared["b_router"] = f(inp['b_router'][0].reshape(1, -1))
    shared["w_moe1"] = f(inp['w_moe1'][0]); shared["w_moe2"] = f(inp['w_moe2'][0]); shared["b_moe2"] = f(inp['b_moe2'][0]); shared["gf_row"] = f(inp['g_final'].reshape(1, -1))
    shared["b1T"] = f(inp['b_moe1'][0].reshape(32, 32, 128).transpose(2, 0, 1))
    pp = np.arange(128)[:, None]; nn = np.arange(256)[None, :]
    shared["M1"] = ((nn - pp >= 0) & (nn - pp <= 128)).astype(np.float32)
    SMc = np.zeros((128, 21, NS), np.float32); SMn = np.zeros((NS, 3, NS), np.float32)
    ti = 0
    for g, (W, dd) in enumerate(((128, 1), (512, 4), (2048, 16))):
        for j in range(W // 128):
            m = 128 * j + np.arange(128)[:, None]; ii = np.arange(NS)[None, :]
            SMc[:, ti, :] = (((ii - m) % dd == 0) & (m >= ii)).astype(np.float32)
            ti += 1
        i1 = np.arange(NS)[:, None]; ii = np.arange(NS)[None, :]
        SMn[:, g, :] = ((i1 <= ii) & ((ii - i1) % dd == 0)).astype(np.float32)
    shared["SMc"] = SMc; shared["SMn"] = SMn
    in_maps = []
    for c in CORES:
        b, q = c // 4, c % 4
        s = q * NT
        xh = np.zeros((HALO + NT, D), np.float32)
        lo = max(0, s - HALO)
        xh[HALO - (s - lo):] = xp[b, lo:s + NT]
        cT = np.stack([f(inp['c_prompt'][b]).reshape(16, 128).T, f(inp['c_sample'][c]).reshape(16, 128).T], axis=-1)
        m = dict(shared)
        kval = np.zeros((128, 53), np.float32)
        p1 = np.arange(128)
        for j in range(9):
            kval[:, j] = (s - 128 + 128 * j + p1 >= 0)
        for r in range(4):
            for j in range(3):
                kval[:, 9 + r * 3 + j] = (s - 512 + 512 * j + 4 * p1 + r >= 0)
        for r in range(16):
            kval[:, 21 + 2 * r] = (s - 2048 + 16 * p1 + r >= 0)
            kval[:, 21 + 2 * r + 1] = (s - 2048 + 16 * (128 + p1) + r >= 0)
        m.update({"xh": xh, "xs": f(xs[c]), "cT": f(cT), "kval": kval, "hprev": np.full((128, 1), 1.0 if q > 0 else 0.0, np.float32),
                  "stT": f(inp['state_conv'][0, c].T), "st": f(inp['state_conv'][0, c]),
                  "cache0": f(inp['cache_kv_w128'][0, c]), "cache1": f(inp['cache_kv_w512'][0, c]), "cache2": f(inp['cache_kv_w2048'][0, c])})
        in_maps.append(m)
    if _NC is None:
        _NC = build()
    res = run_bass_kernel_spmd(_NC, in_maps, core_ids=list(range(len(CORES)))).results
    B, S = 2, 4096
    y_p = np.zeros((B, S, D), np.float32); y_s = np.zeros((8, NS, D), np.float32)
    kvp = [np.zeros((1, B, w, 2, 8, 128), np.float32) for w in (128, 512, 2048)]
    kvs = [np.zeros((1, 8, NS, 2, 8, 128), np.float32) for _ in range(3)]
    conv_p = np.zeros((1, B, 30, 1536), np.float32); conv_s = np.zeros((1, 8, 30, 1536), np.float32)
    for ci, c in enumerate(CORES):
        b, q = c // 4, c % 4
        r = res[ci]
        y_p[b, q * NT:(q + 1) * NT] = r["y_o"]
        y_s[c] = r["ys_o"]
        if q == 3:
            kvp[0][0, b] = r["kv1_o"]; kvp[1][0, b] = r["kv2_o"]; conv_p[0, b] = r["convp_o"]
        if q >= 2:
            kvp[2][0, b, (q - 2) * NT:(q - 1) * NT] = r["kv3_o"]
        for g in range(3):
            kvs[g][0, c] = r["kvs_o"][g]
        conv_s[0, c] = r["convs_o"]
    return (y_p, y_s, kvp[0], kvp[1], kvp[2], conv_p, kvs[0], kvs[1], kvs[2], conv_s)
```
